# Optimizing a Trainium2 kernel written in Bass

```python
import jax
import jax.numpy as jnp
from jax import lax
import numpy as np

D_MODEL = 1024
BATCH = 2
SEQ = 8192
DEPTH = 2

GRID_W = 64
CTX_LEN = 256
EPS = 1e-6

N_BRANCH = 4
BRANCH_W = D_MODEL // 2
CHUNK = GRID_W
CONV_W = 4
CONV_LEFT = 2

RG_W = BRANCH_W
RG_BLOCKS = 8
RG_BW = RG_W // RG_BLOCKS
RG_C = 8.0
GLA_H = 4
GLA_DK = BRANCH_W // (2 * GLA_H)
GLA_DV = BRANCH_W // GLA_H
GLA_RANK = 16
GLA_GATE_NORM = 16.0
HG_H = 4
HG_DK = 128
HG_DV = BRANCH_W // HG_H
ML_H = 4
ML_DK = BRANCH_W // ML_H
ML_DV = BRANCH_W // ML_H
N_GROUPS = 4
EXPERTS_PER_GROUP = 4
N_EXPERTS = N_GROUPS * EXPERTS_PER_GROUP
TOP_K = 2
D_EXPERT = D_MODEL // 2
MOE_BLOCK = 128

IN_PARTS = (
    ('rg_x', RG_W), ('rg_y', RG_W),
    ('gla_q', GLA_H * GLA_DK), ('gla_k', GLA_H * GLA_DK), ('gla_v', GLA_H * GLA_DV),
    ('gla_g', GLA_H * GLA_DV), ('gla_lr', 2 * GLA_RANK),
    ('hg_q', HG_H * HG_DK), ('hg_i', HG_H * HG_DV), ('hg_f', 2 * HG_H * HG_DK), ('hg_g', HG_H * HG_DV),
    ('ml_q', ML_H * ML_DK), ('ml_k', ML_H * ML_DK), ('ml_v', ML_H * ML_DV), ('ml_o', ML_H * ML_DV),
    ('ml_if', 2 * 2 * ML_H),
)
IN_WIDTH = sum(w for _, w in IN_PARTS)

kernel_name = 'hybrid_bidir_recurrent_hmoe_dit'


def _rmsnorm(x, g):
    xf = x.astype(jnp.float32)
    y = xf * lax.rsqrt(jnp.mean(jnp.square(xf), axis=-1, keepdims=True) + EPS)
    return (y * g.astype(jnp.float32)).astype(x.dtype)


def _modulate(h, g, shift, scale):
    return _rmsnorm(h, g) * (1.0 + scale) + shift


def _split_in(z):
    parts, off = {}, 0
    for name, width in IN_PARTS:
        parts[name] = z[..., off:off + width]
        off += width
    return parts


def _seg_flip(a, n_ctx, axis):
    ctx_part, lat_part = jnp.split(a, [n_ctx], axis=axis)
    return jnp.concatenate([jnp.flip(ctx_part, axis), jnp.flip(lat_part, axis)], axis=axis)


def _bidirectional(scan_fn, n_ctx, axis, fwd_args, bwd_args):
    y_fwd = scan_fn(*fwd_args)
    y_bwd = scan_fn(*[_seg_flip(a, n_ctx, axis) for a in bwd_args])
    return y_fwd + _seg_flip(y_bwd, n_ctx, axis)


def _dwconv_centred(u, w, b):
    t = u.shape[1]
    up = jnp.pad(u, ((0, 0), (CONV_LEFT, CONV_W - 1 - CONV_LEFT), (0, 0)))
    return b + sum(w[j] * up[:, j:j + t] for j in range(CONV_W))


def _conv_two_seqs(u, n_ctx, w, b):
    return jnp.concatenate([_dwconv_centred(u[:, :n_ctx], w, b), _dwconv_centred(u[:, n_ctx:], w, b)], axis=1)


def _heads(a, h):
    bsz, n, _ = a.shape
    return a.reshape(bsz, n, h, -1).transpose(0, 2, 1, 3)


def _unheads(a):
    bsz, h, n, d = a.shape
    return a.transpose(0, 2, 1, 3).reshape(bsz, n, h * d)


def _to_chunks(a):
    bsz, h, n = a.shape[:3]
    return jnp.moveaxis(a.reshape((bsz, h, n // CHUNK, CHUNK) + a.shape[3:]), 2, 0)


def _from_chunks(a):
    nc, bsz, h, l = a.shape[:4]
    return jnp.moveaxis(a, 0, 2).reshape((bsz, h, nc * l) + a.shape[4:])


def _lin_combine(left, right):
    a_l, b_l = left
    a_r, b_r = right
    return a_l * a_r, a_r * b_l + b_r


def _linear_scan(a, bterm):
    _, h = lax.associative_scan(_lin_combine, (a, bterm), axis=1)
    return h


def _gated_linear_scan(q, k, v, log_g):
    bsz, h, _, dk = q.shape
    dv = v.shape[-1]
    tril = jnp.tril(jnp.ones((CHUNK, CHUNK), dtype=bool))

    def chunk_step(state, blk):
        qc, kc, vc, gc = blk
        b = jnp.cumsum(gc, axis=2)
        rel = jnp.where(tril[:, :, None], b[:, :, :, None, :] - b[:, :, None, :, :], -jnp.inf)
        scores = jnp.einsum('bhid,bhjd,bhijd->bhij', qc, kc, jnp.exp(rel))
        o = (jnp.einsum('bhij,bhjv->bhiv', scores, vc)
             + jnp.einsum('bhid,bhdv->bhiv', qc * jnp.exp(b), state))
        b_end = b[:, :, -1:, :]
        state = (jnp.exp(b_end[:, :, 0, :, None]) * state
                 + jnp.einsum('bhjd,bhjv->bhdv', kc * jnp.exp(b_end - b), vc))
        return state, o

    s0 = jnp.zeros((bsz, h, dk, dv), jnp.float32)
    _, o = lax.scan(chunk_step, s0, tuple(_to_chunks(a.astype(jnp.float32)) for a in (q, k, v, log_g)))
    return _from_chunks(o)


def _mlstm_scan(q, k, v, i_pre, log_f):
    bsz, h, _, dk = q.shape
    dv = v.shape[-1]
    tril = jnp.tril(jnp.ones((CHUNK, CHUNK), dtype=bool))

    def chunk_step(carry, blk):
        c_mat, n_vec, m_prev = carry
        qc, kc, vc, ic, fc = blk
        b = jnp.cumsum(fc, axis=-1)
        log_w = jnp.where(tril, b[..., :, None] - b[..., None, :] + ic[..., None, :], -jnp.inf)
        log_inter = b + m_prev[..., None]
        m = jnp.maximum(log_inter, jnp.max(log_w, axis=-1))
        w_inter = jnp.exp(log_inter - m)
        s = jnp.einsum('bhid,bhjd->bhij', qc, kc) * jnp.exp(log_w - m[..., None])
        num = (jnp.einsum('bhij,bhjv->bhiv', s, vc)
               + w_inter[..., None] * jnp.einsum('bhid,bhdv->bhiv', qc, c_mat))
        den = jnp.sum(s, axis=-1) + w_inter * jnp.einsum('bhid,bhd->bhi', qc, n_vec)
        h_out = num / jnp.maximum(jnp.abs(den), jnp.exp(-m))[..., None]
        m_new = m[..., -1]
        w_end = jnp.exp(b[..., -1:] - b + ic - m_new[..., None])
        decay = jnp.exp(b[..., -1] + m_prev - m_new)
        c_mat = decay[..., None, None] * c_mat + jnp.einsum('bhj,bhjd,bhjv->bhdv', w_end, kc, vc)
        n_vec = decay[..., None] * n_vec + jnp.einsum('bhj,bhjd->bhd', w_end, kc)
        return (c_mat, n_vec, m_new), h_out

    init = (jnp.zeros((bsz, h, dk, dv), jnp.float32), jnp.zeros((bsz, h, dk), jnp.float32),
            jnp.zeros((bsz, h), jnp.float32))
    _, o = lax.scan(chunk_step, init, tuple(_to_chunks(a.astype(jnp.float32)) for a in (q, k, v, i_pre, log_f)))
    return _from_chunks(o)


def _rglru_coeffs(xb, gate_w, gate_b, lam):
    bsz, n, _ = xb.shape
    blk = xb.reshape(bsz, n, RG_BLOCKS, RG_BW)
    pre = (jnp.einsum('bnki,gkij->gbnkj', blk, gate_w.astype(jnp.float32)).reshape(2, bsz, n, RG_W)
           + gate_b.astype(jnp.float32)[:, None, None, :])
    r, i = jax.nn.sigmoid(pre[0]), jax.nn.sigmoid(pre[1])
    log_a = -RG_C * r * jax.nn.softplus(-lam.astype(jnp.float32))
    return jnp.exp(log_a), jnp.sqrt(-jnp.expm1(2.0 * log_a)) * (i * xb)


def _rglru_branch(p, lp, n_ctx):
    xb = _conv_two_seqs(p['rg_x'], n_ctx, lp['rg_conv_w'], lp['rg_conv_b']).astype(jnp.float32)
    a_f, b_f = _rglru_coeffs(xb, lp['rg_gate_w'][0], lp['rg_gate_b'][0], lp['rg_lambda'][0])
    a_b, b_b = _rglru_coeffs(xb, lp['rg_gate_w'][1], lp['rg_gate_b'][1], lp['rg_lambda'][1])
    h = _bidirectional(_linear_scan, n_ctx, 1, (a_f, b_f), (a_b, b_b))
    return (jax.nn.gelu(p['rg_y'].astype(jnp.float32)) * h).astype(p['rg_x'].dtype)


def _gla_branch(p, lp, n_ctx):
    f32 = jnp.float32
    bsz, n, _ = p['gla_q'].shape
    q = _heads(p['gla_q'].astype(f32), GLA_H) * GLA_DK ** -0.5
    k = _heads(p['gla_k'].astype(f32), GLA_H)
    v = _heads(p['gla_v'].astype(f32), GLA_H)
    lr = p['gla_lr'].astype(f32).reshape(bsz, n, 2, GLA_RANK)
    pre = (jnp.einsum('bndr,drk->dbnk', lr, lp['gla_w_lr'].astype(f32))
           + lp['gla_b_lr'].astype(f32)[:, None, None, :])
    log_a = jax.nn.log_sigmoid(pre) / GLA_GATE_NORM
    o = _bidirectional(_gated_linear_scan, n_ctx, 2,
                       (q, k, v, _heads(log_a[0], GLA_H)), (q, k, v, _heads(log_a[1], GLA_H)))
    o = _rmsnorm(o, lp['gla_norm_g'])
    return (_unheads(o) * jax.nn.silu(p['gla_g'].astype(f32))).astype(p['gla_q'].dtype)


def _hgrn2_branch(p, lp, lb, n_ctx):
    f32 = jnp.float32
    bsz, n, _ = p['hg_q'].shape
    q = _heads(jax.nn.silu(p['hg_q'].astype(f32)), HG_H) * HG_DK ** -0.5
    v = _heads(p['hg_i'].astype(f32), HG_H)
    f = p['hg_f'].astype(f32).reshape(bsz, n, 2, HG_H * HG_DK)
    log_forget = jnp.logaddexp(jnp.log(lb), jnp.log1p(-lb) + jax.nn.log_sigmoid(f))
    key = (1.0 - lb) * jax.nn.sigmoid(-f)
    o = _bidirectional(_gated_linear_scan, n_ctx, 2,
                       (q, _heads(key[:, :, 0], HG_H), v, _heads(log_forget[:, :, 0], HG_H)),
                       (q, _heads(key[:, :, 1], HG_H), v, _heads(log_forget[:, :, 1], HG_H)))
    o = _rmsnorm(o, lp['hgrn_norm_g'])
    return (_unheads(o) * jax.nn.silu(p['hg_g'].astype(f32))).astype(p['hg_q'].dtype)


def _mlstm_branch(p, lp, n_ctx):
    f32 = jnp.float32
    bsz, n, _ = p['ml_q'].shape
    qk = _conv_two_seqs(jnp.concatenate([p['ml_q'], p['ml_k']], axis=-1), n_ctx, lp['ml_conv_w'], lp['ml_conv_b'])
    qk = jax.nn.silu(qk.astype(f32))
    q = _heads(qk[..., :ML_H * ML_DK], ML_H) * ML_DK ** -0.5
    k = _heads(qk[..., ML_H * ML_DK:], ML_H)
    v = _heads(p['ml_v'].astype(f32), ML_H)
    g = p['ml_if'].astype(f32).reshape(bsz, n, 2, 2, ML_H) + lp['ml_gate_b'].astype(f32)
    g = g.transpose(2, 3, 0, 4, 1)
    i_pre, log_f = g[:, 0], jax.nn.log_sigmoid(g[:, 1])
    h = _bidirectional(_mlstm_scan, n_ctx, 2, (q, k, v, i_pre[0], log_f[0]), (q, k, v, i_pre[1], log_f[1]))
    h = _rmsnorm(h, lp['ml_norm_g'])
    return (jax.nn.sigmoid(p['ml_o'].astype(f32)) * _unheads(h)).astype(p['ml_q'].dtype)


def _token_mixer(u, n_ctx, lp, lb, latent_only):
    p = _split_in(u @ lp['w_in'])
    ys = jnp.stack([_rglru_branch(p, lp, n_ctx), _gla_branch(p, lp, n_ctx),
                    _hgrn2_branch(p, lp, lb, n_ctx), _mlstm_branch(p, lp, n_ctx)], axis=2)
    if latent_only:
        ys, u = ys[:, n_ctx:], u[:, n_ctx:]
    bsz, n, _ = u.shape
    gates = jax.nn.sigmoid(u @ lp['w_merge'] + lp['b_merge']).reshape(bsz, n, N_BRANCH, D_MODEL)
    branch_out = jnp.einsum('bnkc,kcd->bnkd', ys, lp['w_branch'])
    return jnp.sum(gates * branch_out, axis=2) @ lp['w_out']


def _routed_experts(xf, experts, weights, w1, w3, w2):
    n_tok, d = xf.shape
    n_asg = n_tok * TOP_K
    n_blocks = (n_asg + N_EXPERTS * (MOE_BLOCK - 1) + MOE_BLOCK - 1) // MOE_BLOCK
    flat_e = experts.reshape(-1)
    order = jnp.argsort(flat_e)
    e_sorted = flat_e[order]
    counts = jnp.bincount(flat_e, length=N_EXPERTS)
    padded = (counts + MOE_BLOCK - 1) // MOE_BLOCK * MOE_BLOCK
    pad_end = jnp.cumsum(padded)
    seg_start = jnp.cumsum(counts) - counts
    slot = (pad_end - padded)[e_sorted] + jnp.arange(n_asg) - seg_start[e_sorted]
    tok_sorted = (order // TOP_K).astype(jnp.int32)
    slot_tok = jnp.full((n_blocks * MOE_BLOCK,), n_tok, jnp.int32).at[slot].set(tok_sorted)
    x_pad = jnp.concatenate([xf, jnp.zeros((1, d), xf.dtype)], axis=0)
    x_blocks = x_pad[slot_tok].reshape(n_blocks, MOE_BLOCK, d)
    block_expert = jnp.minimum(jnp.searchsorted(pad_end, jnp.arange(n_blocks) * MOE_BLOCK, side='right'),
                               N_EXPERTS - 1)

    def expert_mlp(args):
        xb, e = args
        return (jax.nn.silu(xb @ w1[e]) * (xb @ w3[e])) @ w2[e]

    y_slots = lax.map(expert_mlp, (x_blocks, block_expert)).reshape(-1, d)
    y_asg = (y_slots[slot] * weights.reshape(-1)[order][:, None]).astype(xf.dtype)
    return jnp.zeros_like(xf).at[tok_sorted].add(y_asg)


def _hier_moe(u, lp):
    bsz, n, d = u.shape
    xf = u.reshape(-1, d)
    n_tok = xf.shape[0]
    g_logits = (xf @ lp['moe_w_group'] + lp['moe_b_group']).astype(jnp.float32)
    g_top, grp = lax.top_k(g_logits, 1)
    p_grp = jnp.exp(g_top - jax.nn.logsumexp(g_logits, axis=-1, keepdims=True))
    e_logits = (xf @ lp['moe_w_expert'] + lp['moe_b_expert']).astype(jnp.float32)
    e_logits = e_logits.reshape(n_tok, N_GROUPS, EXPERTS_PER_GROUP)
    idx = jnp.broadcast_to(grp[:, :, None], (n_tok, 1, EXPERTS_PER_GROUP))
    e_in = jnp.take_along_axis(e_logits, idx, axis=1)[:, 0]
    e_top, e_idx = lax.top_k(e_in, TOP_K)
    weights = p_grp * jax.nn.softmax(e_top, axis=-1)
    experts = grp * EXPERTS_PER_GROUP + e_idx
    y = _routed_experts(xf, experts, weights, lp['moe_w1'], lp['moe_w3'], lp['moe_w2'])
    return y.reshape(bsz, n, d)


def _layer(h_ctx, h_lat, c, c_ctx, lp, lb, last):
    n_ctx = h_ctx.shape[1]
    mod_lat = jax.nn.silu(c) @ lp['ada_w'] + lp['ada_b']
    mod_ctx = jax.nn.silu(c_ctx) @ lp['ada_w'] + lp['ada_b']
    sh1_l, sc1_l, g1_l, sh2_l, sc2_l, g2_l = jnp.split(mod_lat[:, None, :], 6, axis=-1)
    sh1_c, sc1_c, g1_c, sh2_c, sc2_c, g2_c = jnp.split(mod_ctx, 6, axis=-1)
    u = jnp.concatenate([_modulate(h_ctx, lp['norm_mix_g'], sh1_c, sc1_c),
                         _modulate(h_lat, lp['norm_mix_g'], sh1_l, sc1_l)], axis=1)
    mix = _token_mixer(u, n_ctx, lp, lb, latent_only=last)
    if last:
        h_lat = h_lat + g1_l * mix
        u2 = _modulate(h_lat, lp['norm_ffn_g'], sh2_l, sc2_l)
        return None, h_lat + g2_l * _hier_moe(u2, lp)
    h_ctx = h_ctx + g1_c * mix[:, :n_ctx]
    h_lat = h_lat + g1_l * mix[:, n_ctx:]
    u2 = jnp.concatenate([_modulate(h_ctx, lp['norm_ffn_g'], sh2_c, sc2_c),
                          _modulate(h_lat, lp['norm_ffn_g'], sh2_l, sc2_l)], axis=1)
    ffn = _hier_moe(u2, lp)
    return h_ctx + g2_c * ffn[:, :n_ctx], h_lat + g2_l * ffn[:, n_ctx:]


def setup_inputs(seed: int = 0) -> dict:
    key = jax.random.key(seed)
    ks = iter(jax.random.split(key, 48))

    def nrm(shape, scale):
        return scale * jax.random.normal(next(ks), shape, jnp.float32)

    def gain(shape):
        return 1.0 + nrm(shape, 0.05)

    d = D_MODEL
    return {
        'x': nrm((BATCH, SEQ, d), 1.0),
        'c': nrm((BATCH, d), 1.0),
        'ctx': nrm((BATCH, CTX_LEN, d), 1.0),
        'c_ctx': nrm((d,), 1.0),
        'ada_w': nrm((DEPTH, d, 6 * d), 0.5 * d ** -0.5),
        'ada_b': nrm((DEPTH, 6 * d), 0.01),
        'norm_mix_g': gain((DEPTH, d)),
        'norm_ffn_g': gain((DEPTH, d)),
        'w_in': nrm((DEPTH, d, IN_WIDTH), d ** -0.5),
        'rg_conv_w': nrm((DEPTH, CONV_W, RG_W), 0.5),
        'rg_conv_b': nrm((DEPTH, RG_W), 0.01),
        'rg_gate_w': nrm((DEPTH, 2, 2, RG_BLOCKS, RG_BW, RG_BW), RG_BW ** -0.5),
        'rg_gate_b': nrm((DEPTH, 2, 2, RG_W), 0.1),
        'rg_lambda': 5.0 + nrm((DEPTH, 2, RG_W), 0.5),
        'gla_w_lr': nrm((DEPTH, 2, GLA_RANK, GLA_H * GLA_DK), GLA_RANK ** -0.5),
        'gla_b_lr': nrm((DEPTH, 2, GLA_H * GLA_DK), 0.1),
        'gla_norm_g': gain((DEPTH, GLA_DV)),
        'hgrn_lb_logits': nrm((DEPTH, 2, HG_H * HG_DK), 1.0),
        'hgrn_norm_g': gain((DEPTH, HG_DV)),
        'ml_conv_w': nrm((DEPTH, CONV_W, 2 * ML_H * ML_DK), 0.5),
        'ml_conv_b': nrm((DEPTH, 2 * ML_H * ML_DK), 0.01),
        'ml_gate_b': nrm((DEPTH, 2, 2, ML_H), 0.5) + jnp.array([0.0, 3.0], jnp.float32)[:, None],
        'ml_norm_g': gain((DEPTH, ML_DV)),
        'w_branch': nrm((DEPTH, N_BRANCH, BRANCH_W, d), BRANCH_W ** -0.5),
        'w_merge': nrm((DEPTH, d, N_BRANCH * d), d ** -0.5),
        'b_merge': nrm((DEPTH, N_BRANCH * d), 0.1),
        'w_out': nrm((DEPTH, d, d), d ** -0.5),
        'moe_w_group': nrm((DEPTH, d, N_GROUPS), d ** -0.5),
        'moe_b_group': nrm((DEPTH, N_GROUPS), 0.01),
        'moe_w_expert': nrm((DEPTH, d, N_EXPERTS), d ** -0.5),
        'moe_b_expert': nrm((DEPTH, N_EXPERTS), 0.01),
        'moe_w1': nrm((DEPTH, N_EXPERTS, d, D_EXPERT), d ** -0.5),
        'moe_w3': nrm((DEPTH, N_EXPERTS, d, D_EXPERT), d ** -0.5),
        'moe_w2': nrm((DEPTH, N_EXPERTS, D_EXPERT, d), D_EXPERT ** -0.5),
        'final_norm_g': gain((d,)),
    }


def reference(x, c, ctx, c_ctx, ada_w, ada_b, norm_mix_g, norm_ffn_g, w_in, rg_conv_w, rg_conv_b,
              rg_gate_w, rg_gate_b, rg_lambda, gla_w_lr, gla_b_lr, gla_norm_g, hgrn_lb_logits,
              hgrn_norm_g, ml_conv_w, ml_conv_b, ml_gate_b, ml_norm_g, w_branch, w_merge, b_merge,
              w_out, moe_w_group, moe_b_group, moe_w_expert, moe_b_expert, moe_w1, moe_w3, moe_w2,
              final_norm_g):
    lb_cum = jnp.cumsum(jax.nn.softmax(hgrn_lb_logits.astype(jnp.float32), axis=0), axis=0)
    hgrn_lb = lb_cum - lb_cum[:1]
    h_ctx, h_lat = ctx, x
    for l in range(DEPTH):
        lp = dict(ada_w=ada_w[l], ada_b=ada_b[l], norm_mix_g=norm_mix_g[l], norm_ffn_g=norm_ffn_g[l],
                  w_in=w_in[l], rg_conv_w=rg_conv_w[l], rg_conv_b=rg_conv_b[l], rg_gate_w=rg_gate_w[l],
                  rg_gate_b=rg_gate_b[l], rg_lambda=rg_lambda[l], gla_w_lr=gla_w_lr[l], gla_b_lr=gla_b_lr[l],
                  gla_norm_g=gla_norm_g[l], hgrn_norm_g=hgrn_norm_g[l], ml_conv_w=ml_conv_w[l],
                  ml_conv_b=ml_conv_b[l], ml_gate_b=ml_gate_b[l], ml_norm_g=ml_norm_g[l],
                  w_branch=w_branch[l], w_merge=w_merge[l], b_merge=b_merge[l], w_out=w_out[l],
                  moe_w_group=moe_w_group[l], moe_b_group=moe_b_group[l], moe_w_expert=moe_w_expert[l],
                  moe_b_expert=moe_b_expert[l], moe_w1=moe_w1[l], moe_w3=moe_w3[l], moe_w2=moe_w2[l])
        h_ctx, h_lat = _layer(h_ctx, h_lat, c, c_ctx, lp, hgrn_lb[l], last=(l == DEPTH - 1))
    return _rmsnorm(h_lat, final_norm_g)
```

```python
from contextlib import ExitStack
import numpy as np
import concourse.bass as bass
import concourse.mybir as mybir
from concourse.bass_utils import run_bass_kernel_spmd

F32 = mybir.dt.float32
BF16 = mybir.dt.bfloat16
ALU = mybir.AluOpType
AF = mybir.ActivationFunctionType

ENGS = ['pe', 'act', 'dve', 'pool', 'sp']
NDMASEM = 4

D = 1024
NCTX = 256
SEQ = 8192
NT = NCTX + SEQ
EPS = 1e-6
CH = 64
NCHUNK = NT // CH


class Res:
    __slots__ = ('name', 'w', 'r')

    def __init__(self, name=None):
        self.name = name
        self.w = None
        self.r = {}


MAX_EPOCH = 4
EMBED_WAIT = 1
SEM_SWITCH = 28000


class Sched:
    def __init__(self, nc, stack):
        self.nc = nc
        self.stack = stack
        self.epoch = 0
        self.tot = {}
        self.cur = None
        self.sim_tw = {}
        self.sim_tr = {}
        self.sim_free = {}
        self._new_sems()
        self.prog = {e: [] for e in ENGS}
        self.ninst = 0

    def _new_sems(self):
        nc, stack, ep = self.nc, self.stack, self.epoch
        self.sem = {e: stack.enter_context(nc.semaphore('sm%d_%s' % (ep, e))) for e in ENGS}
        self.cnt = {e: 0 for e in ENGS}
        self.dsem = {e: [stack.enter_context(nc.semaphore('dq%d_%s%d' % (ep, e, i))) for i in range(NDMASEM)]
                     for e in ('sp', 'act', 'pool')}
        self.dcnt = {e: 0 for e in ('sp', 'act', 'pool')}
        self.seen = {e: {} for e in ENGS}
        self.hist = {}
        self.hq = []
        self.gseq = 0

    def _semof(self, key, count):
        if isinstance(key, str):
            return self.sem[key], count
        q, slot = key
        return self.dsem[q][slot], 16 * count

    def _deps(self, eng, reads, writes):
        need = {}
        ep = self.epoch

        def add(key, count, epoch):
            if epoch != ep:
                return
            if key == 'pe' and eng == 'pe':
                return
            if need.get(key, 0) < count:
                need[key] = count
        for r in reads:
            if r.w is not None:
                add(*r.w)
        for w in writes:
            if w.w is not None:
                add(*w.w)
            for k, (c, e_) in w.r.items():
                add(k, c, e_)
        out = []
        clock = self.seen[eng]
        hist = self.hist
        items = sorted(need.items(), key=lambda kc: -hist.get(kc, (0, None))[0])
        for key, count in items:
            if clock.get(key, 0) >= count:
                continue
            out.append(self._semof(key, count))
            h = hist.get((key, count))
            if h is not None and h[1] is not None:
                for k2, c2 in h[1].items():
                    if clock.get(k2, 0) < c2:
                        clock[k2] = c2
            clock[key] = count
        return out

    def _record(self, eng, key, count):
        self.gseq += 1
        snap = dict(self.seen[eng])
        self.hist[(key, count)] = (self.gseq, snap)
        self.hq.append((key, count))
        if len(self.hq) > 6000:
            old = self.hq.pop(0)
            self.hist.pop(old, None)

    def _emit(self, eng, waits, fn, sem, inc):
        def run(e, waits=waits, fn=fn, sem=sem, inc=inc):
            ne = min(len(waits), EMBED_WAIT)
            for s, v in waits[:len(waits) - ne]:
                e.wait_ge(s, v)
            ins = fn(e)
            for s, v in waits[len(waits) - ne:]:
                ins._wait_ge(s, v)
            ins.then_inc(sem, inc)
        self.prog[eng].append(run)
        self.ninst += 1

    def _sim_start(self, eng, reads, writes, isdma):
        t = 0.0
        tw, tr = self.sim_tw, self.sim_tr
        for r in reads:
            v = tw.get(id(r))
            if v is not None and v[0] > t and not (v[1] == 'pe' and eng == 'pe'):
                t = v[0]
        for w in writes:
            v = tw.get(id(w))
            if v is not None and v[0] > t and not (v[1] == 'pe' and eng == 'pe'):
                t = v[0]
            v = tr.get(id(w))
            if v is not None and v > t:
                t = v
        t += 0.15
        key = ('q', eng) if isdma else eng
        return max(t, self.sim_free.get(key, 0.0))

    def _sim_commit(self, eng, reads, writes, isdma, cost):
        st = self._sim_start(eng, reads, writes, isdma)
        key = ('q', eng) if isdma else eng
        if isdma:
            self.sim_free[key] = st + 0.1
            fin = st + (cost or 3.0)
        else:
            fin = st + (cost or 0.5)
            self.sim_free[key] = fin
        for r in reads:
            if self.sim_tr.get(id(r), 0.0) < fin:
                self.sim_tr[id(r)] = fin
        for w in writes:
            self.sim_tw[id(w)] = (fin, eng)
            self.sim_tr.pop(id(w), None)

    def run_streams(self, builders):
        assert self.cur is None
        lists = []
        for b in builders:
            self.cur = []
            b()
            lists.append(self.cur)
        self.cur = None
        pos = [0] * len(lists)
        while True:
            best, bt = None, None
            for k, L in enumerate(lists):
                if pos[k] < len(L):
                    kind, a, cost = L[pos[k]]
                    t = self._sim_start(a[0], a[2], a[3], kind == 'dma')
                    if bt is None or t < bt:
                        best, bt = k, t
            if best is None:
                break
            kind, a, cost = lists[best][pos[best]]
            pos[best] += 1
            (self.op if kind == 'op' else self.dma)(*a, cost=cost)

    def op(self, eng, fn, reads=(), writes=(), cost=None):
        if self.cur is not None:
            self.cur.append(('op', (eng, fn, tuple(reads), tuple(writes)), cost))
            return
        self._sim_commit(eng, reads, writes, False, cost)
        waits = self._deps(eng, reads, writes)
        self.cnt[eng] += 1
        c = self.cnt[eng]
        ep = self.epoch
        self._emit(eng, waits, fn, self.sem[eng], 1)
        self._record(eng, eng, c)
        for r in reads:
            old = r.r.get(eng)
            if old is None or old[1] != ep or old[0] < c:
                r.r[eng] = (c, ep)
        for w in writes:
            w.w = (eng, c, ep)
            w.r = {}

    def dma(self, q, fn, reads=(), writes=(), cost=None):
        if self.cur is not None:
            self.cur.append(('dma', (q, fn, tuple(reads), tuple(writes)), cost))
            return
        self._sim_commit(q, reads, writes, True, cost)
        i = self.dcnt[q]
        self.dcnt[q] += 1
        slot = i % NDMASEM
        count = i // NDMASEM + 1
        key = (q, slot)
        ep = self.epoch
        waits = self._deps(q, reads, writes)
        if count > 1 and self.seen[q].get(key, 0) < count - 1:
            self.seen[q][key] = count - 1
            waits.append(self._semof(key, count - 1))
        self._emit(q, waits, fn, self.dsem[q][slot], 16)
        self._record(q, key, count)
        for r in reads:
            old = r.r.get(key)
            if old is None or old[1] != ep or old[0] < count:
                r.r[key] = (count, ep)
        for w in writes:
            w.w = (key, count, ep)
            w.r = {}

    def barrier(self):
        assert self.cur is None
        keys = [(e, self.cnt[e]) for e in ENGS if self.cnt[e] > 0]
        for q in ('sp', 'act', 'pool'):
            n = self.dcnt[q]
            for slot in range(NDMASEM):
                if n > slot:
                    keys.append(((q, slot), (n - 1 - slot) // NDMASEM + 1))
        for eng in ENGS:
            waits = []
            seen = self.seen[eng]
            for key, count in keys:
                if key == eng or seen.get(key, 0) >= count:
                    continue
                seen[key] = count
                waits.append(self._semof(key, count))

            def run(e, waits=waits):
                for s, v in waits:
                    e.wait_ge(s, v)
            self.prog[eng].append(run)
        if max(list(self.cnt.values()) + [v // NDMASEM for v in self.dcnt.values()]) > SEM_SWITCH \
                and self.epoch < MAX_EPOCH:
            self.tot = {e: self.tot.get(e, 0) + self.cnt[e] for e in ENGS}
            self.epoch += 1
            self._new_sems()

    def replay(self):
        nc = self.nc
        with nc.Block() as block:
            def mk(name):
                def f(e):
                    for run in self.prog[name]:
                        run(e)
                return f
            block.tensor(mk('pe'))
            block.scalar(mk('act'))
            block.vector(mk('dve'))
            block.gpsimd(mk('pool'))
            block.sync(mk('sp'))


class KB:
    def __init__(self, nc, st):
        self.nc = nc
        self.st = st
        self.S = Sched(nc, st)
        self.n = 0

    def sb(self, st, shape, dt=F32, name=None):
        self.n += 1
        return st.enter_context(self.nc.sbuf_tensor('%s_s%d' % (name or 't', self.n), list(shape), dt))

    def ps(self, st, shape, dt=F32, name=None):
        self.n += 1
        return st.enter_context(self.nc.psum_tensor('%s_p%d' % (name or 'p', self.n), list(shape), dt))

    @staticmethod
    def ecost(eng, out):
        n = int(np.prod(out.shape[1:]))
        return (0.13 + n / 900.0) if eng == 'dve' else (0.2 + n / 430.0)

    def mm(self, out, lhsT, rhs, start, stop, reads, writes):
        self.S.op('pe', lambda e: e.matmul(out, lhsT=lhsT, rhs=rhs, start=start, stop=stop), reads, writes,
                  cost=0.065 + int(np.prod(out.shape[1:])) / 2400.0)

    def tr(self, out, in_, ident, reads, writes):
        self.S.op('pe', lambda e: e.transpose(out=out, in_=in_, identity=ident), reads, writes, cost=0.12)

    def act(self, out, in_, func, reads, writes, bias=None, scale=None):
        kw = {}
        if bias is not None:
            kw['bias'] = bias
        if scale is not None:
            kw['scale'] = scale
        self.S.op('act', lambda e: e.activation(out=out, in_=in_, func=func, **kw), reads, writes,
                  cost=0.2 + int(np.prod(out.shape[1:])) / 1100.0)

    def tt(self, eng, out, in0, in1, op, reads, writes):
        self.S.op(eng, lambda e: e.tensor_tensor(out=out, in0=in0, in1=in1, op=op), reads, writes,
                  cost=self.ecost(eng, out))

    def ts(self, eng, out, in0, s1, s2, op0, op1, reads, writes):
        if s2 is None:
            self.S.op(eng, lambda e: e.tensor_scalar(out=out, in0=in0, scalar1=s1, scalar2=None, op0=op0),
                      reads, writes, cost=self.ecost(eng, out))
        else:
            self.S.op(eng, lambda e: e.tensor_scalar(out=out, in0=in0, scalar1=s1, scalar2=s2, op0=op0, op1=op1),
                      reads, writes, cost=self.ecost(eng, out))

    def stt(self, eng, out, in0, scalar, in1, op0, op1, reads, writes):
        self.S.op(eng, lambda e: e.scalar_tensor_tensor(out=out, in0=in0, scalar=scalar, in1=in1, op0=op0, op1=op1),
                  reads, writes, cost=self.ecost(eng, out))

    def copy(self, eng, out, in_, reads, writes):
        if eng == 'act':
            self.act(out, in_, AF.Copy, reads, writes)
        else:
            self.S.op(eng, lambda e: e.tensor_copy(out=out, in_=in_), reads, writes, cost=self.ecost(eng, out))

    def scan(self, out, d0, d1, init, op0, op1, reads, writes):
        self.S.op('dve', lambda e: e.tensor_tensor_scan(out=out, data0=d0, data1=d1, initial=init, op0=op0, op1=op1),
                  reads, writes, cost=self.ecost('dve', out))

    def recip(self, out, in_, reads, writes):
        self.S.op('dve', lambda e: e.reciprocal(out=out, in_=in_), reads, writes, cost=self.ecost('dve', out))

    def memset(self, eng, ap, val, writes):
        self.S.op(eng, lambda e: e.memset(ap, val), (), writes)

    def asel(self, out, in_, pattern, cmp, fill, base, cm, reads, writes):
        self.S.op('pool', lambda e: e.affine_select(out=out, in_=in_, pattern=pattern, compare_op=cmp, fill=fill,
                                                    base=base, channel_multiplier=cm), reads, writes)

    def dma(self, q, out, in_, reads, writes):
        def fn(e):
            o = out(e) if callable(out) else out
            i = in_(e) if callable(in_) else in_
            return e.dma_start(out=o, in_=i)
        self.S.dma(q, fn, reads, writes)


def rev(ap):
    (ps_, pn), (st_, n) = ap.ap
    return bass.AP(ap.tensor, ap.offset + (n - 1) * st_, [[ps_, pn], [-st_, n]])


def colT(v):
    v = np.asarray(v, np.float32)
    return np.ascontiguousarray(v.reshape(-1, 128).T)


class Pack:
    def __init__(self):
        self.items = []
        self.off = {}
        self.w = 0

    def add(self, name, arr):
        arr = np.asarray(arr, np.float32)
        if arr.ndim == 1:
            arr = arr[:, None]
        assert arr.shape[0] <= 128
        if arr.shape[0] < 128:
            arr = np.concatenate([arr, np.zeros((128 - arr.shape[0], arr.shape[1]), np.float32)], 0)
        self.off[name] = (self.w, arr.shape[1])
        self.items.append(arr)
        self.w += arr.shape[1]

    def build(self):
        return np.ascontiguousarray(np.concatenate(self.items, axis=1))


IN_OFF = {}
_o = 0
for _n, _w in (('rg_x', 512), ('rg_y', 512), ('gla_q', 256), ('gla_k', 256), ('gla_v', 512), ('gla_g', 512),
               ('gla_lr', 32), ('hg_q', 512), ('hg_i', 512), ('hg_f', 1024), ('hg_g', 512), ('ml_q', 512),
               ('ml_k', 512), ('ml_v', 512), ('ml_o', 512), ('ml_if', 16)):
    IN_OFF[_n] = _o
    _o += _w
assert _o == 7216

A_COLS = [('rg_x', 128), ('rg_y', 128),
          ('gla_q', 64), ('gla_k', 64), ('gla_g', 128), ('gla_lr', 32), ('gla_v', 128),
          ('hg_q', 128), ('hg_f0', 128), ('hg_f1', 128), ('hg_g', 128), ('hg_i', 128),
          ('ml_q', 128), ('ml_k', 128), ('ml_o', 128), ('ml_v', 128),
          ('ml_i0', 128), ('ml_f0', 128), ('ml_i1', 128), ('ml_f1', 128)]
A_OFF = {}
_o = 0
for _n, _w in A_COLS:
    A_OFF[_n] = (_o, _w)
    _o += _w
A_NCOL = _o


def gather_w_in(w_in_l, hd):
    cols = {}
    o = IN_OFF
    cols['rg_x'] = w_in_l[:, o['rg_x'] + hd * 128: o['rg_x'] + (hd + 1) * 128]
    cols['rg_y'] = w_in_l[:, o['rg_y'] + hd * 128: o['rg_y'] + (hd + 1) * 128]
    cols['gla_q'] = w_in_l[:, o['gla_q'] + hd * 64: o['gla_q'] + (hd + 1) * 64]
    cols['gla_k'] = w_in_l[:, o['gla_k'] + hd * 64: o['gla_k'] + (hd + 1) * 64]
    cols['gla_v'] = w_in_l[:, o['gla_v'] + hd * 128: o['gla_v'] + (hd + 1) * 128]
    cols['gla_g'] = w_in_l[:, o['gla_g'] + hd * 128: o['gla_g'] + (hd + 1) * 128]
    cols['gla_lr'] = w_in_l[:, o['gla_lr']: o['gla_lr'] + 32]
    cols['hg_q'] = w_in_l[:, o['hg_q'] + hd * 128: o['hg_q'] + (hd + 1) * 128]
    cols['hg_i'] = w_in_l[:, o['hg_i'] + hd * 128: o['hg_i'] + (hd + 1) * 128]
    for d in range(2):
        cols['hg_f%d' % d] = w_in_l[:, o['hg_f'] + d * 512 + hd * 128: o['hg_f'] + d * 512 + (hd + 1) * 128]
    cols['hg_g'] = w_in_l[:, o['hg_g'] + hd * 128: o['hg_g'] + (hd + 1) * 128]
    for nm in ('ml_q', 'ml_k', 'ml_v', 'ml_o'):
        cols[nm] = w_in_l[:, o[nm] + hd * 128: o[nm] + (hd + 1) * 128]
    for d in range(2):
        for g, gn in enumerate(('i', 'f')):
            c = o['ml_if'] + d * 8 + g * 4 + hd
            cols['ml_%s%d' % (gn, d)] = np.repeat(w_in_l[:, c:c + 1], 128, axis=1)
    return np.ascontiguousarray(np.concatenate([cols[n] for n, _ in A_COLS], axis=1))


A_BLOCKS = [(0, NCTX)] + [(NCTX + i * 512, 512) for i in range(SEQ // 512)]
NBLK = len(A_BLOCKS)


def blk_order(d):
    return list(range(NBLK)) if d == 0 else [0] + list(range(NBLK - 1, 0, -1))


def emit_consts(K, st):
    C = {}
    r = Res()
    C['res'] = r
    ones_f = K.sb(st, [128, 512], F32, 'ones_f')
    K.memset('pool', ones_f[:], 1.0, [r])
    C['ones_f'] = ones_f
    ones_b = K.sb(st, [128, 128], BF16, 'ones_b')
    K.memset('pool', ones_b[:], 1.0, [r])
    C['ones_b'] = ones_b
    cc = K.sb(st, [128, 4], F32, 'cconst')
    K.memset('pool', cc[:, 0:1], 1.0, [r])
    K.memset('pool', cc[:, 1:2], EPS, [r])
    K.memset('pool', cc[:, 2:3], 0.0, [r])
    K.memset('pool', cc[:, 3:4], float(np.log(128.0 ** -0.5)), [r])
    C['one'] = cc[:, 0:1]
    C['eps'] = cc[:, 1:2]
    C['zero'] = cc[:, 2:3]
    C['lns'] = cc[:, 3:4]
    ident_f = K.sb(st, [128, 128], F32, 'ident_f')
    K.memset('pool', ident_f[:], 0.0, [r])
    K.asel(ident_f[:], ident_f[:], [[-1, 128]], ALU.not_equal, 1.0, 0, 1, [r], [r])
    C['ident_f'] = ident_f
    ident_b = K.sb(st, [128, 128], BF16, 'ident_b')
    K.copy('pool', ident_b[:], ident_f[:], [r], [r])
    C['ident_b'] = ident_b
    return C


def emit_mod(K, st, C, ada_w_d, ncol, cT_ap, adab_ap, r_prm):
    nch = ncol // 128
    sc = K.sb(st, [128, 16], F32, 'silu_c')
    r_sc = Res()
    K.act(sc[:], cT_ap, AF.Silu, [r_prm], [r_sc])
    mod = K.sb(st, [128, nch, 2], F32, 'mod')
    r_mod = Res()
    with ExitStack() as st2:
        wbuf = [K.sb(st2, [128, 8, 512], F32, 'adaw%d' % i) for i in range(2)]
        rw = [Res(), Res()]
        pm = K.ps(st2, [128, 512], F32, 'ps_mod')
        r_pm = Res()
        wv = ada_w_d.rearrange("(k p) n -> p k n", p=128)
        for g in range(ncol // 512):
            wb, rb = wbuf[g % 2], rw[g % 2]
            K.dma('sp' if g % 2 == 0 else 'act', wb[:], wv[:, :, g * 512:(g + 1) * 512], [], [rb])
            for j in range(4):
                ch = g * 4 + j
                for k in range(8):
                    K.mm(pm[:, ch * 2:ch * 2 + 2], wb[:, k, j * 128:(j + 1) * 128], sc[:, k * 2:k * 2 + 2],
                         k == 0, k == 7, [rb, r_sc], [r_pm])
        K.tt('dve', mod[:], pm[:, 0:nch * 2].rearrange("p (c t) -> p c t", t=2),
             adab_ap.unsqueeze(2).to_broadcast([128, nch, 2]), ALU.add, [r_pm, r_prm], [r_mod])
        K.S.barrier()
    return mod, r_mod


def emit_norm_mod(K, C, x, rx, nb, ty, gsc, sh, r_gs, sq, r_sq, pss, r_pss, rstd, r_rstd, tmp, r_tmp, out, r_out,
                  out_f32=None, r_of=None, mul_eng='pool'):
    K.act(sq[:, :, 0:nb], x[:, :, 0:nb], AF.Square, [rx], [r_sq])
    for k in range(8):
        K.mm(pss[:, 0:nb], C['ones_b'][:], sq[:, k, 0:nb], k == 0, k == 7, [r_sq, C['res']], [r_pss])
    K.act(rstd[:, 0:nb], pss[:, 0:nb], AF.Sqrt, [r_pss, C['res']], [r_rstd], bias=C['eps'], scale=1.0 / D)
    K.recip(rstd[:, 0:nb], rstd[:, 0:nb], [r_rstd], [r_rstd])
    for k in range(8):
        K.tt('dve', tmp[:, k, 0:nb], x[:, k, 0:nb], rstd[:, 0:nb], ALU.mult, [rx, r_rstd], [r_tmp])
    for k in range(8):
        if out_f32 is not None:
            K.ts(mul_eng, out_f32[:, k, 0:nb], tmp[:, k, 0:nb], gsc[:, k, ty:ty + 1], sh[:, k, ty:ty + 1],
                 ALU.mult, ALU.add, [r_tmp, r_gs], [r_of])
            K.copy('act', out[:, k, 0:nb], out_f32[:, k, 0:nb], [r_of], [r_out])
        else:
            K.ts(mul_eng, out[:, k, 0:nb], tmp[:, k, 0:nb], gsc[:, k, ty:ty + 1], sh[:, k, ty:ty + 1],
                 ALU.mult, ALU.add, [r_tmp, r_gs], [r_out])


def pack_A(inp, l, b, hd):
    P = Pack()
    cT = np.stack([colT(inp['c'][b]), colT(inp['c_ctx'])], axis=2).reshape(128, 16)
    P.add('cT', cT)
    P.add('adab', colT(inp['ada_b'][l][0:2048]))
    P.add('gmix', colT(inp['norm_mix_g'][l]))
    hs = slice(hd * 128, (hd + 1) * 128)
    P.add('rg_cw', inp['rg_conv_w'][l][:, hs].T)
    P.add('rg_cb', inp['rg_conv_b'][l][hs])
    P.add('rg_gb', inp['rg_gate_b'][l][:, :, hs].reshape(4, 128).T)
    P.add('rg_lam', inp['rg_lambda'][l][:, hs].T)
    P.add('gla_blr', inp['gla_b_lr'][l][:, hd * 64:(hd + 1) * 64].T)
    P.add('gla_ng', inp['gla_norm_g'][l])
    P.add('hg_l0', inp['hgrn_lb_logits'][0][:, hs].T)
    P.add('hg_l1', inp['hgrn_lb_logits'][1][:, hs].T)
    P.add('hg_ng', inp['hgrn_norm_g'][l])
    P.add('ml_cwq', inp['ml_conv_w'][l][:, hs].T)
    P.add('ml_cwk', inp['ml_conv_w'][l][:, 512 + hd * 128: 512 + (hd + 1) * 128].T)
    P.add('ml_cbq', inp['ml_conv_b'][l][hs])
    P.add('ml_cbk', inp['ml_conv_b'][l][512 + hd * 128: 512 + (hd + 1) * 128])
    gb = inp['ml_gate_b'][l][:, :, hd].reshape(4)
    P.add('ml_gb', np.repeat(gb[None, :], 128, axis=0))
    P.add('ml_ng', inp['ml_norm_g'][l])
    return P


def rg_gate_blockdiag(inp, l, hd):
    out = np.zeros((128, 4, 128), np.float32)
    gw = inp['rg_gate_w'][l]
    for d in range(2):
        for g in range(2):
            for kk in range(2):
                out[kk * 64:(kk + 1) * 64, d * 2 + g, kk * 64:(kk + 1) * 64] = gw[d, g, hd * 2 + kk]
    return np.ascontiguousarray(out.reshape(128, 512))


def gla_wlr_pad(inp, l, hd):
    out = np.zeros((32, 2, 64), np.float32)
    for d in range(2):
        out[d * 16:(d + 1) * 16, d, :] = inp['gla_w_lr'][l][d][:, hd * 64:(hd + 1) * 64]
    return np.ascontiguousarray(out.reshape(32, 128))


def emit_A(K, C, l, hsrc, adaw_d, heads, prm_off, uT_d, mixers=('rg', 'gla', 'hg', 'ml')):
    S = K.S
    PRM, r_prm = heads[0]['PRM'], heads[0]['r_prm']
    with ExitStack() as st:
        gsc = K.sb(st, [128, 8, 2], F32, 'gsc')
        sh = K.sb(st, [128, 8, 2], F32, 'sh')
        r_gs = Res()
        with ExitStack() as st0:
            mod, r_mod = emit_mod(K, st0, C, adaw_d, 2048, PRM('cT'), PRM('adab'), r_prm)
            K.ts('dve', gsc[:], mod[:, 8:16, :], 1.0, None, ALU.add, None, [r_mod], [r_gs])
            K.tt('dve', gsc[:], gsc[:], PRM('gmix').unsqueeze(2).to_broadcast([128, 8, 2]), ALU.mult,
                 [r_gs, r_prm], [r_gs])
            K.copy('dve', sh[:], mod[:, 0:8, :], [r_mod], [r_gs])
            S.barrier()

        ures = [Res() for _ in range(NBLK)]
        with ExitStack() as st1:
            xb_ = [K.sb(st1, [128, 8, 512], F32, 'x%d' % i) for i in range(2)]
            rx_ = [Res(), Res()]
            sq = K.sb(st1, [128, 8, 512], BF16, 'sq')
            r_sq = Res()
            pss = [K.ps(st1, [128, 512], F32, 'pss%d' % i) for i in range(2)]
            r_pss = [Res(), Res()]
            rstd = [K.sb(st1, [128, 512], F32, 'rstd%d' % i) for i in range(2)]
            r_rstd = [Res(), Res()]
            tmp = K.sb(st1, [128, 8, 512], F32, 'tmp')
            r_tmp = Res()
            ub = [K.sb(st1, [128, 8, 512], BF16, 'u%d' % i) for i in range(2)]
            r_ub = [Res(), Res()]
            for bi, (t0, nb) in enumerate(A_BLOCKS):
                i2 = bi % 2
                ty = 1 if bi == 0 else 0
                K.dma('sp', xb_[i2][:, :, 0:nb], hsrc[:, :, t0:t0 + nb], [], [rx_[i2]])
                emit_norm_mod(K, C, xb_[i2], rx_[i2], nb, ty, gsc, sh, r_gs, sq, r_sq, pss[i2], r_pss[i2],
                              rstd[i2], r_rstd[i2], tmp, r_tmp, ub[i2], r_ub[i2])
                K.dma('act', uT_d[:, :, t0:t0 + nb], ub[i2][:, :, 0:nb], [r_ub[i2]], [ures[bi]])
            S.barrier()

        outres = []
        for hdd in heads:
            for mx in mixers:
                with ExitStack() as stm:
                    emit_mixer(K, stm, C, mx, l, hdd['PRM'], hdd['r_prm'], prm_off, hdd['winv'], hdd['rgw_d'],
                               hdd['wlr_d'], uT_d, ures, hdd['ys_dst'], outres)
                    S.barrier()
        S.barrier()


def emit_mixer(K, st, C, mx, l, PRM, r_prm, prm_off, winv, rgw_d, wlr_d, uT_d, ures, ys_d, outres):
    S = K.S
    branch = {'rg': 0, 'gla': 1, 'hg': 2, 'ml': 3}[mx]
    wnames = {'rg': ['rg_x', 'rg_y'],
              'gla': ['gla_q', 'gla_k', 'gla_g', 'gla_lr', 'gla_v'],
              'hg': ['hg_q', 'hg_f0', 'hg_f1', 'hg_g', 'hg_i'],
              'ml': ['ml_q', 'ml_k', 'ml_o', 'ml_v', 'ml_i0', 'ml_f0', 'ml_i1', 'ml_f1']}[mx]
    c_lo = A_OFF[wnames[0]][0]
    c_hi = A_OFF[wnames[-1]][0] + A_OFF[wnames[-1]][1]
    ncol = c_hi - c_lo
    wt = K.sb(st, [128, 8, ncol], BF16, 'w_' + mx)
    r_w = Res()
    for k in range(8):
        K.dma('pool', wt[:, k, :], winv[:, k, c_lo:c_hi], [], [r_w])

    def W(name, k):
        o, w = A_OFF[name]
        return wt[:, k, o - c_lo:o - c_lo + w]

    ubuf = [K.sb(st, [128, 8, 512], BF16, 'ub%d' % i) for i in range(2)]
    r_ubuf = [Res(), Res()]
    uctr = [0]

    def load_u(bi):
        t0, nb = A_BLOCKS[bi]
        i = uctr[0] % 2
        uctr[0] += 1
        K.dma('sp', ubuf[i][:, :, 0:nb], uT_d[:, :, t0:t0 + nb], [ures[bi]], [r_ubuf[i]])
        return ubuf[i], r_ubuf[i]

    pproj = [K.ps(st, [128, 512], F32, 'pproj%d' % i) for i in range(2)]
    r_pproj = [Res(), Res()]
    pctr = [0]

    def proj(u, ru, name, nb, M=None):
        o, w = A_OFF[name]
        M = M or w
        i = pctr[0] % 2
        pctr[0] += 1
        for k in range(8):
            K.mm(pproj[i][0:M, 0:nb], W(name, k)[:, 0:M], u[:, k, 0:nb], k == 0, k == 7, [r_w, ru], [r_pproj[i]])
        return pproj[i][0:M, 0:nb], r_pproj[i]

    def conv_block(raw, rraw, bi, cw, cb, outt, r_out):
        t0, nb = A_BLOCKS[bi]
        s0, s1 = (0, NCTX) if bi == 0 else (NCTX, NT)
        rd = [rraw[j] for j in (bi - 1, bi, bi + 1) if 0 <= j < NBLK]
        K.ts('pool', outt[:, 0:nb], raw[:, t0:t0 + nb], cw[:, 2:3], cb, ALU.mult, ALU.add, rd + [r_prm], [r_out])
        for j in (0, 1, 3):
            o = j - 2
            a = max(t0, s0 - o)
            e = min(t0 + nb, s1 - o)
            K.stt('dve', outt[:, a - t0:e - t0], raw[:, a + o:e + o], cw[:, j:j + 1], outt[:, a - t0:e - t0],
                  ALU.mult, ALU.add, rd + [r_prm, r_out], [r_out])

    def final_alloc():
        sqf = [K.sb(st, [128, 512], BF16, 'sqf%d' % i) for i in range(2)]
        rsf = [K.sb(st, [128, 512], F32, 'rsf%d' % i) for i in range(2)]
        yf = [K.sb(st, [128, 512], F32, 'yf%d' % i) for i in range(2)]
        yb = [K.sb(st, [128, 512], BF16, 'yb%d' % i) for i in range(2)]
        return dict(sqf=sqf, rsf=rsf, yf=yf, yb=yb, r_sqf=[Res(), Res()], r_rsf=[Res(), Res()],
                    r_yf=[Res(), Res()], r_yb=[Res(), Res()], n=[0])

    def final_block(T, bi, o_acc, r_oacc, gate, r_gate, ng_ap, pfin, r_pfin):
        t0, nb = A_BLOCKS[bi]
        i = T['n'][0] % 2
        T['n'][0] += 1
        sqf, rsf, yf, yb = T['sqf'], T['rsf'], T['yf'], T['yb']
        r_sqf, r_rsf, r_yf, r_yb = T['r_sqf'], T['r_rsf'], T['r_yf'], T['r_yb']
        K.act(sqf[i][:, 0:nb], o_acc[:, t0:t0 + nb], AF.Square, [r_oacc[bi]], [r_sqf[i]])
        K.mm(pfin[i][:, 0:nb], C['ones_b'][:], sqf[i][:, 0:nb], True, True, [r_sqf[i], C['res']], [r_pfin[i]])
        K.act(rsf[i][:, 0:nb], pfin[i][:, 0:nb], AF.Sqrt, [r_pfin[i], C['res']], [r_rsf[i]], bias=C['eps'],
              scale=1.0 / 128)
        K.recip(rsf[i][:, 0:nb], rsf[i][:, 0:nb], [r_rsf[i]], [r_rsf[i]])
        K.stt('dve', yf[i][:, 0:nb], o_acc[:, t0:t0 + nb], ng_ap, rsf[i][:, 0:nb], ALU.mult, ALU.mult,
              [r_oacc[bi], r_rsf[i], r_prm], [r_yf[i]])
        K.tt('pool', yb[i][:, 0:nb], yf[i][:, 0:nb], gate[:, t0:t0 + nb], ALU.mult, [r_yf[i], r_gate[bi]],
             [r_yb[i]])
        ro = Res()
        K.dma('act', ys_d(branch, t0, nb), yb[i][:, 0:nb], [r_yb[i]], [ro])
        outres.append(ro)

    if mx == 'rg':
        raw = K.sb(st, [128, NT], F32, 'rg_raw')
        r_raw = [Res() for _ in range(NBLK)]
        xb = K.sb(st, [128, NT], F32, 'rg_xb')
        xbb = K.sb(st, [128, NT], BF16, 'rg_xbb')
        r_xb = [Res() for _ in range(NBLK)]
        gy = K.sb(st, [128, NT], BF16, 'rg_gy')
        r_gy = [Res() for _ in range(NBLK)]
        wg = K.sb(st, [128, 512], BF16, 'rg_wg')
        r_wg = Res()
        K.dma('pool', wg[:], rgw_d, [], [r_wg])
        clam = K.sb(st, [128, 2], F32, 'rg_clam')
        r_clam = Res()
        K.act(clam[:], PRM('rg_lam'), AF.Exp, [r_prm], [r_clam], scale=-1.0)
        K.act(clam[:], clam[:], AF.Ln, [r_clam, C['res']], [r_clam], bias=C['one'])
        K.ts('dve', clam[:], clam[:], -8.0, None, ALU.mult, None, [r_clam], [r_clam])
        t1 = [K.sb(st, [128, 512], F32, 'rg_t1_%d' % i) for i in range(2)]
        r_t1 = [Res(), Res()]
        t2 = [K.sb(st, [128, 512], F32, 'rg_t2_%d' % i) for i in range(2)]
        r_t2 = [Res(), Res()]
        xs = [K.sb(st, [128, 512], F32, 'rg_xs_%d' % i) for i in range(2)]
        r_xs = [Res(), Res()]
        for bi, (t0, nb) in enumerate(A_BLOCKS):
            i = bi % 2
            u, ru = load_u(bi)
            p, rp = proj(u, ru, 'rg_x', nb)
            K.copy('act', raw[:, t0:t0 + nb], p, [rp], [r_raw[bi]])
            p, rp = proj(u, ru, 'rg_y', nb)
            K.act(t1[i][:, 0:nb], p, AF.Square, [rp], [r_t1[i]])
            K.copy('act', xs[i][:, 0:nb], p, [rp], [r_xs[i]])
            K.ts('dve', t1[i][:, 0:nb], t1[i][:, 0:nb], 0.044715, 1.0, ALU.mult, ALU.add, [r_t1[i]], [r_t1[i]])
            K.tt('dve', t2[i][:, 0:nb], t1[i][:, 0:nb], xs[i][:, 0:nb], ALU.mult, [r_t1[i], r_xs[i]], [r_t2[i]])
            K.act(t2[i][:, 0:nb], t2[i][:, 0:nb], AF.Sigmoid, [r_t2[i]], [r_t2[i]], scale=1.5957691216)
            K.tt('dve', gy[:, t0:t0 + nb], xs[i][:, 0:nb], t2[i][:, 0:nb], ALU.mult, [r_xs[i], r_t2[i]],
                 [r_gy[bi]])
        for bi, (t0, nb) in enumerate(A_BLOCKS):
            conv_block(raw, r_raw, bi, PRM('rg_cw'), PRM('rg_cb'), xb[:, t0:t0 + nb], r_xb[bi])
            K.copy('act', xbb[:, t0:t0 + nb], xb[:, t0:t0 + nb], [r_xb[bi]], [r_xb[bi]])
        rr = [K.sb(st, [128, 512], F32, 'rg_r%d' % i) for i in range(2)]
        r_rr = [Res(), Res()]
        ii = [K.sb(st, [128, 512], F32, 'rg_i%d' % i) for i in range(2)]
        r_ii = [Res(), Res()]
        aa = [K.sb(st, [128, 512], F32, 'rg_a%d' % i) for i in range(2)]
        r_aa = [Res(), Res()]
        ss = [K.sb(st, [128, 512], F32, 'rg_s%d' % i) for i in range(2)]
        r_ss = [Res(), Res()]
        hh = [K.sb(st, [128, 512], F32, 'rg_h%d' % i) for i in range(2)]
        r_hh = [Res(), Res()]
        gb = PRM('rg_gb')
        n = 0
        for d in range(2):
            carry = 0.0
            r_carry = []
            for bi in blk_order(d):
                t0, nb = A_BLOCKS[bi]
                i = n % 2
                n += 1
                pr = pproj[pctr[0] % 2]
                rpr = r_pproj[pctr[0] % 2]
                pctr[0] += 1
                K.mm(pr[:, 0:nb], wg[:, (d * 2) * 128:(d * 2 + 1) * 128], xbb[:, t0:t0 + nb], True, True,
                     [r_wg, r_xb[bi]], [rpr])
                K.act(rr[i][:, 0:nb], pr[:, 0:nb], AF.Sigmoid, [rpr, r_prm], [r_rr[i]], bias=gb[:, d * 2:d * 2 + 1])
                pi = pproj[pctr[0] % 2]
                rpi = r_pproj[pctr[0] % 2]
                pctr[0] += 1
                K.mm(pi[:, 0:nb], wg[:, (d * 2 + 1) * 128:(d * 2 + 2) * 128], xbb[:, t0:t0 + nb], True, True,
                     [r_wg, r_xb[bi]], [rpi])
                K.act(ii[i][:, 0:nb], pi[:, 0:nb], AF.Sigmoid, [rpi, r_prm], [r_ii[i]],
                      bias=gb[:, d * 2 + 1:d * 2 + 2])
                K.act(aa[i][:, 0:nb], rr[i][:, 0:nb], AF.Exp, [r_rr[i], r_clam], [r_aa[i]], scale=clam[:, d:d + 1])
                K.tt('dve', ss[i][:, 0:nb], aa[i][:, 0:nb], aa[i][:, 0:nb], ALU.mult, [r_aa[i]], [r_ss[i]])
                K.act(ss[i][:, 0:nb], ss[i][:, 0:nb], AF.Sqrt, [r_ss[i], C['res']], [r_ss[i]], bias=C['one'],
                      scale=-1.0)
                K.tt('dve', ii[i][:, 0:nb], ii[i][:, 0:nb], xb[:, t0:t0 + nb], ALU.mult, [r_ii[i], r_xb[bi]],
                     [r_ii[i]])
                K.tt('dve', ii[i][:, 0:nb], ii[i][:, 0:nb], ss[i][:, 0:nb], ALU.mult, [r_ii[i], r_ss[i]],
                     [r_ii[i]])
                ha, aa_, bt_ = hh[i][:, 0:nb], aa[i][:, 0:nb], ii[i][:, 0:nb]
                if d == 1:
                    ha, aa_, bt_ = rev(ha), rev(aa_), rev(bt_)
                K.scan(ha, aa_, bt_, carry, ALU.mult, ALU.add, [r_aa[i], r_ii[i]] + r_carry, [r_hh[i]])
                carry = hh[i][:, nb - 1:nb] if d == 0 else hh[i][:, 0:1]
                r_carry = [r_hh[i]]
                if d == 0:
                    K.copy('pool', raw[:, t0:t0 + nb], hh[i][:, 0:nb], [r_hh[i]], [r_raw[bi]])
                else:
                    K.tt('pool', raw[:, t0:t0 + nb], raw[:, t0:t0 + nb], hh[i][:, 0:nb], ALU.add,
                         [r_hh[i], r_raw[bi]], [r_raw[bi]])
        yb = [K.sb(st, [128, 512], BF16, 'rg_yb%d' % i) for i in range(2)]
        r_yb = [Res(), Res()]
        for bi, (t0, nb) in enumerate(A_BLOCKS):
            i = bi % 2
            K.tt('dve', yb[i][:, 0:nb], raw[:, t0:t0 + nb], gy[:, t0:t0 + nb], ALU.mult, [r_raw[bi], r_gy[bi]],
                 [r_yb[i]])
            ro = Res()
            K.dma('act', ys_d(branch, t0, nb), yb[i][:, 0:nb], [r_yb[i]], [ro])
            outres.append(ro)
        return

    dk = 64 if mx == 'gla' else 128
    SW = 256 if mx == 'ml' else 128
    vname = {'gla': 'gla_v', 'hg': 'hg_i', 'ml': 'ml_v'}[mx]
    q_bf = K.sb(st, [dk, NT], BF16, mx + '_q')
    r_q = [Res() for _ in range(NBLK)]
    if mx != 'hg':
        k_bf = K.sb(st, [dk, NT], BF16, mx + '_k')
        r_k = [Res() for _ in range(NBLK)]
    v_bf = K.sb(st, [64, NCHUNK, 128], BF16, mx + '_v')
    r_v = [Res() for _ in range(NBLK)]
    gate = K.sb(st, [128, NT], BF16, mx + '_gate')
    r_gate = [Res() for _ in range(NBLK)]
    o_acc = K.sb(st, [128, NT], F32, mx + '_oacc')
    r_oacc = [Res() for _ in range(NBLK)]
    stpv = ExitStack()
    pv = K.ps(stpv, [64, 4, 128], F32, 'pv')
    r_pv = Res()

    def proj_v(u, ru, bi, scale):
        t0, nb = A_BLOCKS[bi]
        o, w = A_OFF[vname]
        for g0 in range(0, nb // 64, 4):
            for j in range(4):
                for k in range(8):
                    K.mm(pv[:, j, :], u[:, k, (g0 + j) * 64:(g0 + j + 1) * 64], W(vname, k), k == 0, k == 7,
                         [ru, r_w], [r_pv])
            c0 = t0 // 64 + g0
            K.act(v_bf[:, c0:c0 + 4, :], pv[:], AF.Copy, [r_pv], [r_v[bi]], scale=scale)

    if mx == 'gla':
        def p_work(bi, u, ru):
            t0, nb = A_BLOCKS[bi]
            p, rp = proj(u, ru, 'gla_q', nb)
            K.act(q_bf[:, t0:t0 + nb], p, AF.Copy, [rp], [r_q[bi]], scale=0.125)
            p, rp = proj(u, ru, 'gla_k', nb)
            K.copy('dve', k_bf[:, t0:t0 + nb], p, [rp], [r_k[bi]])
            p, rp = proj(u, ru, 'gla_g', nb)
            K.act(gate[:, t0:t0 + nb], p, AF.Silu, [rp], [r_gate[bi]])
            proj_v(u, ru, bi, 1.0)
    elif mx == 'hg':
        def p_work(bi, u, ru):
            t0, nb = A_BLOCKS[bi]
            p, rp = proj(u, ru, 'hg_q', nb)
            K.act(q_bf[:, t0:t0 + nb], p, AF.Silu, [rp], [r_q[bi]])
            p, rp = proj(u, ru, 'hg_g', nb)
            K.act(gate[:, t0:t0 + nb], p, AF.Silu, [rp], [r_gate[bi]])
            proj_v(u, ru, bi, 128.0 ** -0.5)
    else:
        stp = ExitStack()
        ctmp = [K.sb(stp, [128, 512], F32, 'ml_ct%d' % i) for i in range(2)]
        r_ct = [Res(), Res()]
        for which in range(2):
            nm = ('ml_q', 'ml_k')[which]
            dst, rdst = ((q_bf, r_q), (k_bf, r_k))[which]
            cw = PRM(('ml_cwq', 'ml_cwk')[which])
            cb = PRM(('ml_cbq', 'ml_cbk')[which])
            for bi, (t0, nb) in enumerate(A_BLOCKS):
                u, ru = load_u(bi)
                p, rp = proj(u, ru, nm, nb)
                K.copy('act', o_acc[:, t0:t0 + nb], p, [rp], [r_oacc[bi]])
                if which == 0:
                    p, rp = proj(u, ru, 'ml_o', nb)
                    K.act(gate[:, t0:t0 + nb], p, AF.Sigmoid, [rp], [r_gate[bi]])
                    proj_v(u, ru, bi, 1.0)
            for bi, (t0, nb) in enumerate(A_BLOCKS):
                i = bi % 2
                conv_block(o_acc, r_oacc, bi, cw, cb, ctmp[i][:, 0:nb], r_ct[i])
                K.act(dst[:, t0:t0 + nb], ctmp[i][:, 0:nb], AF.Silu, [r_ct[i]], [rdst[bi]])
        S.barrier()
        stp.close()

    if mx == 'ml':
        S.barrier()
        stpv.close()
    else:
        fin_t = final_alloc()
    std = ExitStack()
    mask = K.sb(std, [64, 2, 64], F32, 'mask')
    r_mask = Res()
    K.memset('pool', mask[:], 1.0, [r_mask])
    K.asel(mask[:, 0, :], mask[:, 0, :], [[1, 64]], ALU.is_ge, 0.0, 0, -1, [r_mask], [r_mask])
    K.asel(mask[:, 1, :], mask[:, 1, :], [[-1, 64]], ALU.is_ge, 0.0, 0, 1, [r_mask], [r_mask])
    rmask = K.sb(std, [128, 2, 512], F32, 'rmask')
    r_rmask = Res()
    K.memset('pool', rmask[:], 1.0, [r_rmask])
    K.memset('pool', rmask[:, 0, :].rearrange("p (c i) -> p c i", i=64)[:, :, 0:1], 0.0, [r_rmask])
    K.memset('pool', rmask[:, 1, :].rearrange("p (c i) -> p c i", i=64)[:, :, 63:64], 0.0, [r_rmask])
    hmask = K.sb(std, [128, 2, 512], BF16, 'hmask')
    K.memset('pool', hmask[:], 1.0, [r_rmask])
    K.memset('pool', hmask[:, 0, :].rearrange("p (c i) -> p c i", i=64)[:, :, 32:64], 0.0, [r_rmask])
    K.memset('pool', hmask[:, 1, :].rearrange("p (c i) -> p c i", i=64)[:, :, 0:32], 0.0, [r_rmask])

    if mx == 'gla':
        wlr = K.sb(std, [32, 128], BF16, 'wlr')
        r_wlr = Res()
        K.dma('pool', wlr[:], wlr_d, [], [r_wlr])
        nblr = K.sb(std, [64, 2], F32, 'nblr')
        r_nblr = Res()
        K.ts('dve', nblr[:], PRM('gla_blr', 64), -1.0, None, ALU.mult, None, [r_prm], [r_nblr])
        lrs = [K.sb(std, [32, 512], BF16, 'lrs%d' % i) for i in range(2)]
        r_lrs = [Res(), Res()]
    if mx == 'hg':
        lbt = K.sb(std, [128, 4], F32, 'lbt')
        r_lbt = Res()
        if l == 0:
            K.memset('pool', lbt[:, 0:2], 0.0, [r_lbt])
        else:
            K.tt('dve', lbt[:, 0:2], PRM('hg_l1'), PRM('hg_l0'), ALU.subtract, [r_prm], [r_lbt])
            K.act(lbt[:, 0:2], lbt[:, 0:2], AF.Sigmoid, [r_lbt], [r_lbt])
        K.ts('dve', lbt[:, 2:4], lbt[:, 0:2], -1.0, 1.0, ALU.mult, ALU.add, [r_lbt], [r_lbt])
    if mx == 'ml':
        ngb = K.sb(std, [128, 4], F32, 'ngb')
        r_ngb = Res()
        K.ts('dve', ngb[:], PRM('ml_gb'), -1.0, None, ALU.mult, None, [r_prm], [r_ngb])
        carG = K.sb(std, [128, 1], F32, 'carG')
        carM = K.sb(std, [128, 1], F32, 'carM')
        r_car = Res()

    def T2(name, shape, dt=F32):
        return [K.sb(std, shape, dt, '%s_%s%d' % (mx, name, i)) for i in range(2)], [Res(), Res()]

    glog, r_glog = T2('glog', [dk, 512])
    bb, r_bb = T2('b', [dk, 512])
    d1, r_d1 = T2('d1', [dk, 512])
    if mx == 'ml':
        d2, r_d2 = T2('d2', [dk, 512])
        d3, r_d3 = T2('d3', [dk, 512])
    qt, r_qt = T2('qt', [dk, 512], BF16)
    kt, r_kt = T2('kt', [dk, 512], BF16)
    if mx != 'ml':
        ktz, r_ktz = T2('ktz', [dk, 512], BF16)
    e2b, r_e2b = T2('e2b', [dk, 512], BF16)
    e3b, r_e3b = T2('e3b', [dk, 512], BF16)
    ke, r_ke = T2('ke', [dk, 512], BF16)
    keT, r_keT = T2('keT', [64, 8, dk], BF16)
    sm, r_sm = T2('sm', [dk, 32])
    if mx == 'hg':
        kraw, r_kraw = T2('kraw', [dk, 512])
    if mx == 'ml':
        ip, r_ip = T2('ip', [128, 512])
        clampt, r_cl = T2('clamp', [128, 512])
        hht, r_hht = ip, r_ip
    stm, r_stm = T2('stm', [64, 64], BF16)
    Sfl = [K.sb(std, [dk, SW], F32, mx + '_Sf%d' % i) for i in range(2)]
    r_Sfl = [Res(), Res()]
    Sb, r_Sb = T2('Sb', [dk, SW], BF16)
    p_stt = K.ps(std, [128, 512], F32, 'p_st')
    p_st = [p_stt[0:64, 0:64], p_stt[0:64, 0:64]]
    r_pst = [Res()] * 2
    p_o = K.ps(std, [128, 512], F32, 'p_o')
    r_po = Res()
    if mx == 'ml':
        p_den = K.ps(std, [128, 512], F32, 'p_den')
        r_pden = Res()
    p_trt = K.ps(std, [64, 8, 128], BF16, 'p_tr')
    p_tr = p_trt[:, :, 0:dk]
    r_ptr = Res()
    p_dst = [K.ps(std, [128, 512], F32, 'p_ds%d' % i) for i in range(2)]
    p_dsl = [p_dst[0][0:dk, 0:SW], p_dst[1][0:dk, 0:SW]]
    r_pdsl = [Res(), Res()]

    nch_ctr = [0]
    ng = PRM({'gla': 'gla_ng', 'hg': 'hg_ng', 'ml': 'ml_ng'}[mx])

    def gate_phase(d, bi, i, first):
        t0, nb = A_BLOCKS[bi]
        nchk = nb // 64
        if first and mx == 'ml':
            K.memset('dve', carG[:], 0.0, [r_car])
            K.memset('dve', carM[:], 0.0, [r_car])
        u, ru = load_u(bi)
        if d == 0 and mx != 'ml':
            p_work(bi, u, ru)

        def c3(ap):
            return ap.rearrange("p (c i) -> p c i", i=64)
        endi = 63 if d == 0 else 0

        if mx in ('gla', 'hg'):
            if mx == 'gla':
                p, rp = proj(u, ru, 'gla_lr', nb)
                K.copy('act', lrs[i][:, 0:nb], p, [rp], [r_lrs[i]])
                pp = pproj[pctr[0] % 2]
                rpp = r_pproj[pctr[0] % 2]
                pctr[0] += 1
                K.mm(pp[0:64, 0:nb], wlr[:, d * 64:(d + 1) * 64], lrs[i][:, 0:nb], True, True,
                     [r_wlr, r_lrs[i]], [rpp])
                K.act(glog[i][:, 0:nb], pp[0:64, 0:nb], AF.Exp, [rpp, r_nblr], [r_glog[i]],
                      bias=nblr[:, d:d + 1], scale=-1.0)
                K.act(glog[i][:, 0:nb], glog[i][:, 0:nb], AF.Ln, [r_glog[i], C['res']], [r_glog[i]],
                      bias=C['one'][0:64, :])
                ksrc, r_ksrc = k_bf[:, t0:t0 + nb], r_k[bi]
            else:
                p, rp = proj(u, ru, 'hg_f%d' % d, nb)
                K.act(kraw[i][:, 0:nb], p, AF.Sigmoid, [rp], [r_kraw[i]])
                K.ts('dve', kraw[i][:, 0:nb], kraw[i][:, 0:nb], lbt[:, 2 + d:3 + d], lbt[:, d:d + 1],
                     ALU.mult, ALU.add, [r_kraw[i], r_lbt], [r_kraw[i]])
                K.act(glog[i][:, 0:nb], kraw[i][:, 0:nb], AF.Ln, [r_kraw[i]], [r_glog[i]])
                K.ts('pool', kraw[i][:, 0:nb], kraw[i][:, 0:nb], -1.0, 1.0, ALU.mult, ALU.add, [r_kraw[i]],
                     [r_kraw[i]])
                ksrc, r_ksrc = kraw[i][:, 0:nb], r_kraw[i]
            ba, ga, ma = bb[i][:, 0:nb], glog[i][:, 0:nb], rmask[0:dk, d, 0:nb]
            if d == 1:
                ba, ga, ma = rev(ba), rev(ga), rev(ma)
            K.scan(ba, ma, ga, 0.0, ALU.mult, ALU.add, [r_glog[i], r_rmask], [r_bb[i]])
            b3 = c3(bb[i][:, 0:nb])
            bend = b3[:, :, endi]
            href = sm[i][:, 0:nchk]
            midi = 31 if d == 0 else 32
            sc = -1.0 / 16.0 if mx == 'gla' else 1.0
            K.copy('pool', href, b3[:, :, midi], [r_bb[i]], [r_sm[i]])
            K.act(sm[i][:, 8:8 + nchk], bend, AF.Exp, [r_bb[i]], [r_sm[i]], scale=sc)
            K.act(sm[i][:, 16:16 + nchk], b3[:, :, midi], AF.Exp, [r_bb[i]], [r_sm[i]], scale=sc)
            K.tt('dve', c3(d1[i][:, 0:nb]), b3, href.unsqueeze(2).to_broadcast([dk, nchk, 64]), ALU.subtract,
                 [r_bb[i], r_sm[i]], [r_d1[i]])
            K.act(e2b[i][:, 0:nb], d1[i][:, 0:nb], AF.Exp, [r_d1[i]], [r_e2b[i]], scale=sc)
            K.act(e3b[i][:, 0:nb], d1[i][:, 0:nb], AF.Exp, [r_d1[i]], [r_e3b[i]], scale=-sc)
            ratio = sm[i][:, 24:24 + nchk]
            K.tt('pool', ratio, bend, href, ALU.subtract, [r_bb[i], r_sm[i]], [r_sm[i]])
            K.act(ratio, ratio, AF.Exp, [r_sm[i]], [r_sm[i]], scale=sc)
            K.tt('dve', qt[i][:, 0:nb], q_bf[:, t0:t0 + nb], e2b[i][:, 0:nb], ALU.mult, [r_q[bi], r_e2b[i]],
                 [r_qt[i]])
            K.tt('dve', kt[i][:, 0:nb], ksrc, e3b[i][:, 0:nb], ALU.mult, [r_ksrc, r_e3b[i]], [r_kt[i]])
            K.tt('dve', ktz[i][:, 0:nb], kt[i][:, 0:nb], hmask[0:dk, d, 0:nb], ALU.mult, [r_kt[i], r_rmask],
                 [r_ktz[i]])
            K.tt('dve', c3(ke[i][:, 0:nb]), c3(kt[i][:, 0:nb]), ratio.unsqueeze(2).to_broadcast([dk, nchk, 64]),
                 ALU.mult, [r_kt[i], r_sm[i]], [r_ke[i]])
            dec_col = lambda c: sm[i][:, 8 + c:9 + c]
            eref_col = lambda c: sm[i][:, 16 + c:17 + c]
        else:
            p, rp = proj(u, ru, 'ml_i%d' % d, nb)
            K.act(ip[i][:, 0:nb], p, AF.Identity, [rp, r_prm], [r_ip[i]],
                  bias=PRM('ml_gb')[:, d * 2:d * 2 + 1])
            p, rp = proj(u, ru, 'ml_f%d' % d, nb)
            K.act(glog[i][:, 0:nb], p, AF.Exp, [rp, r_ngb], [r_glog[i]], bias=ngb[:, d * 2 + 1:d * 2 + 2],
                  scale=-1.0)
            K.act(glog[i][:, 0:nb], glog[i][:, 0:nb], AF.Ln, [r_glog[i], C['res']], [r_glog[i]],
                  bias=C['one'])
            ba, ga, oa = bb[i][:, 0:nb], glog[i][:, 0:nb], C['ones_f'][:, 0:nb]
            if d == 1:
                ba, ga = rev(ba), rev(ga)
            K.scan(ba, oa, ga, carG[:, 0:1], ALU.mult, ALU.add, [r_glog[i], C['res'], r_car], [r_bb[i]])
            K.tt('dve', d1[i][:, 0:nb], ip[i][:, 0:nb], bb[i][:, 0:nb], ALU.add, [r_ip[i], r_bb[i]],
                 [r_d1[i]])
            ma, aa_ = d2[i][:, 0:nb], d1[i][:, 0:nb]
            if d == 1:
                ma, aa_ = rev(ma), rev(aa_)
            K.scan(ma, aa_, aa_, carM[:, 0:1], ALU.max, ALU.max, [r_d1[i], r_car], [r_d2[i]])
            A3, M3 = c3(d1[i][:, 0:nb]), c3(d2[i][:, 0:nb])
            Rb = sm[i][:, 24:24 + nchk]
            if d == 0:
                K.copy('pool', sm[i][:, 24:25], carM[:, 0:1], [r_car], [r_sm[i]])
                if nchk > 1:
                    K.copy('pool', sm[i][:, 25:24 + nchk], M3[:, 0:nchk - 1, 63], [r_d2[i]], [r_sm[i]])
                Rn = M3[:, :, 63]
                last = nb - 1
            else:
                K.copy('pool', sm[i][:, 24 + nchk - 1:24 + nchk], carM[:, 0:1], [r_car], [r_sm[i]])
                if nchk > 1:
                    K.copy('pool', sm[i][:, 24:24 + nchk - 1], M3[:, 1:nchk, 0], [r_d2[i]], [r_sm[i]])
                Rn = M3[:, :, 0]
                last = 0
            K.tt('dve', clampt[i][:, 0:nb], bb[i][:, 0:nb], d2[i][:, 0:nb], ALU.subtract, [r_bb[i], r_d2[i]],
                 [r_cl[i]])
            K.act(clampt[i][:, 0:nb], clampt[i][:, 0:nb], AF.Exp, [r_cl[i]], [r_cl[i]])
            K.tt('dve', sm[i][:, 8:8 + nchk], Rb, Rn, ALU.subtract, [r_sm[i], r_d2[i]], [r_sm[i]])
            K.act(sm[i][:, 8:8 + nchk], sm[i][:, 8:8 + nchk], AF.Exp, [r_sm[i]], [r_sm[i]])
            K.copy('pool', carG[:, 0:1], bb[i][:, last:last + 1], [r_bb[i]], [r_car])
            K.copy('pool', carM[:, 0:1], d2[i][:, last:last + 1], [r_d2[i]], [r_car])
            Rbb = Rb.unsqueeze(2).to_broadcast([128, nchk, 64])
            K.tt('dve', c3(d3[i][:, 0:nb]), A3, Rbb, ALU.subtract, [r_d1[i], r_sm[i]], [r_d3[i]])
            K.act(e3b[i][:, 0:nb], d3[i][:, 0:nb], AF.Exp, [r_d3[i]], [r_e3b[i]])
            K.tt('dve', kt[i][:, 0:nb], k_bf[:, t0:t0 + nb], e3b[i][:, 0:nb], ALU.mult, [r_k[bi], r_e3b[i]],
                 [r_kt[i]])
            K.tt('dve', c3(glog[i][:, 0:nb]), M3, Rbb, ALU.subtract, [r_d2[i], r_sm[i]], [r_glog[i]])
            K.act(e2b[i][:, 0:nb], glog[i][:, 0:nb], AF.Exp, [r_glog[i], C['res']], [r_e2b[i]],
                  bias=C['lns'], scale=-1.0)
            K.tt('dve', qt[i][:, 0:nb], q_bf[:, t0:t0 + nb], e2b[i][:, 0:nb], ALU.mult, [r_q[bi], r_e2b[i]],
                 [r_qt[i]])
            K.tt('dve', c3(ke[i][:, 0:nb]), c3(kt[i][:, 0:nb]),
                 sm[i][:, 8:8 + nchk].unsqueeze(2).to_broadcast([128, nchk, 64]), ALU.mult, [r_kt[i], r_sm[i]],
                 [r_ke[i]])
            dec_col = lambda c: sm[i][:, 8 + c:9 + c]
            eref_col = None
        for c in range(nchk):
            K.tr(p_tr[:, c, :], ke[i][:, c * 64:(c + 1) * 64], C['ident_b'][0:dk, 0:dk], [r_ke[i], C['res']],
                 [r_ptr])
        K.copy('act', keT[i][:, 0:nchk, :], p_tr[:, 0:nchk, :], [r_ptr], [r_keT[i]])

    def chunk_phase(d, bi, i, first):
        t0, nb = A_BLOCKS[bi]
        nchk = nb // 64
        if first:
            K.memset('dve', Sfl[nch_ctr[0] % 2][:], 0.0, [r_Sfl[nch_ctr[0] % 2]])
        dec_col = lambda c: sm[i][:, 8 + c:9 + c]
        eref_col = (lambda c: sm[i][:, 16 + c:17 + c]) if mx != 'ml' else None
        corder = range(nchk) if d == 0 else range(nchk - 1, -1, -1)
        for c in corder:
            gch = t0 // 64 + c
            j = nch_ctr[0] % 2
            nch_ctr[0] += 1
            Sf, r_Sf = Sfl[j], r_Sfl[j]
            Sn, r_Sn = Sfl[1 - j], r_Sfl[1 - j]
            p_ds, r_pds = p_dsl[j], r_pdsl[j]
            cs = slice(c * 64, (c + 1) * 64)
            if eref_col is not None:
                K.act(Sb[j][:], Sf[:], AF.Copy, [r_Sf, r_sm[i]], [r_Sb[j]], scale=eref_col(c))
            else:
                K.copy('act', Sb[j][:], Sf[:], [r_Sf], [r_Sb[j]])
            if mx == 'ml':
                K.mm(p_st[j], kt[i][:, cs], qt[i][:, cs], True, True, [r_kt[i], r_qt[i]], [r_pst[j]])
            else:
                lo = slice(c * 64, c * 64 + 32)
                hi = slice(c * 64 + 32, c * 64 + 64)
                full_i, full_o, z_i, z_o = (hi, slice(32, 64), lo, slice(0, 32)) if d == 0 else \
                    (lo, slice(0, 32), hi, slice(32, 64))
                K.mm(p_st[j][:, full_o], kt[i][:, cs], qt[i][:, full_i], True, True, [r_kt[i], r_qt[i]],
                     [r_pst[j]])
                K.mm(p_st[j][:, z_o], ktz[i][:, cs], qt[i][:, z_i], True, True, [r_ktz[i], r_qt[i]],
                     [r_pst[j]])
            K.mm(p_ds[:, 0:128], keT[i][:, c, :], v_bf[:, gch, :], True, True, [r_keT[i], r_v[bi]], [r_pds])
            if mx == 'ml':
                K.mm(p_ds[:, 128:256], keT[i][:, c, :], C['ones_b'][0:64, :], True, True,
                     [r_keT[i], C['res']], [r_pds])
            K.tt('dve', stm[j][:], p_st[j], mask[:, d, :], ALU.mult, [r_pst[j], r_mask], [r_stm[j]])
            K.stt('dve', Sn[:], Sf[:], dec_col(c), p_ds, ALU.mult, ALU.add, [r_Sf, r_sm[i], r_pds], [r_Sn])
            K.mm(p_o[:, cs], v_bf[:, gch, :], stm[j][:], True, False, [r_v[bi], r_stm[j]], [r_po])
            K.mm(p_o[:, cs], Sb[j][:, 0:128], qt[i][:, cs], False, True, [r_Sb[j], r_qt[i]], [r_po])
            if mx == 'ml':
                K.mm(p_den[:, cs], C['ones_b'][0:64, :], stm[j][:], True, False, [r_stm[j], C['res']], [r_pden])
                K.mm(p_den[:, cs], Sb[j][:, 128:256], qt[i][:, cs], False, True, [r_Sb[j], r_qt[i]], [r_pden])
        if mx == 'ml':
            K.act(hht[i][:, 0:nb], p_den[:, 0:nb], AF.Abs, [r_pden], [r_hht[i]])
            K.tt('dve', hht[i][:, 0:nb], hht[i][:, 0:nb], clampt[i][:, 0:nb], ALU.max, [r_hht[i], r_cl[i]],
                 [r_hht[i]])
            K.recip(hht[i][:, 0:nb], hht[i][:, 0:nb], [r_hht[i]], [r_hht[i]])
            K.tt('dve', hht[i][:, 0:nb], p_o[:, 0:nb], hht[i][:, 0:nb], ALU.mult, [r_po, r_hht[i]], [r_hht[i]])
            if d == 0:
                K.copy('pool', o_acc[:, t0:t0 + nb], hht[i][:, 0:nb], [r_hht[i]], [r_oacc[bi]])
            else:
                K.tt('pool', o_acc[:, t0:t0 + nb], o_acc[:, t0:t0 + nb], hht[i][:, 0:nb], ALU.add,
                     [r_hht[i], r_oacc[bi]], [r_oacc[bi]])
        else:
            if d == 0:
                K.copy('act', o_acc[:, t0:t0 + nb], p_o[:, 0:nb], [r_po], [r_oacc[bi]])
            else:
                K.tt('dve', o_acc[:, t0:t0 + nb], p_o[:, 0:nb], o_acc[:, t0:t0 + nb], ALU.add,
                     [r_po, r_oacc[bi]], [r_oacc[bi]])
                final_block(fin_t, bi, o_acc, r_oacc, gate, r_gate, ng, pproj, r_pproj)

    seq = [(d, bi, k == 0) for d in range(2) for k, bi in enumerate(blk_order(d))]
    for s_ in range(len(seq) + 1):
        builders = []
        if s_ < len(seq):
            builders.append(lambda a=seq[s_], i=s_ % 2: gate_phase(a[0], a[1], i, a[2]))
        if s_ >= 1:
            builders.append(lambda a=seq[s_ - 1], i=(s_ - 1) % 2: chunk_phase(a[0], a[1], i, a[2]))
        S.run_streams(builders)
    S.barrier()
    std.close()
    if mx == 'ml':
        fin_t = final_alloc()
        for bi in range(NBLK):
            final_block(fin_t, bi, o_acc, r_oacc, gate, r_gate, ng, pproj, r_pproj)
    else:
        S.barrier()
        stpv.close()


def b_blocks(n):
    blks = [(0, 256, 1)]
    t = 256
    while t < n:
        nb = min(512, n - t)
        blks.append((t, nb, 0))
        t += nb
    return blks


def pack_B(inp, l, b, q, last):
    P = Pack()
    ca = inp['c_ctx'] if (not last and q == 0) else inp['c'][b]
    cT = np.stack([colT(inp['c'][b]), colT(ca)], axis=2).reshape(128, 16)
    P.add('cT', cT)
    P.add('adab', colT(inp['ada_b'][l]))
    P.add('gmix', colT(inp['norm_mix_g'][l]))
    P.add('gffn', colT(inp['norm_ffn_g'][l]))
    P.add('bm', colT(inp['b_merge'][l]))
    P.add('gfin', colT(inp['final_norm_g']))
    rb = np.concatenate([inp['moe_b_group'][l], inp['moe_b_expert'][l]])[None, :]
    P.add('rb', np.repeat(rb, 128, axis=0))
    return P


def emit_B_mods(K, st, C, adaw_d, PRM, r_prm):
    S = K.S
    mods = K.sb(st, [128, 6, 8, 2], F32, 'mods')
    r_mods = Res()
    with ExitStack() as st0:
        mod, r_mod = emit_mod(K, st0, C, adaw_d, 6144, PRM('cT'), PRM('adab'), r_prm)
        for dst, src, gname in ((0, 1, 'gmix'), (3, 4, 'gffn')):
            K.ts('dve', mods[:, dst], mod[:, src * 8:(src + 1) * 8, :], 1.0, None, ALU.add, None, [r_mod],
                 [r_mods])
            K.tt('dve', mods[:, dst], mods[:, dst], PRM(gname).unsqueeze(2).to_broadcast([128, 8, 2]),
                 ALU.mult, [r_mods, r_prm], [r_mods])
        for dst, src in ((1, 0), (2, 2), (4, 3), (5, 5)):
            K.copy('dve', mods[:, dst], mod[:, src * 8:(src + 1) * 8, :], [r_mod], [r_mods])
        S.barrier()
    return mods, r_mods


def emit_B(K, C, last, n, blks, mods, r_mods, PRM, r_prm, Wd, h_src, h_reads, ys_src, out_dst, out_res, hmid_d, u2_d):
    S = K.S
    gsc1, sh1, g1, gsc2, sh2, g2 = [mods[:, i] for i in range(6)]
    with ExitStack() as st:
        wtsT = K.sb(st, [16, n], F32, 'wtsT')
        r_wtsT = [Res() for _ in blks]
        r_hmid = [Res() for _ in blks]
        r_u2 = [Res() for _ in blks]

        with ExitStack() as st1:
            wm = K.sb(st1, [128, 8, 4096], BF16, 'wm')
            r_wm = Res()
            wmv = Wd['wm'].rearrange("(k p) n -> p k n", p=128)
            for k in range(8):
                K.dma('pool', wm[:, k, :], wmv[:, k, :], [], [r_wm])
            wbr = K.sb(st1, [128, 16, D], BF16, 'wbr')
            r_wbr = Res()
            wbrv = Wd['wbr'].rearrange("(j p) n -> p j n", p=128)
            for j in range(0, 16, 4):
                K.dma('pool', wbr[:, j:j + 4, :], wbrv[:, j:j + 4, :], [], [r_wbr])
            wo = K.sb(st1, [128, 8, D], BF16, 'wo')
            r_wo = Res()
            wov = Wd['wo'].rearrange("(k p) n -> p k n", p=128)
            for k in range(0, 8, 4):
                K.dma('pool', wo[:, k:k + 4, :], wov[:, k:k + 4, :], [], [r_wo])
            wr = K.sb(st1, [128, 8, 20], F32, 'wr')
            r_wr = Res()
            K.dma('act', wr[:], Wd['wr'].rearrange("(k p) n -> p k n", p=128), [], [r_wr])
            x = K.sb(st1, [128, 8, 512], F32, 'bx')
            r_x = Res()
            ysb = K.sb(st1, [128, 16, 512], BF16, 'bys')
            r_ys = Res()
            rstd = K.sb(st1, [128, 512], F32, 'brstd')
            r_rstd = Res()
            tmp = K.sb(st1, [128, 8, 512], F32, 'btmp')
            r_tmp = Res()
            u = K.sb(st1, [128, 8, 512], BF16, 'bu')
            r_u = Res()
            u2f, r_u2f = tmp, r_tmp
            merged = K.sb(st1, [128, 8, 512], BF16, 'bmerged')
            r_merged = Res()
            sq, r_sq = merged, r_merged
            gt = [K.sb(st1, [128, 512], F32, 'bgt%d' % i) for i in range(2)]
            r_gt = [Res(), Res()]
            mt = [K.sb(st1, [128, 512], F32, 'bmt%d' % i) for i in range(2)]
            r_mt = [Res(), Res()]
            macc = K.sb(st1, [128, 512], F32, 'bmacc')
            r_macc = Res()
            rt = K.sb(st1, [128, 64], F32, 'brt')
            r_rt = Res()
            pss = K.ps(st1, [128, 512], F32, 'bpss')
            r_pss = Res()
            pg = [K.ps(st1, [128, 512], F32, 'bpg%d' % i) for i in range(2)]
            r_pg = [Res(), Res()]
            pb = [K.ps(st1, [128, 512], F32, 'bpb%d' % i) for i in range(2)]
            r_pb = [Res(), Res()]
            pm = K.ps(st1, [128, 512], F32, 'bpm')
            r_pm = Res()
            pr = K.ps(st1, [128, 512], F32, 'bpr')
            r_pr = Res()
            ctr = 0
            for bi, (t0, nb, ty) in enumerate(blks):
                K.dma('sp', x[:, :, 0:nb], h_src(t0, nb), h_reads, [r_x])
                K.dma('act', ysb[:, :, 0:nb], ys_src(t0, nb), [], [r_ys])
                emit_norm_mod(K, C, x, r_x, nb, ty, gsc1, sh1, r_mods, sq, r_sq, pss, r_pss, rstd, r_rstd, tmp, r_tmp,
                              u, r_u)
                for dc in range(8):
                    for k in range(4):
                        i = ctr % 2
                        ctr += 1
                        for kc in range(8):
                            K.mm(pg[i][:, 0:nb], wm[:, kc, k * 1024 + dc * 128:k * 1024 + (dc + 1) * 128],
                                 u[:, kc, 0:nb], kc == 0, kc == 7, [r_wm, r_u], [r_pg[i]])
                        K.act(gt[i][:, 0:nb], pg[i][:, 0:nb], AF.Sigmoid, [r_pg[i], r_prm], [r_gt[i]],
                              bias=PRM('bm')[:, k * 8 + dc:k * 8 + dc + 1])
                        for cc in range(4):
                            K.mm(pb[i][:, 0:nb], wbr[:, k * 4 + cc, dc * 128:(dc + 1) * 128], ysb[:, k * 4 + cc, 0:nb],
                                 cc == 0, cc == 3, [r_wbr, r_ys], [r_pb[i]])
                        if k == 0:
                            K.tt('dve', macc[:, 0:nb], pb[i][:, 0:nb], gt[i][:, 0:nb], ALU.mult, [r_pb[i], r_gt[i]],
                                 [r_macc])
                        else:
                            K.tt('dve', mt[i][:, 0:nb], pb[i][:, 0:nb], gt[i][:, 0:nb], ALU.mult,
                                 [r_pb[i], r_gt[i]], [r_mt[i]])
                            if k < 3:
                                K.tt('pool', macc[:, 0:nb], macc[:, 0:nb], mt[i][:, 0:nb], ALU.add,
                                     [r_macc, r_mt[i]], [r_macc])
                            else:
                                K.tt('pool', merged[:, dc, 0:nb], macc[:, 0:nb], mt[i][:, 0:nb], ALU.add,
                                     [r_macc, r_mt[i]], [r_merged])
                for dc in range(8):
                    for kc in range(8):
                        K.mm(pm[:, 0:nb], wo[:, kc, dc * 128:(dc + 1) * 128], merged[:, kc, 0:nb], kc == 0, kc == 7,
                             [r_wo, r_merged], [r_pm])
                    K.stt('dve', x[:, dc, 0:nb], pm[:, 0:nb], g1[:, dc, ty:ty + 1], x[:, dc, 0:nb], ALU.mult, ALU.add,
                          [r_pm, r_mods, r_x], [r_x])
                K.dma('sp', hmid_d[:, :, t0:t0 + nb], x[:, :, 0:nb], [r_x], [r_hmid[bi]])
                emit_norm_mod(K, C, x, r_x, nb, ty, gsc2, sh2, r_mods, sq, r_sq, pss, r_pss, rstd, r_rstd, tmp, r_tmp,
                              u, r_u, out_f32=u2f, r_of=r_u2f)
                K.dma('act', u2_d[:, :, t0:t0 + nb], u[:, :, 0:nb], [r_u], [r_u2[bi]])
                for s0 in range(0, nb, 128):
                    m = min(128, nb - s0)
                    for k in range(8):
                        K.mm(pr[0:m, 0:20], u2f[:, k, s0:s0 + m], wr[:, k, :], k == 0, k == 7, [r_u2f, r_wr], [r_pr])
                    lg = rt[0:m, 0:20]
                    K.tt('dve', lg, pr[0:m, 0:20], PRM('rb', m), ALU.add, [r_pr, r_prm], [r_rt])
                    gmax, ngmax, gsum = rt[0:m, 20:21], rt[0:m, 21:22], rt[0:m, 22:23]
                    K.S.op('dve', (lambda o, i_: (lambda e: e.tensor_reduce(out=o, in_=i_, axis=mybir.AxisListType.X,
                                                                             op=ALU.max)))(gmax, rt[0:m, 0:4]),
                           [r_rt], [r_rt])
                    K.ts('dve', ngmax, gmax, -1.0, None, ALU.mult, None, [r_rt], [r_rt])
                    ge = rt[0:m, 24:28]
                    K.act(ge, rt[0:m, 0:4], AF.Exp, [r_rt], [r_rt], bias=ngmax)
                    K.S.op('dve', (lambda o, i_: (lambda e: e.tensor_reduce(out=o, in_=i_, axis=mybir.AxisListType.X,
                                                                             op=ALU.add)))(gsum, ge), [r_rt], [r_rt])
                    pgr = rt[0:m, 23:24]
                    K.recip(pgr, gsum, [r_rt], [r_rt])
                    pen = rt[0:m, 28:32]
                    K.ts('dve', pen, rt[0:m, 0:4], gmax, None, ALU.is_equal, None, [r_rt], [r_rt])
                    K.ts('dve', pen, pen, 1e30, -1e30, ALU.mult, ALU.add, [r_rt], [r_rt])
                    em = rt[0:m, 32:48]
                    K.tt('dve', em.rearrange("p (g e) -> p g e", e=4), rt[0:m, 4:20].rearrange("p (g e) -> p g e", e=4),
                         pen.unsqueeze(2).to_broadcast([m, 4, 4]), ALU.add, [r_rt], [r_rt])
                    m1, m2, dd = rt[0:m, 48:49], rt[0:m, 49:50], rt[0:m, 50:51]
                    K.S.op('dve', (lambda o, i_: (lambda e: e.tensor_reduce(out=o, in_=i_, axis=mybir.AxisListType.X,
                                                                             op=ALU.max)))(m1, em), [r_rt], [r_rt])
                    mk1 = rt[0:m, 4:20]
                    K.ts('dve', mk1, em, m1, None, ALU.is_equal, None, [r_rt], [r_rt])
                    K.stt('dve', em, mk1, -1e30, em, ALU.mult, ALU.add, [r_rt], [r_rt])
                    K.S.op('dve', (lambda o, i_: (lambda e: e.tensor_reduce(out=o, in_=i_, axis=mybir.AxisListType.X,
                                                                             op=ALU.max)))(m2, em), [r_rt], [r_rt])
                    K.ts('dve', em, em, m2, None, ALU.is_equal, None, [r_rt], [r_rt])
                    K.tt('dve', dd, m2, m1, ALU.subtract, [r_rt], [r_rt])
                    ee, wa, wb_ = rt[0:m, 51:52], rt[0:m, 52:53], rt[0:m, 53:54]
                    K.act(ee, dd, AF.Exp, [r_rt], [r_rt])
                    K.ts('dve', wa, ee, 1.0, None, ALU.add, None, [r_rt], [r_rt])
                    K.recip(wa, wa, [r_rt], [r_rt])
                    K.tt('dve', wb_, ee, wa, ALU.mult, [r_rt], [r_rt])
                    K.tt('dve', wa, wa, pgr, ALU.mult, [r_rt], [r_rt])
                    K.tt('dve', wb_, wb_, pgr, ALU.mult, [r_rt], [r_rt])
                    K.ts('dve', mk1, mk1, wa, None, ALU.mult, None, [r_rt], [r_rt])
                    K.stt('dve', mk1, em, wb_, mk1, ALU.mult, ALU.add, [r_rt], [r_rt])
                    K.tr(pr[0:16, 128:128 + m], mk1, C['ident_f'][0:m, 0:m], [r_rt, C['res']], [r_pr])
                    K.copy('act', wtsT[:, t0 + s0:t0 + s0 + m], pr[0:16, 128:128 + m], [r_pr], [r_wtsT[bi]])
            S.barrier()

        with ExitStack() as st2:
            h = K.sb(st2, [128, 8, n], F32, 'bh')
            r_h = [Res() for _ in blks]
            u2 = K.sb(st2, [128, 8, n], BF16, 'bu2')
            r_u2s = [Res() for _ in blks]
            for bi, (t0, nb, ty) in enumerate(blks):
                K.dma('sp', h[:, :, t0:t0 + nb], hmid_d[:, :, t0:t0 + nb], [r_hmid[bi]], [r_h[bi]])
                K.dma('act', u2[:, :, t0:t0 + nb], u2_d[:, :, t0:t0 + nb], [r_u2[bi]], [r_u2s[bi]])
            st2o = st2
            st2 = ExitStack()
            sel = K.sb(st2, [16, 16, 128], F32, 'sel')
            r_sel = Res()
            K.copy('dve', sel[:], C['ident_f'][0:16, 0:16].unsqueeze(2).to_broadcast([16, 16, 128]), [C['res']],
                   [r_sel])
            w1b = [K.sb(st2, [128, 8, 512], BF16, 'w1b%d' % i) for i in range(2)]
            w3b = [K.sb(st2, [128, 8, 512], BF16, 'w3b%d' % i) for i in range(2)]
            w2b = [K.sb(st2, [128, 4, D], BF16, 'w2b%d' % i) for i in range(2)]
            r_we = [Res(), Res()]
            wrep = [K.sb(st2, [128, 512], F32, 'wrep%d' % i) for i in range(2)]
            r_wrep = [Res(), Res()]
            sa = [K.sb(st2, [128, 512], F32, 'sa%d' % i) for i in range(2)]
            r_sa = [Res(), Res()]
            hid = [K.sb(st2, [128, 4, 512], BF16, 'hid%d' % i) for i in range(2)]
            r_hid = [Res(), Res()]
            pa = [K.ps(st2, [128, 512], F32, 'mpa%d' % i) for i in range(2)]
            r_pa = [Res(), Res()]
            pb2 = [K.ps(st2, [128, 512], F32, 'mpb%d' % i) for i in range(2)]
            r_pb2 = [Res(), Res()]
            py = [K.ps(st2, [128, 512], F32, 'mpy%d' % i) for i in range(2)]
            r_py = [Res(), Res()]
            pw = K.ps(st2, [128, 512], F32, 'mpw')
            r_pw = Res()
            cc = [0, 0]

            def stage1(e, bi, ib):
                ie = e % 2
                t0, nb, ty = blks[bi]
                if bi == 0:
                    K.dma('pool', w1b[ie][:], Wd['w1'][e].rearrange("(k p) n -> p k n", p=128), [], [r_we[ie]])
                    K.dma('pool', w3b[ie][:], Wd['w3'][e].rearrange("(k p) n -> p k n", p=128), [], [r_we[ie]])
                    K.dma('pool', w2b[ie][:], Wd['w2'][e].rearrange("(k p) n -> p k n", p=128), [], [r_we[ie]])
                K.mm(pw[:, 0:nb], sel[:, e, :], wtsT[:, t0:t0 + nb], True, True, [r_sel, r_wtsT[bi]], [r_pw])
                K.copy('act', wrep[ib][:, 0:nb], pw[:, 0:nb], [r_pw], [r_wrep[ib]])
                for hc in range(4):
                    i = cc[0] % 2
                    cc[0] += 1
                    for k in range(8):
                        K.mm(pa[i][:, 0:nb], w1b[ie][:, k, hc * 128:(hc + 1) * 128], u2[:, k, t0:t0 + nb],
                             k == 0, k == 7, [r_we[ie], r_u2s[bi]], [r_pa[i]])
                    for k in range(8):
                        K.mm(pb2[i][:, 0:nb], w3b[ie][:, k, hc * 128:(hc + 1) * 128], u2[:, k, t0:t0 + nb],
                             k == 0, k == 7, [r_we[ie], r_u2s[bi]], [r_pb2[i]])
                    K.act(sa[i][:, 0:nb], pa[i][:, 0:nb], AF.Silu, [r_pa[i]], [r_sa[i]])
                    K.tt('dve', sa[i][:, 0:nb], pb2[i][:, 0:nb], sa[i][:, 0:nb], ALU.mult, [r_pb2[i], r_sa[i]],
                         [r_sa[i]])
                    K.tt('pool' if hc % 2 else 'dve', hid[ib][:, hc, 0:nb], sa[i][:, 0:nb], wrep[ib][:, 0:nb],
                         ALU.mult, [r_sa[i], r_wrep[ib]], [r_hid[ib]])

            def stage2(e, bi, ib):
                ie = e % 2
                t0, nb, ty = blks[bi]
                for dc in range(8):
                    i = cc[1] % 2
                    cc[1] += 1
                    for hc in range(4):
                        K.mm(py[i][:, 0:nb], w2b[ie][:, hc, dc * 128:(dc + 1) * 128], hid[ib][:, hc, 0:nb],
                             hc == 0, hc == 3, [r_we[ie], r_hid[ib]], [r_py[i]])
                    K.stt('dve', h[:, dc, t0:t0 + nb], py[i][:, 0:nb], g2[:, dc, ty:ty + 1], h[:, dc, t0:t0 + nb],
                          ALU.mult, ALU.add, [r_py[i], r_mods, r_h[bi]], [r_h[bi]])

            items = [(e, bi) for e in range(16) for bi in range(len(blks))]
            for s_ in range(len(items) + 1):
                builders = []
                if s_ < len(items):
                    builders.append(lambda a=items[s_], ib=s_ % 2: stage1(a[0], a[1], ib))
                if s_ >= 1:
                    builders.append(lambda a=items[s_ - 1], ib=(s_ - 1) % 2: stage2(a[0], a[1], ib))
                S.run_streams(builders)
            S.barrier()
            st2.close()
            st2 = st2o
            if not last:
                for bi, (t0, nb, ty) in enumerate(blks):
                    K.dma('sp', out_dst(t0, nb), h[:, :, t0:t0 + nb], [r_h[bi]], [out_res])
            else:
                S.barrier()
                gz = K.sb(st2, [128, 2, 8, 1], F32, 'gz')
                r_gz = Res()
                K.copy('dve', gz[:, 0, :, 0], PRM('gfin'), [r_prm], [r_gz])
                K.memset('dve', gz[:, 1], 0.0, [r_gz])
                sq = K.sb(st2, [128, 8, 512], BF16, 'fsq')
                r_sq = Res()
                rstd = K.sb(st2, [128, 512], F32, 'frstd')
                r_rstd = Res()
                tmp = K.sb(st2, [128, 8, 512], F32, 'ftmp')
                r_tmp = Res()
                of = [K.sb(st2, [128, 8, 512], F32, 'fo%d' % i) for i in range(2)]
                r_of = [Res(), Res()]
                pw = K.ps(st2, [128, 512], F32, 'fpss')
                r_pw = Res()
                for bi, (t0, nb, ty) in enumerate(blks):
                    i = bi % 2
                    emit_norm_mod(K, C, h[:, :, t0:t0 + nb], r_h[bi], nb, 0, gz[:, 0], gz[:, 1], r_gz, sq, r_sq,
                                  pw, r_pw, rstd, r_rstd, tmp, r_tmp, of[i], r_of[i])
                    K.dma('sp', out_dst(t0, nb), of[i][:, :, 0:nb], [r_of[i]], [out_res])
            S.barrier()
        S.barrier()


NQ0 = NT // 4
NQ1 = SEQ // 4


def pack_Bf(inp, l, b):
    P = Pack()
    cT = np.stack([colT(inp['c'][b]), colT(inp['c_ctx'])], axis=2).reshape(128, 16)
    P.add('cT', cT)
    P.add('adab', colT(inp['ada_b'][l]))
    P.add('gmix', colT(inp['norm_mix_g'][l]))
    P.add('gffn', colT(inp['norm_ffn_g'][l]))
    P.add('bm', colT(inp['b_merge'][l]))
    P.add('gfin', colT(inp['final_norm_g']))
    rb = np.concatenate([inp['moe_b_group'][l], inp['moe_b_expert'][l]])[None, :]
    P.add('rb', np.repeat(rb, 128, axis=0))
    return P


def build_fused(offA, offB):
    nc = bass.Bass("TRN2", target_bir_lowering=False)
    wA, wB = offA['_w'], offB['_w']
    hT_d = nc.dram_tensor("hT", [D, NT], F32, kind="ExternalInput").ap()
    IN = []
    for l in range(2):
        d = {}
        d['adaw'] = nc.dram_tensor("adaw%d" % l, [D, 6144], F32, kind="ExternalInput").ap()
        d['win'] = nc.dram_tensor("win%d" % l, [4 * D, A_NCOL], F32, kind="ExternalInput").ap()
        d['prmA'] = nc.dram_tensor("prmA%d" % l, [4 * 128, wA], F32, kind="ExternalInput").ap()
        d['rgw'] = nc.dram_tensor("rgw%d" % l, [4 * 128, 512], F32, kind="ExternalInput").ap()
        d['wlr'] = nc.dram_tensor("wlr%d" % l, [4 * 32, 128], F32, kind="ExternalInput").ap()
        d['prmB'] = nc.dram_tensor("prmB%d" % l, [128, wB], F32, kind="ExternalInput").ap()
        d['wm'] = nc.dram_tensor("wm%d" % l, [D, 4096], F32, kind="ExternalInput").ap()
        d['wbr'] = nc.dram_tensor("wbr%d" % l, [2048, D], F32, kind="ExternalInput").ap()
        d['wo'] = nc.dram_tensor("wo%d" % l, [D, D], F32, kind="ExternalInput").ap()
        d['wr'] = nc.dram_tensor("wr%d" % l, [D, 20], F32, kind="ExternalInput").ap()
        d['w1'] = nc.dram_tensor("w1_%d" % l, [16, D, 512], F32, kind="ExternalInput").ap()
        d['w3'] = nc.dram_tensor("w3_%d" % l, [16, D, 512], F32, kind="ExternalInput").ap()
        d['w2'] = nc.dram_tensor("w2_%d" % l, [16, 512, D], F32, kind="ExternalInput").ap()
        IN.append(d)
    out_d = nc.dram_tensor("outT", [D, NQ1], F32, kind="ExternalOutput").ap()
    uT_d = nc.dram_tensor("uT_scr", [128, 8, NT], BF16).ap()
    ys_scr = [nc.dram_tensor("ys_scr%d" % l, [4, 4, 128, NT], BF16).ap() for l in range(2)]
    h1_d = nc.dram_tensor("h1_scr", [128, 8, NT], F32).ap()
    hmid_d = nc.dram_tensor("hmid_scr", [128, 8, NQ0], F32).ap()
    u2_d = nc.dram_tensor("u2_scr", [128, 8, NQ0], BF16).ap()
    hTv = hT_d.rearrange("(k p) t -> p k t", p=128)
    outv = out_d.rearrange("(k p) t -> p k t", p=128)

    with ExitStack() as st:
        K = KB(nc, st)
        S = K.S
        C = emit_consts(K, st)
        for l in range(2):
            d = IN[l]
            with ExitStack() as stl:
                prmA = K.sb(stl, [128, 4, wA], F32, 'prmA')
                r_prmA = Res()
                K.dma('sp', prmA[:], d['prmA'].rearrange("(h p) w -> p h w", p=128), [], [r_prmA])
                heads = []
                for hd in range(4):
                    def PRMh(name, rows=128, hd=hd):
                        o, w = offA[name]
                        return prmA[0:rows, hd, o:o + w]

                    def ys_dst(branch, t0, nb, hd=hd, l=l):
                        return ys_scr[l][branch, hd, :, t0:t0 + nb]
                    heads.append(dict(PRM=PRMh, r_prm=r_prmA,
                                      winv=d['win'][hd * D:(hd + 1) * D, :].rearrange("(k p) n -> p k n", p=128),
                                      rgw_d=d['rgw'][hd * 128:(hd + 1) * 128, :],
                                      wlr_d=d['wlr'][hd * 32:(hd + 1) * 32, :], ys_dst=ys_dst))
                emit_A(K, C, l, hTv if l == 0 else h1_d, d['adaw'][:, 0:2048], heads, offA, uT_d)
                S.barrier()
            with ExitStack() as stl:
                prmB = K.sb(stl, [128, wB], F32, 'prmB')
                r_prmB = Res()
                K.dma('sp', prmB[:], d['prmB'], [], [r_prmB])

                def PRMB(name, rows=128):
                    o, w = offB[name]
                    return prmB[0:rows, o:o + w]
                mods, r_mods = emit_B_mods(K, stl, C, d['adaw'], PRMB, r_prmB)
                ysv = ys_scr[l].rearrange("k c p t -> p (k c) t")
                if l == 0:
                    for j in range(4):
                        base = j * NQ0
                        if j == 0:
                            blks = [(0, 256, 1), (256, 512, 0), (768, 512, 0), (1280, 512, 0), (1792, 320, 0)]
                        else:
                            blks = [(0, 512, 0), (512, 512, 0), (1024, 512, 0), (1536, 512, 0), (2048, 64, 0)]
                        emit_B(K, C, False, NQ0, blks, mods, r_mods, PRMB, r_prmB, d,
                               (lambda t0, nb, base=base: hTv[:, :, base + t0:base + t0 + nb]), [],
                               (lambda t0, nb, base=base: ysv[:, :, base + t0:base + t0 + nb]),
                               (lambda t0, nb, base=base: h1_d[:, :, base + t0:base + t0 + nb]), Res(),
                               hmid_d, u2_d)
                else:
                    blks = [(i * 512, 512, 0) for i in range(4)]

                    def dyn(ap3):
                        def f(t0, nb):
                            return lambda e: ap3[:, :, bass.ds((e.partition_id() % 4) * NQ1 + (NCTX + t0), nb)]
                        return f
                    emit_B(K, C, True, NQ1, blks, mods, r_mods, PRMB, r_prmB, d,
                           dyn(h1_d), [], dyn(ysv), (lambda t0, nb: outv[:, :, t0:t0 + nb]), Res(),
                           hmid_d, u2_d)
                S.barrier()
        S.barrier()
        S.replay()
    return nc


_NC_CACHE = {}


def kernel(**inputs):
    inp = {k: np.asarray(v) for k, v in inputs.items()}
    in_maps = []
    offA = offB = None
    shared = {}
    for l in range(2):
        shared['adaw%d' % l] = inp['ada_w'][l]
        shared['win%d' % l] = np.ascontiguousarray(
            np.concatenate([gather_w_in(inp['w_in'][l], hd) for hd in range(4)], axis=0))
        shared['rgw%d' % l] = np.ascontiguousarray(
            np.concatenate([rg_gate_blockdiag(inp, l, hd) for hd in range(4)], axis=0))
        shared['wlr%d' % l] = np.ascontiguousarray(
            np.concatenate([gla_wlr_pad(inp, l, hd) for hd in range(4)], axis=0))
        shared['wm%d' % l] = inp['w_merge'][l]
        shared['wbr%d' % l] = np.ascontiguousarray(inp['w_branch'][l].reshape(2048, D))
        shared['wo%d' % l] = inp['w_out'][l]
        shared['wr%d' % l] = np.ascontiguousarray(
            np.concatenate([inp['moe_w_group'][l], inp['moe_w_expert'][l]], axis=1))
        shared['w1_%d' % l] = inp['moe_w1'][l]
        shared['w3_%d' % l] = inp['moe_w3'][l]
        shared['w2_%d' % l] = inp['moe_w2'][l]
    per_b = []
    for b in range(2):
        m = {'hT': np.ascontiguousarray(np.concatenate([inp['ctx'][b], inp['x'][b]], axis=0).T)}
        for l in range(2):
            packs = [pack_A(inp, l, b, hd) for hd in range(4)]
            offA = dict(packs[0].off)
            offA['_w'] = packs[0].w
            m['prmA%d' % l] = np.ascontiguousarray(np.concatenate([p.build() for p in packs], axis=0))
            PB = pack_Bf(inp, l, b)
            offB = dict(PB.off)
            offB['_w'] = PB.w
            m['prmB%d' % l] = PB.build()
        per_b.append(m)
    for core in range(8):
        m = dict(shared)
        m.update(per_b[core // 4])
        in_maps.append(m)
    if 'fused' not in _NC_CACHE:
        _NC_CACHE['fused'] = build_fused(offA, offB)
    res = run_bass_kernel_spmd(_NC_CACHE['fused'], in_maps, core_ids=list(range(8)))
    out = np.zeros((2, SEQ, D), np.float32)
    for c in range(8):
        b, q = c // 4, c % 4
        out[b, q * NQ1:(q + 1) * NQ1, :] = np.asarray(res.results[c]['outT']).T
    return out
```

```python
from contextlib import ExitStack
import numpy as np
import concourse.bass as bass
import concourse.mybir as mybir
from concourse.bass_utils import run_bass_kernel_spmd

F32 = mybir.dt.float32
BF16 = mybir.dt.bfloat16
ALU = mybir.AluOpType
AF = mybir.ActivationFunctionType

ENGS = ['pe', 'act', 'dve', 'pool', 'sp']
NDMASEM = 4

D = 1024
NCTX = 256
SEQ = 8192
NT = NCTX + SEQ
EPS = 1e-6
CH = 64
NCHUNK = NT // CH


class Res:
    __slots__ = ('name', 'w', 'r')

    def __init__(self, name=None):
        self.name = name
        self.w = None
        self.r = {}


MAX_EPOCH = 4
EMBED_WAIT = 1
SEM_SWITCH = 28000


class Sched:
    def __init__(self, nc, stack):
        self.nc = nc
        self.stack = stack
        self.epoch = 0
        self.tot = {}
        self.cur = None
        self.sim_tw = {}
        self.sim_tr = {}
        self.sim_free = {}
        self._new_sems()
        self.prog = {e: [] for e in ENGS}
        self.ninst = 0

    def _new_sems(self):
        nc, stack, ep = self.nc, self.stack, self.epoch
        self.sem = {e: stack.enter_context(nc.semaphore('sm%d_%s' % (ep, e))) for e in ENGS}
        self.cnt = {e: 0 for e in ENGS}
        self.dsem = {e: [stack.enter_context(nc.semaphore('dq%d_%s%d' % (ep, e, i))) for i in range(NDMASEM)]
                     for e in ('sp', 'act', 'pool')}
        self.dcnt = {e: 0 for e in ('sp', 'act', 'pool')}
        self.seen = {e: {} for e in ENGS}
        self.hist = {}
        self.hq = []
        self.gseq = 0

    def _semof(self, key, count):
        if isinstance(key, str):
            return self.sem[key], count
        q, slot = key
        return self.dsem[q][slot], 16 * count

    def _deps(self, eng, reads, writes):
        need = {}
        ep = self.epoch

        def add(key, count, epoch):
            if epoch != ep:
                return
            if key == 'pe' and eng == 'pe':
                return
            if need.get(key, 0) < count:
                need[key] = count
        for r in reads:
            if r.w is not None:
                add(*r.w)
        for w in writes:
            if w.w is not None:
                add(*w.w)
            for k, (c, e_) in w.r.items():
                add(k, c, e_)
        out = []
        clock = self.seen[eng]
        hist = self.hist
        items = sorted(need.items(), key=lambda kc: -hist.get(kc, (0, None))[0])
        for key, count in items:
            if clock.get(key, 0) >= count:
                continue
            out.append(self._semof(key, count))
            h = hist.get((key, count))
            if h is not None and h[1] is not None:
                for k2, c2 in h[1].items():
                    if clock.get(k2, 0) < c2:
                        clock[k2] = c2
            clock[key] = count
        return out

    def _record(self, eng, key, count):
        self.gseq += 1
        snap = dict(self.seen[eng])
        self.hist[(key, count)] = (self.gseq, snap)
        self.hq.append((key, count))
        if len(self.hq) > 6000:
            old = self.hq.pop(0)
            self.hist.pop(old, None)

    def _emit(self, eng, waits, fn, sem, inc):
        def run(e, waits=waits, fn=fn, sem=sem, inc=inc):
            ne = min(len(waits), EMBED_WAIT)
            for s, v in waits[:len(waits) - ne]:
                e.wait_ge(s, v)
            ins = fn(e)
            for s, v in waits[len(waits) - ne:]:
                ins._wait_ge(s, v)
            ins.then_inc(sem, inc)
        self.prog[eng].append(run)
        self.ninst += 1

    def _sim_start(self, eng, reads, writes, isdma):
        t = 0.0
        tw, tr = self.sim_tw, self.sim_tr
        for r in reads:
            v = tw.get(id(r))
            if v is not None and v[0] > t and not (v[1] == 'pe' and eng == 'pe'):
                t = v[0]
        for w in writes:
            v = tw.get(id(w))
            if v is not None and v[0] > t and not (v[1] == 'pe' and eng == 'pe'):
                t = v[0]
            v = tr.get(id(w))
            if v is not None and v > t:
                t = v
        t += 0.15
        key = ('q', eng) if isdma else eng
        return max(t, self.sim_free.get(key, 0.0))

    def _sim_commit(self, eng, reads, writes, isdma, cost):
        st = self._sim_start(eng, reads, writes, isdma)
        key = ('q', eng) if isdma else eng
        if isdma:
            self.sim_free[key] = st + 0.1
            fin = st + (cost or 3.0)
        else:
            fin = st + (cost or 0.5)
            self.sim_free[key] = fin
        for r in reads:
            if self.sim_tr.get(id(r), 0.0) < fin:
                self.sim_tr[id(r)] = fin
        for w in writes:
            self.sim_tw[id(w)] = (fin, eng)
            self.sim_tr.pop(id(w), None)

    def run_streams(self, builders):
        assert self.cur is None
        lists = []
        for b in builders:
            self.cur = []
            b()
            lists.append(self.cur)
        self.cur = None
        pos = [0] * len(lists)
        while True:
            best, bt = None, None
            for k, L in enumerate(lists):
                if pos[k] < len(L):
                    kind, a, cost = L[pos[k]]
                    t = self._sim_start(a[0], a[2], a[3], kind == 'dma')
                    if bt is None or t < bt:
                        best, bt = k, t
            if best is None:
                break
            kind, a, cost = lists[best][pos[best]]
            pos[best] += 1
            (self.op if kind == 'op' else self.dma)(*a, cost=cost)

    def op(self, eng, fn, reads=(), writes=(), cost=None):
        if self.cur is not None:
            self.cur.append(('op', (eng, fn, tuple(reads), tuple(writes)), cost))
            return
        self._sim_commit(eng, reads, writes, False, cost)
        waits = self._deps(eng, reads, writes)
        self.cnt[eng] += 1
        c = self.cnt[eng]
        ep = self.epoch
        self._emit(eng, waits, fn, self.sem[eng], 1)
        self._record(eng, eng, c)
        for r in reads:
            old = r.r.get(eng)
            if old is None or old[1] != ep or old[0] < c:
                r.r[eng] = (c, ep)
        for w in writes:
            w.w = (eng, c, ep)
            w.r = {}

    def dma(self, q, fn, reads=(), writes=(), cost=None):
        if self.cur is not None:
            self.cur.append(('dma', (q, fn, tuple(reads), tuple(writes)), cost))
            return
        self._sim_commit(q, reads, writes, True, cost)
        i = self.dcnt[q]
        self.dcnt[q] += 1
        slot = i % NDMASEM
        count = i // NDMASEM + 1
        key = (q, slot)
        ep = self.epoch
        waits = self._deps(q, reads, writes)
        if count > 1 and self.seen[q].get(key, 0) < count - 1:
            self.seen[q][key] = count - 1
            waits.append(self._semof(key, count - 1))
        self._emit(q, waits, fn, self.dsem[q][slot], 16)
        self._record(q, key, count)
        for r in reads:
            old = r.r.get(key)
            if old is None or old[1] != ep or old[0] < count:
                r.r[key] = (count, ep)
        for w in writes:
            w.w = (key, count, ep)
            w.r = {}

    def barrier(self):
        assert self.cur is None
        keys = [(e, self.cnt[e]) for e in ENGS if self.cnt[e] > 0]
        for q in ('sp', 'act', 'pool'):
            n = self.dcnt[q]
            for slot in range(NDMASEM):
                if n > slot:
                    keys.append(((q, slot), (n - 1 - slot) // NDMASEM + 1))
        for eng in ENGS:
            waits = []
            seen = self.seen[eng]
            for key, count in keys:
                if key == eng or seen.get(key, 0) >= count:
                    continue
                seen[key] = count
                waits.append(self._semof(key, count))

            def run(e, waits=waits):
                for s, v in waits:
                    e.wait_ge(s, v)
            self.prog[eng].append(run)
        if max(list(self.cnt.values()) + [v // NDMASEM for v in self.dcnt.values()]) > SEM_SWITCH \
                and self.epoch < MAX_EPOCH:
            self.tot = {e: self.tot.get(e, 0) + self.cnt[e] for e in ENGS}
            self.epoch += 1
            self._new_sems()

    def replay(self):
        nc = self.nc
        with nc.Block() as block:
            def mk(name):
                def f(e):
                    for run in self.prog[name]:
                        run(e)
                return f
            block.tensor(mk('pe'))
            block.scalar(mk('act'))
            block.vector(mk('dve'))
            block.gpsimd(mk('pool'))
            block.sync(mk('sp'))


class KB:
    def __init__(self, nc, st):
        self.nc = nc
        self.st = st
        self.S = Sched(nc, st)
        self.n = 0

    def sb(self, st, shape, dt=F32, name=None):
        self.n += 1
        return st.enter_context(self.nc.sbuf_tensor('%s_s%d' % (name or 't', self.n), list(shape), dt))

    def ps(self, st, shape, dt=F32, name=None):
        self.n += 1
        return st.enter_context(self.nc.psum_tensor('%s_p%d' % (name or 'p', self.n), list(shape), dt))

    @staticmethod
    def ecost(eng, out):
        n = int(np.prod(out.shape[1:]))
        return (0.13 + n / 900.0) if eng == 'dve' else (0.2 + n / 430.0)

    def mm(self, out, lhsT, rhs, start, stop, reads, writes):
        self.S.op('pe', lambda e: e.matmul(out, lhsT=lhsT, rhs=rhs, start=start, stop=stop), reads, writes,
                  cost=0.065 + int(np.prod(out.shape[1:])) / 2400.0)

    def tr(self, out, in_, ident, reads, writes):
        self.S.op('pe', lambda e: e.transpose(out=out, in_=in_, identity=ident), reads, writes, cost=0.12)

    def act(self, out, in_, func, reads, writes, bias=None, scale=None):
        kw = {}
        if bias is not None:
            kw['bias'] = bias
        if scale is not None:
            kw['scale'] = scale
        self.S.op('act', lambda e: e.activation(out=out, in_=in_, func=func, **kw), reads, writes,
                  cost=0.2 + int(np.prod(out.shape[1:])) / 1100.0)

    def tt(self, eng, out, in0, in1, op, reads, writes):
        self.S.op(eng, lambda e: e.tensor_tensor(out=out, in0=in0, in1=in1, op=op), reads, writes,
                  cost=self.ecost(eng, out))

    def ts(self, eng, out, in0, s1, s2, op0, op1, reads, writes):
        if s2 is None:
            self.S.op(eng, lambda e: e.tensor_scalar(out=out, in0=in0, scalar1=s1, scalar2=None, op0=op0),
                      reads, writes, cost=self.ecost(eng, out))
        else:
            self.S.op(eng, lambda e: e.tensor_scalar(out=out, in0=in0, scalar1=s1, scalar2=s2, op0=op0, op1=op1),
                      reads, writes, cost=self.ecost(eng, out))

    def stt(self, eng, out, in0, scalar, in1, op0, op1, reads, writes):
        self.S.op(eng, lambda e: e.scalar_tensor_tensor(out=out, in0=in0, scalar=scalar, in1=in1, op0=op0, op1=op1),
                  reads, writes, cost=self.ecost(eng, out))

    def copy(self, eng, out, in_, reads, writes):
        if eng == 'act':
            self.act(out, in_, AF.Copy, reads, writes)
        else:
            self.S.op(eng, lambda e: e.tensor_copy(out=out, in_=in_), reads, writes, cost=self.ecost(eng, out))

    def scan(self, out, d0, d1, init, op0, op1, reads, writes):
        self.S.op('dve', lambda e: e.tensor_tensor_scan(out=out, data0=d0, data1=d1, initial=init, op0=op0, op1=op1),
                  reads, writes, cost=self.ecost('dve', out))

    def recip(self, out, in_, reads, writes):
        self.S.op('dve', lambda e: e.reciprocal(out=out, in_=in_), reads, writes, cost=self.ecost('dve', out))

    def memset(self, eng, ap, val, writes):
        self.S.op(eng, lambda e: e.memset(ap, val), (), writes)

    def asel(self, out, in_, pattern, cmp, fill, base, cm, reads, writes):
        self.S.op('pool', lambda e: e.affine_select(out=out, in_=in_, pattern=pattern, compare_op=cmp, fill=fill,
                                                    base=base, channel_multiplier=cm), reads, writes)

    def dma(self, q, out, in_, reads, writes):
        def fn(e):
            o = out(e) if callable(out) else out
            i = in_(e) if callable(in_) else in_
            return e.dma_start(out=o, in_=i)
        self.S.dma(q, fn, reads, writes)


def rev(ap):
    (ps_, pn), (st_, n) = ap.ap
    return bass.AP(ap.tensor, ap.offset + (n - 1) * st_, [[ps_, pn], [-st_, n]])


def colT(v):
    v = np.asarray(v, np.float32)
    return np.ascontiguousarray(v.reshape(-1, 128).T)


class Pack:
    def __init__(self):
        self.items = []
        self.off = {}
        self.w = 0

    def add(self, name, arr):
        arr = np.asarray(arr, np.float32)
        if arr.ndim == 1:
            arr = arr[:, None]
        assert arr.shape[0] <= 128
        if arr.shape[0] < 128:
            arr = np.concatenate([arr, np.zeros((128 - arr.shape[0], arr.shape[1]), np.float32)], 0)
        self.off[name] = (self.w, arr.shape[1])
        self.items.append(arr)
        self.w += arr.shape[1]

    def build(self):
        return np.ascontiguousarray(np.concatenate(self.items, axis=1))


IN_OFF = {}
_o = 0
for _n, _w in (('rg_x', 512), ('rg_y', 512), ('gla_q', 256), ('gla_k', 256), ('gla_v', 512), ('gla_g', 512),
               ('gla_lr', 32), ('hg_q', 512), ('hg_i', 512), ('hg_f', 1024), ('hg_g', 512), ('ml_q', 512),
               ('ml_k', 512), ('ml_v', 512), ('ml_o', 512), ('ml_if', 16)):
    IN_OFF[_n] = _o
    _o += _w
assert _o == 7216

A_COLS = [('rg_x', 128), ('rg_y', 128),
          ('gla_q', 64), ('gla_k', 64), ('gla_g', 128), ('gla_lr', 32), ('gla_v', 128),
          ('hg_q', 128), ('hg_f0', 128), ('hg_f1', 128), ('hg_g', 128), ('hg_i', 128),
          ('ml_q', 128), ('ml_k', 128), ('ml_o', 128), ('ml_v', 128),
          ('ml_i0', 128), ('ml_f0', 128), ('ml_i1', 128), ('ml_f1', 128)]
A_OFF = {}
_o = 0
for _n, _w in A_COLS:
    A_OFF[_n] = (_o, _w)
    _o += _w
A_NCOL = _o


def gather_w_in(w_in_l, hd):
    cols = {}
    o = IN_OFF
    cols['rg_x'] = w_in_l[:, o['rg_x'] + hd * 128: o['rg_x'] + (hd + 1) * 128]
    cols['rg_y'] = w_in_l[:, o['rg_y'] + hd * 128: o['rg_y'] + (hd + 1) * 128]
    cols['gla_q'] = w_in_l[:, o['gla_q'] + hd * 64: o['gla_q'] + (hd + 1) * 64]
    cols['gla_k'] = w_in_l[:, o['gla_k'] + hd * 64: o['gla_k'] + (hd + 1) * 64]
    cols['gla_v'] = w_in_l[:, o['gla_v'] + hd * 128: o['gla_v'] + (hd + 1) * 128]
    cols['gla_g'] = w_in_l[:, o['gla_g'] + hd * 128: o['gla_g'] + (hd + 1) * 128]
    cols['gla_lr'] = w_in_l[:, o['gla_lr']: o['gla_lr'] + 32]
    cols['hg_q'] = w_in_l[:, o['hg_q'] + hd * 128: o['hg_q'] + (hd + 1) * 128]
    cols['hg_i'] = w_in_l[:, o['hg_i'] + hd * 128: o['hg_i'] + (hd + 1) * 128]
    for d in range(2):
        cols['hg_f%d' % d] = w_in_l[:, o['hg_f'] + d * 512 + hd * 128: o['hg_f'] + d * 512 + (hd + 1) * 128]
    cols['hg_g'] = w_in_l[:, o['hg_g'] + hd * 128: o['hg_g'] + (hd + 1) * 128]
    for nm in ('ml_q', 'ml_k', 'ml_v', 'ml_o'):
        cols[nm] = w_in_l[:, o[nm] + hd * 128: o[nm] + (hd + 1) * 128]
    for d in range(2):
        for g, gn in enumerate(('i', 'f')):
            c = o['ml_if'] + d * 8 + g * 4 + hd
            cols['ml_%s%d' % (gn, d)] = np.repeat(w_in_l[:, c:c + 1], 128, axis=1)
    return np.ascontiguousarray(np.concatenate([cols[n] for n, _ in A_COLS], axis=1))


A_BLOCKS = [(0, NCTX)] + [(NCTX + i * 512, 512) for i in range(SEQ // 512)]
NBLK = len(A_BLOCKS)


def blk_order(d):
    return list(range(NBLK)) if d == 0 else [0] + list(range(NBLK - 1, 0, -1))


def emit_consts(K, st):
    C = {}
    r = Res()
    C['res'] = r
    ones_f = K.sb(st, [128, 512], F32, 'ones_f')
    K.memset('pool', ones_f[:], 1.0, [r])
    C['ones_f'] = ones_f
    ones_b = K.sb(st, [128, 128], BF16, 'ones_b')
    K.memset('pool', ones_b[:], 1.0, [r])
    C['ones_b'] = ones_b
    cc = K.sb(st, [128, 4], F32, 'cconst')
    K.memset('pool', cc[:, 0:1], 1.0, [r])
    K.memset('pool', cc[:, 1:2], EPS, [r])
    K.memset('pool', cc[:, 2:3], 0.0, [r])
    K.memset('pool', cc[:, 3:4], float(np.log(128.0 ** -0.5)), [r])
    C['one'] = cc[:, 0:1]
    C['eps'] = cc[:, 1:2]
    C['zero'] = cc[:, 2:3]
    C['lns'] = cc[:, 3:4]
    ident_f = K.sb(st, [128, 128], F32, 'ident_f')
    K.memset('pool', ident_f[:], 0.0, [r])
    K.asel(ident_f[:], ident_f[:], [[-1, 128]], ALU.not_equal, 1.0, 0, 1, [r], [r])
    C['ident_f'] = ident_f
    ident_b = K.sb(st, [128, 128], BF16, 'ident_b')
    K.copy('pool', ident_b[:], ident_f[:], [r], [r])
    C['ident_b'] = ident_b
    return C


def emit_mod(K, st, C, ada_w_d, ncol, cT_ap, adab_ap, r_prm):
    nch = ncol // 128
    sc = K.sb(st, [128, 16], F32, 'silu_c')
    r_sc = Res()
    K.act(sc[:], cT_ap, AF.Silu, [r_prm], [r_sc])
    mod = K.sb(st, [128, nch, 2], F32, 'mod')
    r_mod = Res()
    with ExitStack() as st2:
        wbuf = [K.sb(st2, [128, 8, 512], F32, 'adaw%d' % i) for i in range(2)]
        rw = [Res(), Res()]
        row = K.sb(st2, [2, ncol], F32, 'modrow')
        r_row = Res()
        prow = [K.ps(st2, [128, 512], F32, 'ps_row%d' % i) for i in range(2)]
        r_prow = [Res(), Res()]
        pm = K.ps(st2, [128, 512], F32, 'ps_mod')
        r_pm = Res()
        wv = ada_w_d.rearrange("(k p) n -> p k n", p=128)
        sc3 = sc[:].rearrange("p (k t) -> p k t", t=2)
        for g in range(ncol // 512):
            wb, rb = wbuf[g % 2], rw[g % 2]
            pr_, rpr = prow[g % 2], r_prow[g % 2]
            K.dma('sp' if g % 2 == 0 else 'act', wb[:], wv[:, :, g * 512:(g + 1) * 512], [], [rb])
            for k in range(8):
                K.mm(pr_[0:2, :], sc3[:, k, :], wb[:, k, :], k == 0, k == 7, [rb, r_sc], [rpr])
            K.copy('dve', row[:, g * 512:(g + 1) * 512], pr_[0:2, :], [rpr], [r_row])
        for ch in range(nch):
            K.tr(pm[:, ch * 2:ch * 2 + 2], row[:, ch * 128:(ch + 1) * 128], C['ident_f'][0:2, 0:2], [r_row, C['res']],
                 [r_pm])
        K.tt('dve', mod[:], pm[:, 0:nch * 2].rearrange("p (c t) -> p c t", t=2),
             adab_ap.unsqueeze(2).to_broadcast([128, nch, 2]), ALU.add, [r_pm, r_prm], [r_mod])
        K.S.barrier()
    return mod, r_mod


def emit_norm_mod(K, C, x, rx, nb, ty, gsc, sh, r_gs, sq, r_sq, pss, r_pss, rstd, r_rstd, tmp, r_tmp, out, r_out,
                  out_f32=None, r_of=None, mul_eng='pool'):
    K.act(sq[:, :, 0:nb], x[:, :, 0:nb], AF.Square, [rx], [r_sq])
    for k in range(8):
        K.mm(pss[:, 0:nb], C['ones_b'][:], sq[:, k, 0:nb], k == 0, k == 7, [r_sq, C['res']], [r_pss])
    K.act(rstd[:, 0:nb], pss[:, 0:nb], AF.Sqrt, [r_pss, C['res']], [r_rstd], bias=C['eps'], scale=1.0 / D)
    K.recip(rstd[:, 0:nb], rstd[:, 0:nb], [r_rstd], [r_rstd])
    for k in range(8):
        K.tt('dve', tmp[:, k, 0:nb], x[:, k, 0:nb], rstd[:, 0:nb], ALU.mult, [rx, r_rstd], [r_tmp])
    for k in range(8):
        if out_f32 is not None:
            K.ts(mul_eng, out_f32[:, k, 0:nb], tmp[:, k, 0:nb], gsc[:, k, ty:ty + 1], sh[:, k, ty:ty + 1],
                 ALU.mult, ALU.add, [r_tmp, r_gs], [r_of])
            K.copy('act', out[:, k, 0:nb], out_f32[:, k, 0:nb], [r_of], [r_out])
        else:
            K.ts(mul_eng, out[:, k, 0:nb], tmp[:, k, 0:nb], gsc[:, k, ty:ty + 1], sh[:, k, ty:ty + 1],
                 ALU.mult, ALU.add, [r_tmp, r_gs], [r_out])


def pack_A(inp, l, b, hd):
    P = Pack()
    cT = np.stack([colT(inp['c'][b]), colT(inp['c_ctx'])], axis=2).reshape(128, 16)
    P.add('cT', cT)
    P.add('adab', colT(inp['ada_b'][l][0:2048]))
    P.add('gmix', colT(inp['norm_mix_g'][l]))
    hs = slice(hd * 128, (hd + 1) * 128)
    P.add('rg_cw', inp['rg_conv_w'][l][:, hs].T)
    P.add('rg_cb', inp['rg_conv_b'][l][hs])
    P.add('rg_gb', inp['rg_gate_b'][l][:, :, hs].reshape(4, 128).T)
    P.add('rg_lam', inp['rg_lambda'][l][:, hs].T)
    P.add('gla_blr', inp['gla_b_lr'][l][:, hd * 64:(hd + 1) * 64].T)
    P.add('gla_ng', inp['gla_norm_g'][l])
    P.add('hg_l0', inp['hgrn_lb_logits'][0][:, hs].T)
    P.add('hg_l1', inp['hgrn_lb_logits'][1][:, hs].T)
    P.add('hg_ng', inp['hgrn_norm_g'][l])
    P.add('ml_cwq', inp['ml_conv_w'][l][:, hs].T)
    P.add('ml_cwk', inp['ml_conv_w'][l][:, 512 + hd * 128: 512 + (hd + 1) * 128].T)
    P.add('ml_cbq', inp['ml_conv_b'][l][hs])
    P.add('ml_cbk', inp['ml_conv_b'][l][512 + hd * 128: 512 + (hd + 1) * 128])
    gb = inp['ml_gate_b'][l][:, :, hd].reshape(4)
    P.add('ml_gb', np.repeat(gb[None, :], 128, axis=0))
    P.add('ml_ng', inp['ml_norm_g'][l])
    return P


def rg_gate_blockdiag(inp, l, hd):
    out = np.zeros((128, 4, 128), np.float32)
    gw = inp['rg_gate_w'][l]
    for d in range(2):
        for g in range(2):
            for kk in range(2):
                out[kk * 64:(kk + 1) * 64, d * 2 + g, kk * 64:(kk + 1) * 64] = gw[d, g, hd * 2 + kk]
    return np.ascontiguousarray(out.reshape(128, 512))


def gla_wlr_pad(inp, l, hd):
    out = np.zeros((32, 2, 64), np.float32)
    for d in range(2):
        out[d * 16:(d + 1) * 16, d, :] = inp['gla_w_lr'][l][d][:, hd * 64:(hd + 1) * 64]
    return np.ascontiguousarray(out.reshape(32, 128))


def emit_A(K, C, l, hsrc, adaw_d, heads, prm_off, uT_d, mixers=('rg', 'gla', 'hg', 'ml')):
    S = K.S
    PRM, r_prm = heads[0]['PRM'], heads[0]['r_prm']
    with ExitStack() as st:
        gsc = K.sb(st, [128, 8, 2], F32, 'gsc')
        sh = K.sb(st, [128, 8, 2], F32, 'sh')
        r_gs = Res()
        with ExitStack() as st0:
            mod, r_mod = emit_mod(K, st0, C, adaw_d, 2048, PRM('cT'), PRM('adab'), r_prm)
            K.ts('dve', gsc[:], mod[:, 8:16, :], 1.0, None, ALU.add, None, [r_mod], [r_gs])
            K.tt('dve', gsc[:], gsc[:], PRM('gmix').unsqueeze(2).to_broadcast([128, 8, 2]), ALU.mult,
                 [r_gs, r_prm], [r_gs])
            K.copy('dve', sh[:], mod[:, 0:8, :], [r_mod], [r_gs])
            S.barrier()

        ures = [Res() for _ in range(NBLK)]
        with ExitStack() as st1:
            xb_ = [K.sb(st1, [128, 8, 512], F32, 'x%d' % i) for i in range(2)]
            rx_ = [Res(), Res()]
            sq = K.sb(st1, [128, 8, 512], BF16, 'sq')
            r_sq = Res()
            pss = [K.ps(st1, [128, 512], F32, 'pss%d' % i) for i in range(2)]
            r_pss = [Res(), Res()]
            rstd = [K.sb(st1, [128, 512], F32, 'rstd%d' % i) for i in range(2)]
            r_rstd = [Res(), Res()]
            tmp = K.sb(st1, [128, 8, 512], F32, 'tmp')
            r_tmp = Res()
            ub = [K.sb(st1, [128, 8, 512], BF16, 'u%d' % i) for i in range(2)]
            r_ub = [Res(), Res()]
            for bi, (t0, nb) in enumerate(A_BLOCKS):
                i2 = bi % 2
                ty = 1 if bi == 0 else 0
                K.dma('sp', xb_[i2][:, :, 0:nb], hsrc[:, :, t0:t0 + nb], [], [rx_[i2]])
                emit_norm_mod(K, C, xb_[i2], rx_[i2], nb, ty, gsc, sh, r_gs, sq, r_sq, pss[i2], r_pss[i2],
                              rstd[i2], r_rstd[i2], tmp, r_tmp, ub[i2], r_ub[i2])
                K.dma('act', uT_d[:, :, t0:t0 + nb], ub[i2][:, :, 0:nb], [r_ub[i2]], [ures[bi]])
            S.barrier()

        outres = []
        for hdd in heads:
            for mx in mixers:
                with ExitStack() as stm:
                    emit_mixer(K, stm, C, mx, l, hdd['PRM'], hdd['r_prm'], prm_off, hdd['winv'], hdd['rgw_d'],
                               hdd['wlr_d'], uT_d, ures, hdd['ys_dst'], outres)
                    S.barrier()
        S.barrier()


def emit_mixer(K, st, C, mx, l, PRM, r_prm, prm_off, winv, rgw_d, wlr_d, uT_d, ures, ys_d, outres):
    S = K.S
    branch = {'rg': 0, 'gla': 1, 'hg': 2, 'ml': 3}[mx]
    wnames = {'rg': ['rg_x', 'rg_y'],
              'gla': ['gla_q', 'gla_k', 'gla_g', 'gla_lr', 'gla_v'],
              'hg': ['hg_q', 'hg_f0', 'hg_f1', 'hg_g', 'hg_i'],
              'ml': ['ml_q', 'ml_k', 'ml_o', 'ml_v', 'ml_i0', 'ml_f0', 'ml_i1', 'ml_f1']}[mx]
    c_lo = A_OFF[wnames[0]][0]
    c_hi = A_OFF[wnames[-1]][0] + A_OFF[wnames[-1]][1]
    ncol = c_hi - c_lo
    wt = K.sb(st, [128, 8, ncol], BF16, 'w_' + mx)
    r_w = Res()
    for k in range(8):
        K.dma('pool', wt[:, k, :], winv[:, k, c_lo:c_hi], [], [r_w])

    def W(name, k):
        o, w = A_OFF[name]
        return wt[:, k, o - c_lo:o - c_lo + w]

    ubuf = [K.sb(st, [128, 8, 512], BF16, 'ub%d' % i) for i in range(2)]
    r_ubuf = [Res(), Res()]
    uctr = [0]

    def load_u(bi):
        t0, nb = A_BLOCKS[bi]
        i = uctr[0] % 2
        uctr[0] += 1
        K.dma('sp', ubuf[i][:, :, 0:nb], uT_d[:, :, t0:t0 + nb], [ures[bi]], [r_ubuf[i]])
        return ubuf[i], r_ubuf[i]

    pproj = [K.ps(st, [128, 512], F32, 'pproj%d' % i) for i in range(2)]
    r_pproj = [Res(), Res()]
    pctr = [0]

    def proj(u, ru, name, nb, M=None):
        o, w = A_OFF[name]
        M = M or w
        i = pctr[0] % 2
        pctr[0] += 1
        for k in range(8):
            K.mm(pproj[i][0:M, 0:nb], W(name, k)[:, 0:M], u[:, k, 0:nb], k == 0, k == 7, [r_w, ru], [r_pproj[i]])
        return pproj[i][0:M, 0:nb], r_pproj[i]

    def conv_block(raw, rraw, bi, cw, cb, outt, r_out):
        t0, nb = A_BLOCKS[bi]
        s0, s1 = (0, NCTX) if bi == 0 else (NCTX, NT)
        rd = [rraw[j] for j in (bi - 1, bi, bi + 1) if 0 <= j < NBLK]
        K.ts('pool', outt[:, 0:nb], raw[:, t0:t0 + nb], cw[:, 2:3], cb, ALU.mult, ALU.add, rd + [r_prm], [r_out])
        for j in (0, 1, 3):
            o = j - 2
            a = max(t0, s0 - o)
            e = min(t0 + nb, s1 - o)
            K.stt('dve', outt[:, a - t0:e - t0], raw[:, a + o:e + o], cw[:, j:j + 1], outt[:, a - t0:e - t0],
                  ALU.mult, ALU.add, rd + [r_prm, r_out], [r_out])

    def final_alloc():
        sqf = [K.sb(st, [128, 512], BF16, 'sqf%d' % i) for i in range(2)]
        rsf = [K.sb(st, [128, 512], F32, 'rsf%d' % i) for i in range(2)]
        yf = [K.sb(st, [128, 512], F32, 'yf%d' % i) for i in range(2)]
        yb = [K.sb(st, [128, 512], BF16, 'yb%d' % i) for i in range(2)]
        return dict(sqf=sqf, rsf=rsf, yf=yf, yb=yb, r_sqf=[Res(), Res()], r_rsf=[Res(), Res()],
                    r_yf=[Res(), Res()], r_yb=[Res(), Res()], n=[0])

    def final_block(T, bi, o_acc, r_oacc, gate, r_gate, ng_ap, pfin, r_pfin):
        t0, nb = A_BLOCKS[bi]
        i = T['n'][0] % 2
        T['n'][0] += 1
        sqf, rsf, yf, yb = T['sqf'], T['rsf'], T['yf'], T['yb']
        r_sqf, r_rsf, r_yf, r_yb = T['r_sqf'], T['r_rsf'], T['r_yf'], T['r_yb']
        K.act(sqf[i][:, 0:nb], o_acc[:, t0:t0 + nb], AF.Square, [r_oacc[bi]], [r_sqf[i]])
        K.mm(pfin[i][:, 0:nb], C['ones_b'][:], sqf[i][:, 0:nb], True, True, [r_sqf[i], C['res']], [r_pfin[i]])
        K.act(rsf[i][:, 0:nb], pfin[i][:, 0:nb], AF.Sqrt, [r_pfin[i], C['res']], [r_rsf[i]], bias=C['eps'],
              scale=1.0 / 128)
        K.recip(rsf[i][:, 0:nb], rsf[i][:, 0:nb], [r_rsf[i]], [r_rsf[i]])
        K.stt('dve', yf[i][:, 0:nb], o_acc[:, t0:t0 + nb], ng_ap, rsf[i][:, 0:nb], ALU.mult, ALU.mult,
              [r_oacc[bi], r_rsf[i], r_prm], [r_yf[i]])
        K.tt('pool', yb[i][:, 0:nb], yf[i][:, 0:nb], gate[:, t0:t0 + nb], ALU.mult, [r_yf[i], r_gate[bi]],
             [r_yb[i]])
        ro = Res()
        K.dma('act', ys_d(branch, t0, nb), yb[i][:, 0:nb], [r_yb[i]], [ro])
        outres.append(ro)

    if mx == 'rg':
        raw = K.sb(st, [128, NT], F32, 'rg_raw')
        r_raw = [Res() for _ in range(NBLK)]
        xb = K.sb(st, [128, NT], F32, 'rg_xb')
        xbb = K.sb(st, [128, NT], BF16, 'rg_xbb')
        r_xb = [Res() for _ in range(NBLK)]
        gy = K.sb(st, [128, NT], BF16, 'rg_gy')
        r_gy = [Res() for _ in range(NBLK)]
        wg = K.sb(st, [128, 512], BF16, 'rg_wg')
        r_wg = Res()
        K.dma('pool', wg[:], rgw_d, [], [r_wg])
        clam = K.sb(st, [128, 2], F32, 'rg_clam')
        r_clam = Res()
        K.act(clam[:], PRM('rg_lam'), AF.Exp, [r_prm], [r_clam], scale=-1.0)
        K.act(clam[:], clam[:], AF.Ln, [r_clam, C['res']], [r_clam], bias=C['one'])
        K.ts('dve', clam[:], clam[:], -8.0, None, ALU.mult, None, [r_clam], [r_clam])
        t1 = [K.sb(st, [128, 512], F32, 'rg_t1_%d' % i) for i in range(2)]
        r_t1 = [Res(), Res()]
        t2 = [K.sb(st, [128, 512], F32, 'rg_t2_%d' % i) for i in range(2)]
        r_t2 = [Res(), Res()]
        xs = [K.sb(st, [128, 512], F32, 'rg_xs_%d' % i) for i in range(2)]
        r_xs = [Res(), Res()]
        for bi, (t0, nb) in enumerate(A_BLOCKS):
            i = bi % 2
            u, ru = load_u(bi)
            p, rp = proj(u, ru, 'rg_x', nb)
            K.copy('act', raw[:, t0:t0 + nb], p, [rp], [r_raw[bi]])
            p, rp = proj(u, ru, 'rg_y', nb)
            K.act(t1[i][:, 0:nb], p, AF.Square, [rp], [r_t1[i]])
            K.copy('act', xs[i][:, 0:nb], p, [rp], [r_xs[i]])
            K.ts('dve', t1[i][:, 0:nb], t1[i][:, 0:nb], 0.044715, 1.0, ALU.mult, ALU.add, [r_t1[i]], [r_t1[i]])
            K.tt('dve', t2[i][:, 0:nb], t1[i][:, 0:nb], xs[i][:, 0:nb], ALU.mult, [r_t1[i], r_xs[i]], [r_t2[i]])
            K.act(t2[i][:, 0:nb], t2[i][:, 0:nb], AF.Sigmoid, [r_t2[i]], [r_t2[i]], scale=1.5957691216)
            K.tt('dve', gy[:, t0:t0 + nb], xs[i][:, 0:nb], t2[i][:, 0:nb], ALU.mult, [r_xs[i], r_t2[i]],
                 [r_gy[bi]])
        for bi, (t0, nb) in enumerate(A_BLOCKS):
            conv_block(raw, r_raw, bi, PRM('rg_cw'), PRM('rg_cb'), xb[:, t0:t0 + nb], r_xb[bi])
            K.copy('act', xbb[:, t0:t0 + nb], xb[:, t0:t0 + nb], [r_xb[bi]], [r_xb[bi]])
        rr = [K.sb(st, [128, 512], F32, 'rg_r%d' % i) for i in range(2)]
        r_rr = [Res(), Res()]
        ii = [K.sb(st, [128, 512], F32, 'rg_i%d' % i) for i in range(2)]
        r_ii = [Res(), Res()]
        aa = [K.sb(st, [128, 512], F32, 'rg_a%d' % i) for i in range(2)]
        r_aa = [Res(), Res()]
        ss = [K.sb(st, [128, 512], F32, 'rg_s%d' % i) for i in range(2)]
        r_ss = [Res(), Res()]
        hh = [K.sb(st, [128, 512], F32, 'rg_h%d' % i) for i in range(2)]
        r_hh = [Res(), Res()]
        gb = PRM('rg_gb')
        n = 0
        for d in range(2):
            carry = 0.0
            r_carry = []
            for bi in blk_order(d):
                t0, nb = A_BLOCKS[bi]
                i = n % 2
                n += 1
                pr = pproj[pctr[0] % 2]
                rpr = r_pproj[pctr[0] % 2]
                pctr[0] += 1
                K.mm(pr[:, 0:nb], wg[:, (d * 2) * 128:(d * 2 + 1) * 128], xbb[:, t0:t0 + nb], True, True,
                     [r_wg, r_xb[bi]], [rpr])
                K.act(rr[i][:, 0:nb], pr[:, 0:nb], AF.Sigmoid, [rpr, r_prm], [r_rr[i]], bias=gb[:, d * 2:d * 2 + 1])
                pi = pproj[pctr[0] % 2]
                rpi = r_pproj[pctr[0] % 2]
                pctr[0] += 1
                K.mm(pi[:, 0:nb], wg[:, (d * 2 + 1) * 128:(d * 2 + 2) * 128], xbb[:, t0:t0 + nb], True, True,
                     [r_wg, r_xb[bi]], [rpi])
                K.act(ii[i][:, 0:nb], pi[:, 0:nb], AF.Sigmoid, [rpi, r_prm], [r_ii[i]],
                      bias=gb[:, d * 2 + 1:d * 2 + 2])
                K.act(aa[i][:, 0:nb], rr[i][:, 0:nb], AF.Exp, [r_rr[i], r_clam], [r_aa[i]], scale=clam[:, d:d + 1])
                K.tt('dve', ss[i][:, 0:nb], aa[i][:, 0:nb], aa[i][:, 0:nb], ALU.mult, [r_aa[i]], [r_ss[i]])
                K.act(ss[i][:, 0:nb], ss[i][:, 0:nb], AF.Sqrt, [r_ss[i], C['res']], [r_ss[i]], bias=C['one'],
                      scale=-1.0)
                K.tt('dve', ii[i][:, 0:nb], ii[i][:, 0:nb], xb[:, t0:t0 + nb], ALU.mult, [r_ii[i], r_xb[bi]],
                     [r_ii[i]])
                K.tt('dve', ii[i][:, 0:nb], ii[i][:, 0:nb], ss[i][:, 0:nb], ALU.mult, [r_ii[i], r_ss[i]],
                     [r_ii[i]])
                ha, aa_, bt_ = hh[i][:, 0:nb], aa[i][:, 0:nb], ii[i][:, 0:nb]
                if d == 1:
                    ha, aa_, bt_ = rev(ha), rev(aa_), rev(bt_)
                K.scan(ha, aa_, bt_, carry, ALU.mult, ALU.add, [r_aa[i], r_ii[i]] + r_carry, [r_hh[i]])
                carry = hh[i][:, nb - 1:nb] if d == 0 else hh[i][:, 0:1]
                r_carry = [r_hh[i]]
                if d == 0:
                    K.copy('pool', raw[:, t0:t0 + nb], hh[i][:, 0:nb], [r_hh[i]], [r_raw[bi]])
                else:
                    K.tt('pool', raw[:, t0:t0 + nb], raw[:, t0:t0 + nb], hh[i][:, 0:nb], ALU.add,
                         [r_hh[i], r_raw[bi]], [r_raw[bi]])
        yb = [K.sb(st, [128, 512], BF16, 'rg_yb%d' % i) for i in range(2)]
        r_yb = [Res(), Res()]
        for bi, (t0, nb) in enumerate(A_BLOCKS):
            i = bi % 2
            K.tt('dve', yb[i][:, 0:nb], raw[:, t0:t0 + nb], gy[:, t0:t0 + nb], ALU.mult, [r_raw[bi], r_gy[bi]],
                 [r_yb[i]])
            ro = Res()
            K.dma('act', ys_d(branch, t0, nb), yb[i][:, 0:nb], [r_yb[i]], [ro])
            outres.append(ro)
        return

    dk = 64 if mx == 'gla' else 128
    SW = 256 if mx == 'ml' else 128
    vname = {'gla': 'gla_v', 'hg': 'hg_i', 'ml': 'ml_v'}[mx]
    q_bf = K.sb(st, [dk, NT], BF16, mx + '_q')
    r_q = [Res() for _ in range(NBLK)]
    if mx != 'hg':
        k_bf = K.sb(st, [dk, NT], BF16, mx + '_k')
        r_k = [Res() for _ in range(NBLK)]
    v_bf = K.sb(st, [64, NCHUNK, 128], BF16, mx + '_v')
    r_v = [Res() for _ in range(NBLK)]
    gate = K.sb(st, [128, NT], BF16, mx + '_gate')
    r_gate = [Res() for _ in range(NBLK)]
    o_acc = K.sb(st, [128, NT], F32, mx + '_oacc')
    r_oacc = [Res() for _ in range(NBLK)]
    stpv = ExitStack()
    pv = K.ps(stpv, [64, 4, 128], F32, 'pv')
    r_pv = Res()

    def proj_v(u, ru, bi, scale):
        t0, nb = A_BLOCKS[bi]
        o, w = A_OFF[vname]
        for g0 in range(0, nb // 64, 4):
            for j in range(4):
                for k in range(8):
                    K.mm(pv[:, j, :], u[:, k, (g0 + j) * 64:(g0 + j + 1) * 64], W(vname, k), k == 0, k == 7,
                         [ru, r_w], [r_pv])
            c0 = t0 // 64 + g0
            K.act(v_bf[:, c0:c0 + 4, :], pv[:], AF.Copy, [r_pv], [r_v[bi]], scale=scale)

    if mx == 'gla':
        def p_work(bi, u, ru):
            t0, nb = A_BLOCKS[bi]
            p, rp = proj(u, ru, 'gla_q', nb)
            K.act(q_bf[:, t0:t0 + nb], p, AF.Copy, [rp], [r_q[bi]], scale=0.125)
            p, rp = proj(u, ru, 'gla_k', nb)
            K.copy('dve', k_bf[:, t0:t0 + nb], p, [rp], [r_k[bi]])
            p, rp = proj(u, ru, 'gla_g', nb)
            K.act(gate[:, t0:t0 + nb], p, AF.Silu, [rp], [r_gate[bi]])
            proj_v(u, ru, bi, 1.0)
    elif mx == 'hg':
        def p_work(bi, u, ru):
            t0, nb = A_BLOCKS[bi]
            p, rp = proj(u, ru, 'hg_q', nb)
            K.act(q_bf[:, t0:t0 + nb], p, AF.Silu, [rp], [r_q[bi]])
            p, rp = proj(u, ru, 'hg_g', nb)
            K.act(gate[:, t0:t0 + nb], p, AF.Silu, [rp], [r_gate[bi]])
            proj_v(u, ru, bi, 128.0 ** -0.5)
    else:
        stp = ExitStack()
        ctmp = [K.sb(stp, [128, 512], F32, 'ml_ct%d' % i) for i in range(2)]
        r_ct = [Res(), Res()]
        for which in range(2):
            nm = ('ml_q', 'ml_k')[which]
            dst, rdst = ((q_bf, r_q), (k_bf, r_k))[which]
            cw = PRM(('ml_cwq', 'ml_cwk')[which])
            cb = PRM(('ml_cbq', 'ml_cbk')[which])
            for bi, (t0, nb) in enumerate(A_BLOCKS):
                u, ru = load_u(bi)
                p, rp = proj(u, ru, nm, nb)
                K.copy('act', o_acc[:, t0:t0 + nb], p, [rp], [r_oacc[bi]])
                if which == 0:
                    p, rp = proj(u, ru, 'ml_o', nb)
                    K.act(gate[:, t0:t0 + nb], p, AF.Sigmoid, [rp], [r_gate[bi]])
                    proj_v(u, ru, bi, 1.0)
            for bi, (t0, nb) in enumerate(A_BLOCKS):
                i = bi % 2
                conv_block(o_acc, r_oacc, bi, cw, cb, ctmp[i][:, 0:nb], r_ct[i])
                K.act(dst[:, t0:t0 + nb], ctmp[i][:, 0:nb], AF.Silu, [r_ct[i]], [rdst[bi]])
        S.barrier()
        stp.close()

    if mx == 'ml':
        S.barrier()
        stpv.close()
    else:
        fin_t = final_alloc()
    std = ExitStack()
    mask = K.sb(std, [64, 2, 64], F32, 'mask')
    r_mask = Res()
    K.memset('pool', mask[:], 1.0, [r_mask])
    K.asel(mask[:, 0, :], mask[:, 0, :], [[1, 64]], ALU.is_ge, 0.0, 0, -1, [r_mask], [r_mask])
    K.asel(mask[:, 1, :], mask[:, 1, :], [[-1, 64]], ALU.is_ge, 0.0, 0, 1, [r_mask], [r_mask])
    rmask = K.sb(std, [128, 2, 512], F32, 'rmask')
    r_rmask = Res()
    K.memset('pool', rmask[:], 1.0, [r_rmask])
    K.memset('pool', rmask[:, 0, :].rearrange("p (c i) -> p c i", i=64)[:, :, 0:1], 0.0, [r_rmask])
    K.memset('pool', rmask[:, 1, :].rearrange("p (c i) -> p c i", i=64)[:, :, 63:64], 0.0, [r_rmask])
    hmask = K.sb(std, [128, 2, 512], BF16, 'hmask')
    K.memset('pool', hmask[:], 1.0, [r_rmask])
    K.memset('pool', hmask[:, 0, :].rearrange("p (c i) -> p c i", i=64)[:, :, 32:64], 0.0, [r_rmask])
    K.memset('pool', hmask[:, 1, :].rearrange("p (c i) -> p c i", i=64)[:, :, 0:32], 0.0, [r_rmask])

    if mx == 'gla':
        wlr = K.sb(std, [32, 128], BF16, 'wlr')
        r_wlr = Res()
        K.dma('pool', wlr[:], wlr_d, [], [r_wlr])
        nblr = K.sb(std, [64, 2], F32, 'nblr')
        r_nblr = Res()
        K.ts('dve', nblr[:], PRM('gla_blr', 64), -1.0, None, ALU.mult, None, [r_prm], [r_nblr])
        lrs = [K.sb(std, [32, 512], BF16, 'lrs%d' % i) for i in range(2)]
        r_lrs = [Res(), Res()]
    if mx == 'hg':
        lbt = K.sb(std, [128, 4], F32, 'lbt')
        r_lbt = Res()
        if l == 0:
            K.memset('pool', lbt[:, 0:2], 0.0, [r_lbt])
        else:
            K.tt('dve', lbt[:, 0:2], PRM('hg_l1'), PRM('hg_l0'), ALU.subtract, [r_prm], [r_lbt])
            K.act(lbt[:, 0:2], lbt[:, 0:2], AF.Sigmoid, [r_lbt], [r_lbt])
        K.ts('dve', lbt[:, 2:4], lbt[:, 0:2], -1.0, 1.0, ALU.mult, ALU.add, [r_lbt], [r_lbt])
    if mx == 'ml':
        ngb = K.sb(std, [128, 4], F32, 'ngb')
        r_ngb = Res()
        K.ts('dve', ngb[:], PRM('ml_gb'), -1.0, None, ALU.mult, None, [r_prm], [r_ngb])
        carG = K.sb(std, [128, 1], F32, 'carG')
        carM = K.sb(std, [128, 1], F32, 'carM')
        r_car = Res()

    def T2(name, shape, dt=F32):
        return [K.sb(std, shape, dt, '%s_%s%d' % (mx, name, i)) for i in range(2)], [Res(), Res()]

    glog, r_glog = T2('glog', [dk, 512])
    bb, r_bb = T2('b', [dk, 512])
    d1, r_d1 = T2('d1', [dk, 512])
    if mx == 'ml':
        d2, r_d2 = T2('d2', [dk, 512])
        d3, r_d3 = T2('d3', [dk, 512])
    qt, r_qt = T2('qt', [dk, 512], BF16)
    kt, r_kt = T2('kt', [dk, 512], BF16)
    if mx != 'ml':
        ktz, r_ktz = T2('ktz', [dk, 512], BF16)
    e2b, r_e2b = T2('e2b', [dk, 512], BF16)
    e3b, r_e3b = T2('e3b', [dk, 512], BF16)
    ke, r_ke = T2('ke', [dk, 512], BF16)
    keT, r_keT = T2('keT', [64, 8, dk], BF16)
    sm, r_sm = T2('sm', [dk, 32])
    if mx == 'hg':
        kraw, r_kraw = T2('kraw', [dk, 512])
    if mx == 'ml':
        ip, r_ip = T2('ip', [128, 512])
        clampt, r_cl = T2('clamp', [128, 512])
        hht, r_hht = ip, r_ip
    stm, r_stm = T2('stm', [64, 64], BF16)
    Sfl = [K.sb(std, [dk, SW], F32, mx + '_Sf%d' % i) for i in range(2)]
    r_Sfl = [Res(), Res()]
    Sb, r_Sb = T2('Sb', [dk, SW], BF16)
    p_stt = K.ps(std, [128, 512], F32, 'p_st')
    p_st = [p_stt[0:64, 0:64], p_stt[0:64, 0:64]]
    r_pst = [Res()] * 2
    p_o = K.ps(std, [128, 512], F32, 'p_o')
    r_po = Res()
    if mx == 'ml':
        p_den = K.ps(std, [128, 512], F32, 'p_den')
        r_pden = Res()
    p_trt = K.ps(std, [64, 8, 128], BF16, 'p_tr')
    p_tr = p_trt[:, :, 0:dk]
    r_ptr = Res()
    p_dst = [K.ps(std, [128, 512], F32, 'p_ds%d' % i) for i in range(2)]
    p_dsl = [p_dst[0][0:dk, 0:SW], p_dst[1][0:dk, 0:SW]]
    r_pdsl = [Res(), Res()]

    nch_ctr = [0]
    ng = PRM({'gla': 'gla_ng', 'hg': 'hg_ng', 'ml': 'ml_ng'}[mx])

    def gate_phase(d, bi, i, first):
        t0, nb = A_BLOCKS[bi]
        nchk = nb // 64
        if first and mx == 'ml':
            K.memset('dve', carG[:], 0.0, [r_car])
            K.memset('dve', carM[:], 0.0, [r_car])
        u, ru = load_u(bi)
        if d == 0 and mx != 'ml':
            p_work(bi, u, ru)

        def c3(ap):
            return ap.rearrange("p (c i) -> p c i", i=64)
        endi = 63 if d == 0 else 0

        if mx in ('gla', 'hg'):
            if mx == 'gla':
                p, rp = proj(u, ru, 'gla_lr', nb)
                K.copy('act', lrs[i][:, 0:nb], p, [rp], [r_lrs[i]])
                pp = pproj[pctr[0] % 2]
                rpp = r_pproj[pctr[0] % 2]
                pctr[0] += 1
                K.mm(pp[0:64, 0:nb], wlr[:, d * 64:(d + 1) * 64], lrs[i][:, 0:nb], True, True,
                     [r_wlr, r_lrs[i]], [rpp])
                K.act(glog[i][:, 0:nb], pp[0:64, 0:nb], AF.Exp, [rpp, r_nblr], [r_glog[i]],
                      bias=nblr[:, d:d + 1], scale=-1.0)
                K.act(glog[i][:, 0:nb], glog[i][:, 0:nb], AF.Ln, [r_glog[i], C['res']], [r_glog[i]],
                      bias=C['one'][0:64, :])
                ksrc, r_ksrc = k_bf[:, t0:t0 + nb], r_k[bi]
            else:
                p, rp = proj(u, ru, 'hg_f%d' % d, nb)
                K.act(kraw[i][:, 0:nb], p, AF.Sigmoid, [rp], [r_kraw[i]])
                K.ts('dve', kraw[i][:, 0:nb], kraw[i][:, 0:nb], lbt[:, 2 + d:3 + d], lbt[:, d:d + 1],
                     ALU.mult, ALU.add, [r_kraw[i], r_lbt], [r_kraw[i]])
                K.act(glog[i][:, 0:nb], kraw[i][:, 0:nb], AF.Ln, [r_kraw[i]], [r_glog[i]])
                K.ts('pool', kraw[i][:, 0:nb], kraw[i][:, 0:nb], -1.0, 1.0, ALU.mult, ALU.add, [r_kraw[i]],
                     [r_kraw[i]])
                ksrc, r_ksrc = kraw[i][:, 0:nb], r_kraw[i]
            ba, ga, ma = bb[i][:, 0:nb], glog[i][:, 0:nb], rmask[0:dk, d, 0:nb]
            if d == 1:
                ba, ga, ma = rev(ba), rev(ga), rev(ma)
            K.scan(ba, ma, ga, 0.0, ALU.mult, ALU.add, [r_glog[i], r_rmask], [r_bb[i]])
            b3 = c3(bb[i][:, 0:nb])
            bend = b3[:, :, endi]
            href = sm[i][:, 0:nchk]
            midi = 31 if d == 0 else 32
            sc = -1.0 / 16.0 if mx == 'gla' else 1.0
            K.copy('pool', href, b3[:, :, midi], [r_bb[i]], [r_sm[i]])
            K.act(sm[i][:, 8:8 + nchk], bend, AF.Exp, [r_bb[i]], [r_sm[i]], scale=sc)
            K.act(sm[i][:, 16:16 + nchk], b3[:, :, midi], AF.Exp, [r_bb[i]], [r_sm[i]], scale=sc)
            K.tt('dve', c3(d1[i][:, 0:nb]), b3, href.unsqueeze(2).to_broadcast([dk, nchk, 64]), ALU.subtract,
                 [r_bb[i], r_sm[i]], [r_d1[i]])
            K.act(e2b[i][:, 0:nb], d1[i][:, 0:nb], AF.Exp, [r_d1[i]], [r_e2b[i]], scale=sc)
            K.act(e3b[i][:, 0:nb], d1[i][:, 0:nb], AF.Exp, [r_d1[i]], [r_e3b[i]], scale=-sc)
            ratio = sm[i][:, 24:24 + nchk]
            K.tt('pool', ratio, bend, href, ALU.subtract, [r_bb[i], r_sm[i]], [r_sm[i]])
            K.act(ratio, ratio, AF.Exp, [r_sm[i]], [r_sm[i]], scale=sc)
            K.tt('dve', qt[i][:, 0:nb], q_bf[:, t0:t0 + nb], e2b[i][:, 0:nb], ALU.mult, [r_q[bi], r_e2b[i]],
                 [r_qt[i]])
            K.tt('dve', kt[i][:, 0:nb], ksrc, e3b[i][:, 0:nb], ALU.mult, [r_ksrc, r_e3b[i]], [r_kt[i]])
            K.tt('dve', ktz[i][:, 0:nb], kt[i][:, 0:nb], hmask[0:dk, d, 0:nb], ALU.mult, [r_kt[i], r_rmask],
                 [r_ktz[i]])
            K.tt('dve', c3(ke[i][:, 0:nb]), c3(kt[i][:, 0:nb]), ratio.unsqueeze(2).to_broadcast([dk, nchk, 64]),
                 ALU.mult, [r_kt[i], r_sm[i]], [r_ke[i]])
            dec_col = lambda c: sm[i][:, 8 + c:9 + c]
            eref_col = lambda c: sm[i][:, 16 + c:17 + c]
        else:
            p, rp = proj(u, ru, 'ml_i%d' % d, nb)
            K.act(ip[i][:, 0:nb], p, AF.Identity, [rp, r_prm], [r_ip[i]],
                  bias=PRM('ml_gb')[:, d * 2:d * 2 + 1])
            p, rp = proj(u, ru, 'ml_f%d' % d, nb)
            K.act(glog[i][:, 0:nb], p, AF.Exp, [rp, r_ngb], [r_glog[i]], bias=ngb[:, d * 2 + 1:d * 2 + 2],
                  scale=-1.0)
            K.act(glog[i][:, 0:nb], glog[i][:, 0:nb], AF.Ln, [r_glog[i], C['res']], [r_glog[i]],
                  bias=C['one'])
            ba, ga, oa = bb[i][:, 0:nb], glog[i][:, 0:nb], C['ones_f'][:, 0:nb]
            if d == 1:
                ba, ga = rev(ba), rev(ga)
            K.scan(ba, oa, ga, carG[:, 0:1], ALU.mult, ALU.add, [r_glog[i], C['res'], r_car], [r_bb[i]])
            K.tt('dve', d1[i][:, 0:nb], ip[i][:, 0:nb], bb[i][:, 0:nb], ALU.add, [r_ip[i], r_bb[i]],
                 [r_d1[i]])
            ma, aa_ = d2[i][:, 0:nb], d1[i][:, 0:nb]
            if d == 1:
                ma, aa_ = rev(ma), rev(aa_)
            K.scan(ma, aa_, aa_, carM[:, 0:1], ALU.max, ALU.max, [r_d1[i], r_car], [r_d2[i]])
            A3, M3 = c3(d1[i][:, 0:nb]), c3(d2[i][:, 0:nb])
            Rb = sm[i][:, 24:24 + nchk]
            if d == 0:
                K.copy('pool', sm[i][:, 24:25], carM[:, 0:1], [r_car], [r_sm[i]])
                if nchk > 1:
                    K.copy('pool', sm[i][:, 25:24 + nchk], M3[:, 0:nchk - 1, 63], [r_d2[i]], [r_sm[i]])
                Rn = M3[:, :, 63]
                last = nb - 1
            else:
                K.copy('pool', sm[i][:, 24 + nchk - 1:24 + nchk], carM[:, 0:1], [r_car], [r_sm[i]])
                if nchk > 1:
                    K.copy('pool', sm[i][:, 24:24 + nchk - 1], M3[:, 1:nchk, 0], [r_d2[i]], [r_sm[i]])
                Rn = M3[:, :, 0]
                last = 0
            K.tt('dve', clampt[i][:, 0:nb], bb[i][:, 0:nb], d2[i][:, 0:nb], ALU.subtract, [r_bb[i], r_d2[i]],
                 [r_cl[i]])
            K.act(clampt[i][:, 0:nb], clampt[i][:, 0:nb], AF.Exp, [r_cl[i]], [r_cl[i]])
            K.tt('dve', sm[i][:, 8:8 + nchk], Rb, Rn, ALU.subtract, [r_sm[i], r_d2[i]], [r_sm[i]])
            K.act(sm[i][:, 8:8 + nchk], sm[i][:, 8:8 + nchk], AF.Exp, [r_sm[i]], [r_sm[i]])
            K.copy('pool', carG[:, 0:1], bb[i][:, last:last + 1], [r_bb[i]], [r_car])
            K.copy('pool', carM[:, 0:1], d2[i][:, last:last + 1], [r_d2[i]], [r_car])
            Rbb = Rb.unsqueeze(2).to_broadcast([128, nchk, 64])
            K.tt('dve', c3(d3[i][:, 0:nb]), A3, Rbb, ALU.subtract, [r_d1[i], r_sm[i]], [r_d3[i]])
            K.act(e3b[i][:, 0:nb], d3[i][:, 0:nb], AF.Exp, [r_d3[i]], [r_e3b[i]])
            K.tt('dve', kt[i][:, 0:nb], k_bf[:, t0:t0 + nb], e3b[i][:, 0:nb], ALU.mult, [r_k[bi], r_e3b[i]],
                 [r_kt[i]])
            K.tt('dve', c3(glog[i][:, 0:nb]), M3, Rbb, ALU.subtract, [r_d2[i], r_sm[i]], [r_glog[i]])
            K.act(e2b[i][:, 0:nb], glog[i][:, 0:nb], AF.Exp, [r_glog[i], C['res']], [r_e2b[i]],
                  bias=C['lns'], scale=-1.0)
            K.tt('dve', qt[i][:, 0:nb], q_bf[:, t0:t0 + nb], e2b[i][:, 0:nb], ALU.mult, [r_q[bi], r_e2b[i]],
                 [r_qt[i]])
            K.tt('dve', c3(ke[i][:, 0:nb]), c3(kt[i][:, 0:nb]),
                 sm[i][:, 8:8 + nchk].unsqueeze(2).to_broadcast([128, nchk, 64]), ALU.mult, [r_kt[i], r_sm[i]],
                 [r_ke[i]])
            dec_col = lambda c: sm[i][:, 8 + c:9 + c]
            eref_col = None
        for c in range(nchk):
            K.tr(p_tr[:, c, :], ke[i][:, c * 64:(c + 1) * 64], C['ident_b'][0:dk, 0:dk], [r_ke[i], C['res']],
                 [r_ptr])
        K.copy('act', keT[i][:, 0:nchk, :], p_tr[:, 0:nchk, :], [r_ptr], [r_keT[i]])

    def chunk_phase(d, bi, i, first):
        t0, nb = A_BLOCKS[bi]
        nchk = nb // 64
        if first:
            K.memset('dve', Sfl[nch_ctr[0] % 2][:], 0.0, [r_Sfl[nch_ctr[0] % 2]])
        dec_col = lambda c: sm[i][:, 8 + c:9 + c]
        eref_col = (lambda c: sm[i][:, 16 + c:17 + c]) if mx != 'ml' else None
        corder = range(nchk) if d == 0 else range(nchk - 1, -1, -1)
        for c in corder:
            gch = t0 // 64 + c
            j = nch_ctr[0] % 2
            nch_ctr[0] += 1
            Sf, r_Sf = Sfl[j], r_Sfl[j]
            Sn, r_Sn = Sfl[1 - j], r_Sfl[1 - j]
            p_ds, r_pds = p_dsl[j], r_pdsl[j]
            cs = slice(c * 64, (c + 1) * 64)
            if eref_col is not None:
                K.act(Sb[j][:], Sf[:], AF.Copy, [r_Sf, r_sm[i]], [r_Sb[j]], scale=eref_col(c))
            else:
                K.copy('act', Sb[j][:], Sf[:], [r_Sf], [r_Sb[j]])
            if mx == 'ml':
                K.mm(p_st[j], kt[i][:, cs], qt[i][:, cs], True, True, [r_kt[i], r_qt[i]], [r_pst[j]])
            else:
                lo = slice(c * 64, c * 64 + 32)
                hi = slice(c * 64 + 32, c * 64 + 64)
                full_i, full_o, z_i, z_o = (hi, slice(32, 64), lo, slice(0, 32)) if d == 0 else \
                    (lo, slice(0, 32), hi, slice(32, 64))
                K.mm(p_st[j][:, full_o], kt[i][:, cs], qt[i][:, full_i], True, True, [r_kt[i], r_qt[i]],
                     [r_pst[j]])
                K.mm(p_st[j][:, z_o], ktz[i][:, cs], qt[i][:, z_i], True, True, [r_ktz[i], r_qt[i]],
                     [r_pst[j]])
            K.mm(p_ds[:, 0:128], keT[i][:, c, :], v_bf[:, gch, :], True, True, [r_keT[i], r_v[bi]], [r_pds])
            if mx == 'ml':
                K.mm(p_ds[:, 128:256], keT[i][:, c, :], C['ones_b'][0:64, :], True, True,
                     [r_keT[i], C['res']], [r_pds])
            K.tt('dve', stm[j][:], p_st[j], mask[:, d, :], ALU.mult, [r_pst[j], r_mask], [r_stm[j]])
            K.stt('dve', Sn[:], Sf[:], dec_col(c), p_ds, ALU.mult, ALU.add, [r_Sf, r_sm[i], r_pds], [r_Sn])
            K.mm(p_o[:, cs], v_bf[:, gch, :], stm[j][:], True, False, [r_v[bi], r_stm[j]], [r_po])
            K.mm(p_o[:, cs], Sb[j][:, 0:128], qt[i][:, cs], False, True, [r_Sb[j], r_qt[i]], [r_po])
            if mx == 'ml':
                K.mm(p_den[:, cs], C['ones_b'][0:64, :], stm[j][:], True, False, [r_stm[j], C['res']], [r_pden])
                K.mm(p_den[:, cs], Sb[j][:, 128:256], qt[i][:, cs], False, True, [r_Sb[j], r_qt[i]], [r_pden])
        if mx == 'ml':
            K.act(hht[i][:, 0:nb], p_den[:, 0:nb], AF.Abs, [r_pden], [r_hht[i]])
            K.tt('dve', hht[i][:, 0:nb], hht[i][:, 0:nb], clampt[i][:, 0:nb], ALU.max, [r_hht[i], r_cl[i]],
                 [r_hht[i]])
            K.recip(hht[i][:, 0:nb], hht[i][:, 0:nb], [r_hht[i]], [r_hht[i]])
            K.tt('dve', hht[i][:, 0:nb], p_o[:, 0:nb], hht[i][:, 0:nb], ALU.mult, [r_po, r_hht[i]], [r_hht[i]])
            if d == 0:
                K.copy('pool', o_acc[:, t0:t0 + nb], hht[i][:, 0:nb], [r_hht[i]], [r_oacc[bi]])
            else:
                K.tt('pool', o_acc[:, t0:t0 + nb], o_acc[:, t0:t0 + nb], hht[i][:, 0:nb], ALU.add,
                     [r_hht[i], r_oacc[bi]], [r_oacc[bi]])
                Tm = dict(sqf=[e2b[i]] * 2, yb=[e3b[i]] * 2, rsf=[hht[i]] * 2, yf=[clampt[i]] * 2,
                          r_sqf=[r_e2b[i]] * 2, r_yb=[r_e3b[i]] * 2, r_rsf=[r_hht[i]] * 2, r_yf=[r_cl[i]] * 2, n=[0])
                final_block(Tm, bi, o_acc, r_oacc, gate, r_gate, ng, pproj, r_pproj)
        else:
            if d == 0:
                K.copy('act', o_acc[:, t0:t0 + nb], p_o[:, 0:nb], [r_po], [r_oacc[bi]])
            else:
                K.tt('dve', o_acc[:, t0:t0 + nb], p_o[:, 0:nb], o_acc[:, t0:t0 + nb], ALU.add,
                     [r_po, r_oacc[bi]], [r_oacc[bi]])
                final_block(fin_t, bi, o_acc, r_oacc, gate, r_gate, ng, pproj, r_pproj)

    seq = [(d, bi, k == 0) for d in range(2) for k, bi in enumerate(blk_order(d))]
    for s_ in range(len(seq) + 1):
        builders = []
        if s_ < len(seq):
            builders.append(lambda a=seq[s_], i=s_ % 2: gate_phase(a[0], a[1], i, a[2]))
        if s_ >= 1:
            builders.append(lambda a=seq[s_ - 1], i=(s_ - 1) % 2: chunk_phase(a[0], a[1], i, a[2]))
        S.run_streams(builders)
    S.barrier()
    std.close()
    if mx != 'ml':
        S.barrier()
        stpv.close()


def b_blocks(n):
    blks = [(0, 256, 1)]
    t = 256
    while t < n:
        nb = min(512, n - t)
        blks.append((t, nb, 0))
        t += nb
    return blks


def pack_B(inp, l, b, q, last):
    P = Pack()
    ca = inp['c_ctx'] if (not last and q == 0) else inp['c'][b]
    cT = np.stack([colT(inp['c'][b]), colT(ca)], axis=2).reshape(128, 16)
    P.add('cT', cT)
    P.add('adab', colT(inp['ada_b'][l]))
    P.add('gmix', colT(inp['norm_mix_g'][l]))
    P.add('gffn', colT(inp['norm_ffn_g'][l]))
    P.add('bm', colT(inp['b_merge'][l]))
    P.add('gfin', colT(inp['final_norm_g']))
    rb = np.concatenate([inp['moe_b_group'][l], inp['moe_b_expert'][l]])[None, :]
    P.add('rb', np.repeat(rb, 128, axis=0))
    return P


def emit_B_mods(K, st, C, adaw_d, PRM, r_prm):
    S = K.S
    mods = K.sb(st, [128, 6, 8, 2], F32, 'mods')
    r_mods = Res()
    with ExitStack() as st0:
        mod, r_mod = emit_mod(K, st0, C, adaw_d, 6144, PRM('cT'), PRM('adab'), r_prm)
        for dst, src, gname in ((0, 1, 'gmix'), (3, 4, 'gffn')):
            K.ts('dve', mods[:, dst], mod[:, src * 8:(src + 1) * 8, :], 1.0, None, ALU.add, None, [r_mod],
                 [r_mods])
            K.tt('dve', mods[:, dst], mods[:, dst], PRM(gname).unsqueeze(2).to_broadcast([128, 8, 2]),
                 ALU.mult, [r_mods, r_prm], [r_mods])
        for dst, src in ((1, 0), (2, 2), (4, 3), (5, 5)):
            K.copy('dve', mods[:, dst], mod[:, src * 8:(src + 1) * 8, :], [r_mod], [r_mods])
        S.barrier()
    return mods, r_mods


def emit_B(K, C, last, n, blks, mods, r_mods, PRM, r_prm, Wd, h_src, h_reads, ys_src, out_dst, out_res, hmid_d, u2_d):
    S = K.S
    gsc1, sh1, g1, gsc2, sh2, g2 = [mods[:, i] for i in range(6)]
    with ExitStack() as st:
        wtsT = K.sb(st, [16, n], F32, 'wtsT')
        r_wtsT = [Res() for _ in blks]
        r_hmid = [Res() for _ in blks]
        r_u2 = [Res() for _ in blks]

        with ExitStack() as st1:
            wm = K.sb(st1, [128, 8, 4096], BF16, 'wm')
            r_wm = Res()
            wmv = Wd['wm'].rearrange("(k p) n -> p k n", p=128)
            for k in range(8):
                K.dma('pool', wm[:, k, :], wmv[:, k, :], [], [r_wm])
            wbr = K.sb(st1, [128, 16, D], BF16, 'wbr')
            r_wbr = Res()
            wbrv = Wd['wbr'].rearrange("(j p) n -> p j n", p=128)
            for j in range(0, 16, 4):
                K.dma('pool', wbr[:, j:j + 4, :], wbrv[:, j:j + 4, :], [], [r_wbr])
            wo = K.sb(st1, [128, 8, D], BF16, 'wo')
            r_wo = Res()
            wov = Wd['wo'].rearrange("(k p) n -> p k n", p=128)
            for k in range(0, 8, 4):
                K.dma('pool', wo[:, k:k + 4, :], wov[:, k:k + 4, :], [], [r_wo])
            wr = K.sb(st1, [128, 8, 20], F32, 'wr')
            r_wr = Res()
            K.dma('act', wr[:], Wd['wr'].rearrange("(k p) n -> p k n", p=128), [], [r_wr])
            x = K.sb(st1, [128, 8, 512], F32, 'bx')
            r_x = Res()
            ysb = K.sb(st1, [128, 16, 512], BF16, 'bys')
            r_ys = Res()
            rstd = K.sb(st1, [128, 512], F32, 'brstd')
            r_rstd = Res()
            tmp = K.sb(st1, [128, 8, 512], F32, 'btmp')
            r_tmp = Res()
            u = K.sb(st1, [128, 8, 512], BF16, 'bu')
            r_u = Res()
            u2f, r_u2f = tmp, r_tmp
            merged = K.sb(st1, [128, 8, 512], BF16, 'bmerged')
            r_merged = Res()
            sq, r_sq = merged, r_merged
            gt = [K.sb(st1, [128, 512], F32, 'bgt%d' % i) for i in range(2)]
            r_gt = [Res(), Res()]
            mt = [K.sb(st1, [128, 512], F32, 'bmt%d' % i) for i in range(2)]
            r_mt = [Res(), Res()]
            macc = K.sb(st1, [128, 512], F32, 'bmacc')
            r_macc = Res()
            rt = K.sb(st1, [128, 64], F32, 'brt')
            r_rt = Res()
            pss = K.ps(st1, [128, 512], F32, 'bpss')
            r_pss = Res()
            pg = [K.ps(st1, [128, 512], F32, 'bpg%d' % i) for i in range(2)]
            r_pg = [Res(), Res()]
            pb = [K.ps(st1, [128, 512], F32, 'bpb%d' % i) for i in range(2)]
            r_pb = [Res(), Res()]
            pm = K.ps(st1, [128, 512], F32, 'bpm')
            r_pm = Res()
            pr = K.ps(st1, [128, 512], F32, 'bpr')
            r_pr = Res()
            ctr = 0
            for bi, (t0, nb, ty) in enumerate(blks):
                K.dma('sp', x[:, :, 0:nb], h_src(t0, nb), h_reads, [r_x])
                K.dma('act', ysb[:, :, 0:nb], ys_src(t0, nb), [], [r_ys])
                emit_norm_mod(K, C, x, r_x, nb, ty, gsc1, sh1, r_mods, sq, r_sq, pss, r_pss, rstd, r_rstd, tmp, r_tmp,
                              u, r_u)
                for dc in range(8):
                    for k in range(4):
                        i = ctr % 2
                        ctr += 1
                        for kc in range(8):
                            K.mm(pg[i][:, 0:nb], wm[:, kc, k * 1024 + dc * 128:k * 1024 + (dc + 1) * 128],
                                 u[:, kc, 0:nb], kc == 0, kc == 7, [r_wm, r_u], [r_pg[i]])
                        K.act(gt[i][:, 0:nb], pg[i][:, 0:nb], AF.Sigmoid, [r_pg[i], r_prm], [r_gt[i]],
                              bias=PRM('bm')[:, k * 8 + dc:k * 8 + dc + 1])
                        for cc in range(4):
                            K.mm(pb[i][:, 0:nb], wbr[:, k * 4 + cc, dc * 128:(dc + 1) * 128], ysb[:, k * 4 + cc, 0:nb],
                                 cc == 0, cc == 3, [r_wbr, r_ys], [r_pb[i]])
                        if k == 0:
                            K.tt('dve', macc[:, 0:nb], pb[i][:, 0:nb], gt[i][:, 0:nb], ALU.mult, [r_pb[i], r_gt[i]],
                                 [r_macc])
                        else:
                            K.tt('dve', mt[i][:, 0:nb], pb[i][:, 0:nb], gt[i][:, 0:nb], ALU.mult,
                                 [r_pb[i], r_gt[i]], [r_mt[i]])
                            if k < 3:
                                K.tt('pool', macc[:, 0:nb], macc[:, 0:nb], mt[i][:, 0:nb], ALU.add,
                                     [r_macc, r_mt[i]], [r_macc])
                            else:
                                K.tt('pool', merged[:, dc, 0:nb], macc[:, 0:nb], mt[i][:, 0:nb], ALU.add,
                                     [r_macc, r_mt[i]], [r_merged])
                for dc in range(8):
                    for kc in range(8):
                        K.mm(pm[:, 0:nb], wo[:, kc, dc * 128:(dc + 1) * 128], merged[:, kc, 0:nb], kc == 0, kc == 7,
                             [r_wo, r_merged], [r_pm])
                    K.stt('dve', x[:, dc, 0:nb], pm[:, 0:nb], g1[:, dc, ty:ty + 1], x[:, dc, 0:nb], ALU.mult, ALU.add,
                          [r_pm, r_mods, r_x], [r_x])
                K.dma('sp', hmid_d[:, :, t0:t0 + nb], x[:, :, 0:nb], [r_x], [r_hmid[bi]])
                emit_norm_mod(K, C, x, r_x, nb, ty, gsc2, sh2, r_mods, sq, r_sq, pss, r_pss, rstd, r_rstd, tmp, r_tmp,
                              u, r_u, out_f32=u2f, r_of=r_u2f)
                K.dma('act', u2_d[:, :, t0:t0 + nb], u[:, :, 0:nb], [r_u], [r_u2[bi]])
                for s0 in range(0, nb, 128):
                    m = min(128, nb - s0)
                    for k in range(8):
                        K.mm(pr[0:m, 0:20], u2f[:, k, s0:s0 + m], wr[:, k, :], k == 0, k == 7, [r_u2f, r_wr], [r_pr])
                    lg = rt[0:m, 0:20]
                    K.tt('dve', lg, pr[0:m, 0:20], PRM('rb', m), ALU.add, [r_pr, r_prm], [r_rt])
                    gmax, ngmax, gsum = rt[0:m, 20:21], rt[0:m, 21:22], rt[0:m, 22:23]
                    K.S.op('dve', (lambda o, i_: (lambda e: e.tensor_reduce(out=o, in_=i_, axis=mybir.AxisListType.X,
                                                                             op=ALU.max)))(gmax, rt[0:m, 0:4]),
                           [r_rt], [r_rt])
                    K.ts('dve', ngmax, gmax, -1.0, None, ALU.mult, None, [r_rt], [r_rt])
                    ge = rt[0:m, 24:28]
                    K.act(ge, rt[0:m, 0:4], AF.Exp, [r_rt], [r_rt], bias=ngmax)
                    K.S.op('dve', (lambda o, i_: (lambda e: e.tensor_reduce(out=o, in_=i_, axis=mybir.AxisListType.X,
                                                                             op=ALU.add)))(gsum, ge), [r_rt], [r_rt])
                    pgr = rt[0:m, 23:24]
                    K.recip(pgr, gsum, [r_rt], [r_rt])
                    pen = rt[0:m, 28:32]
                    K.ts('dve', pen, rt[0:m, 0:4], gmax, None, ALU.is_equal, None, [r_rt], [r_rt])
                    K.ts('dve', pen, pen, 1e30, -1e30, ALU.mult, ALU.add, [r_rt], [r_rt])
                    em = rt[0:m, 32:48]
                    K.tt('dve', em.rearrange("p (g e) -> p g e", e=4), rt[0:m, 4:20].rearrange("p (g e) -> p g e", e=4),
                         pen.unsqueeze(2).to_broadcast([m, 4, 4]), ALU.add, [r_rt], [r_rt])
                    m1, m2, dd = rt[0:m, 48:49], rt[0:m, 49:50], rt[0:m, 50:51]
                    K.S.op('dve', (lambda o, i_: (lambda e: e.tensor_reduce(out=o, in_=i_, axis=mybir.AxisListType.X,
                                                                             op=ALU.max)))(m1, em), [r_rt], [r_rt])
                    mk1 = rt[0:m, 4:20]
                    K.ts('dve', mk1, em, m1, None, ALU.is_equal, None, [r_rt], [r_rt])
                    K.stt('dve', em, mk1, -1e30, em, ALU.mult, ALU.add, [r_rt], [r_rt])
                    K.S.op('dve', (lambda o, i_: (lambda e: e.tensor_reduce(out=o, in_=i_, axis=mybir.AxisListType.X,
                                                                             op=ALU.max)))(m2, em), [r_rt], [r_rt])
                    K.ts('dve', em, em, m2, None, ALU.is_equal, None, [r_rt], [r_rt])
                    K.tt('dve', dd, m2, m1, ALU.subtract, [r_rt], [r_rt])
                    ee, wa, wb_ = rt[0:m, 51:52], rt[0:m, 52:53], rt[0:m, 53:54]
                    K.act(ee, dd, AF.Exp, [r_rt], [r_rt])
                    K.ts('dve', wa, ee, 1.0, None, ALU.add, None, [r_rt], [r_rt])
                    K.recip(wa, wa, [r_rt], [r_rt])
                    K.tt('dve', wb_, ee, wa, ALU.mult, [r_rt], [r_rt])
                    K.tt('dve', wa, wa, pgr, ALU.mult, [r_rt], [r_rt])
                    K.tt('dve', wb_, wb_, pgr, ALU.mult, [r_rt], [r_rt])
                    K.ts('dve', mk1, mk1, wa, None, ALU.mult, None, [r_rt], [r_rt])
                    K.stt('dve', mk1, em, wb_, mk1, ALU.mult, ALU.add, [r_rt], [r_rt])
                    K.tr(pr[0:16, 128:128 + m], mk1, C['ident_f'][0:m, 0:m], [r_rt, C['res']], [r_pr])
                    K.copy('act', wtsT[:, t0 + s0:t0 + s0 + m], pr[0:16, 128:128 + m], [r_pr], [r_wtsT[bi]])
            S.barrier()

        with ExitStack() as st2:
            h = K.sb(st2, [128, 8, n], F32, 'bh')
            r_h = [Res() for _ in blks]
            u2 = K.sb(st2, [128, 8, n], BF16, 'bu2')
            r_u2s = [Res() for _ in blks]
            for bi, (t0, nb, ty) in enumerate(blks):
                K.dma('sp', h[:, :, t0:t0 + nb], hmid_d[:, :, t0:t0 + nb], [r_hmid[bi]], [r_h[bi]])
                K.dma('act', u2[:, :, t0:t0 + nb], u2_d[:, :, t0:t0 + nb], [r_u2[bi]], [r_u2s[bi]])
            st2o = st2
            st2 = ExitStack()
            sel = K.sb(st2, [16, 16, 128], F32, 'sel')
            r_sel = Res()
            K.copy('dve', sel[:], C['ident_f'][0:16, 0:16].unsqueeze(2).to_broadcast([16, 16, 128]), [C['res']],
                   [r_sel])
            w1b = [K.sb(st2, [128, 8, 512], BF16, 'w1b%d' % i) for i in range(2)]
            w3b = [K.sb(st2, [128, 8, 512], BF16, 'w3b%d' % i) for i in range(2)]
            w2b = [K.sb(st2, [128, 4, D], BF16, 'w2b%d' % i) for i in range(2)]
            r_we = [Res(), Res()]
            wrep = [K.sb(st2, [128, 512], F32, 'wrep%d' % i) for i in range(2)]
            r_wrep = [Res(), Res()]
            sa = [K.sb(st2, [128, 512], F32, 'sa%d' % i) for i in range(2)]
            r_sa = [Res(), Res()]
            hid = [K.sb(st2, [128, 4, 512], BF16, 'hid%d' % i) for i in range(2)]
            r_hid = [Res(), Res()]
            pa = [K.ps(st2, [128, 512], F32, 'mpa%d' % i) for i in range(2)]
            r_pa = [Res(), Res()]
            pb2 = [K.ps(st2, [128, 512], F32, 'mpb%d' % i) for i in range(2)]
            r_pb2 = [Res(), Res()]
            py = [K.ps(st2, [128, 512], F32, 'mpy%d' % i) for i in range(2)]
            r_py = [Res(), Res()]
            pw = K.ps(st2, [128, 512], F32, 'mpw')
            r_pw = Res()
            cc = [0, 0]

            def stage1(e, bi, ib):
                ie = e % 2
                t0, nb, ty = blks[bi]
                if bi == 0:
                    K.dma('pool', w1b[ie][:], Wd['w1'][e].rearrange("(k p) n -> p k n", p=128), [], [r_we[ie]])
                    K.dma('pool', w3b[ie][:], Wd['w3'][e].rearrange("(k p) n -> p k n", p=128), [], [r_we[ie]])
                    K.dma('pool', w2b[ie][:], Wd['w2'][e].rearrange("(k p) n -> p k n", p=128), [], [r_we[ie]])
                K.mm(pw[:, 0:nb], sel[:, e, :], wtsT[:, t0:t0 + nb], True, True, [r_sel, r_wtsT[bi]], [r_pw])
                K.copy('act', wrep[ib][:, 0:nb], pw[:, 0:nb], [r_pw], [r_wrep[ib]])
                for hc in range(4):
                    i = cc[0] % 2
                    cc[0] += 1
                    for k in range(8):
                        K.mm(pa[i][:, 0:nb], w1b[ie][:, k, hc * 128:(hc + 1) * 128], u2[:, k, t0:t0 + nb],
                             k == 0, k == 7, [r_we[ie], r_u2s[bi]], [r_pa[i]])
                    for k in range(8):
                        K.mm(pb2[i][:, 0:nb], w3b[ie][:, k, hc * 128:(hc + 1) * 128], u2[:, k, t0:t0 + nb],
                             k == 0, k == 7, [r_we[ie], r_u2s[bi]], [r_pb2[i]])
                    K.act(sa[i][:, 0:nb], pa[i][:, 0:nb], AF.Silu, [r_pa[i]], [r_sa[i]])
                    K.tt('dve', sa[i][:, 0:nb], pb2[i][:, 0:nb], sa[i][:, 0:nb], ALU.mult, [r_pb2[i], r_sa[i]],
                         [r_sa[i]])
                    K.tt('pool' if hc % 2 else 'dve', hid[ib][:, hc, 0:nb], sa[i][:, 0:nb], wrep[ib][:, 0:nb],
                         ALU.mult, [r_sa[i], r_wrep[ib]], [r_hid[ib]])

            def stage2(e, bi, ib):
                ie = e % 2
                t0, nb, ty = blks[bi]
                for dc in range(8):
                    i = cc[1] % 2
                    cc[1] += 1
                    for hc in range(4):
                        K.mm(py[i][:, 0:nb], w2b[ie][:, hc, dc * 128:(dc + 1) * 128], hid[ib][:, hc, 0:nb],
                             hc == 0, hc == 3, [r_we[ie], r_hid[ib]], [r_py[i]])
                    K.stt('dve', h[:, dc, t0:t0 + nb], py[i][:, 0:nb], g2[:, dc, ty:ty + 1], h[:, dc, t0:t0 + nb],
                          ALU.mult, ALU.add, [r_py[i], r_mods, r_h[bi]], [r_h[bi]])

            items = [(e, bi) for e in range(16) for bi in range(len(blks))]
            for s_ in range(len(items) + 1):
                builders = []
                if s_ < len(items):
                    builders.append(lambda a=items[s_], ib=s_ % 2: stage1(a[0], a[1], ib))
                if s_ >= 1:
                    builders.append(lambda a=items[s_ - 1], ib=(s_ - 1) % 2: stage2(a[0], a[1], ib))
                S.run_streams(builders)
            S.barrier()
            st2.close()
            st2 = st2o
            if not last:
                for bi, (t0, nb, ty) in enumerate(blks):
                    K.dma('sp', out_dst(t0, nb), h[:, :, t0:t0 + nb], [r_h[bi]], [out_res])
            else:
                S.barrier()
                gz = K.sb(st2, [128, 2, 8, 1], F32, 'gz')
                r_gz = Res()
                K.copy('dve', gz[:, 0, :, 0], PRM('gfin'), [r_prm], [r_gz])
                K.memset('dve', gz[:, 1], 0.0, [r_gz])
                sq = K.sb(st2, [128, 8, 512], BF16, 'fsq')
                r_sq = Res()
                rstd = K.sb(st2, [128, 512], F32, 'frstd')
                r_rstd = Res()
                tmp = K.sb(st2, [128, 8, 512], F32, 'ftmp')
                r_tmp = Res()
                of = [K.sb(st2, [128, 8, 512], F32, 'fo%d' % i) for i in range(2)]
                r_of = [Res(), Res()]
                pw = K.ps(st2, [128, 512], F32, 'fpss')
                r_pw = Res()
                for bi, (t0, nb, ty) in enumerate(blks):
                    i = bi % 2
                    emit_norm_mod(K, C, h[:, :, t0:t0 + nb], r_h[bi], nb, 0, gz[:, 0], gz[:, 1], r_gz, sq, r_sq,
                                  pw, r_pw, rstd, r_rstd, tmp, r_tmp, of[i], r_of[i])
                    K.dma('sp', out_dst(t0, nb), of[i][:, :, 0:nb], [r_of[i]], [out_res])
            S.barrier()
        S.barrier()


NQ0 = NT // 4
NQ1 = SEQ // 4


def pack_Bf(inp, l, b):
    P = Pack()
    cT = np.stack([colT(inp['c'][b]), colT(inp['c_ctx'])], axis=2).reshape(128, 16)
    P.add('cT', cT)
    P.add('adab', colT(inp['ada_b'][l]))
    P.add('gmix', colT(inp['norm_mix_g'][l]))
    P.add('gffn', colT(inp['norm_ffn_g'][l]))
    P.add('bm', colT(inp['b_merge'][l]))
    P.add('gfin', colT(inp['final_norm_g']))
    rb = np.concatenate([inp['moe_b_group'][l], inp['moe_b_expert'][l]])[None, :]
    P.add('rb', np.repeat(rb, 128, axis=0))
    return P


def build_fused(offA, offB):
    nc = bass.Bass("TRN2", target_bir_lowering=False)
    wA, wB = offA['_w'], offB['_w']
    hT_d = nc.dram_tensor("hT", [D, NT], F32, kind="ExternalInput").ap()
    IN = []
    for l in range(2):
        d = {}
        d['adaw'] = nc.dram_tensor("adaw%d" % l, [D, 6144], F32, kind="ExternalInput").ap()
        d['win'] = nc.dram_tensor("win%d" % l, [4 * D, A_NCOL], F32, kind="ExternalInput").ap()
        d['prmA'] = nc.dram_tensor("prmA%d" % l, [4 * 128, wA], F32, kind="ExternalInput").ap()
        d['rgw'] = nc.dram_tensor("rgw%d" % l, [4 * 128, 512], F32, kind="ExternalInput").ap()
        d['wlr'] = nc.dram_tensor("wlr%d" % l, [4 * 32, 128], F32, kind="ExternalInput").ap()
        d['prmB'] = nc.dram_tensor("prmB%d" % l, [128, wB], F32, kind="ExternalInput").ap()
        d['wm'] = nc.dram_tensor("wm%d" % l, [D, 4096], F32, kind="ExternalInput").ap()
        d['wbr'] = nc.dram_tensor("wbr%d" % l, [2048, D], F32, kind="ExternalInput").ap()
        d['wo'] = nc.dram_tensor("wo%d" % l, [D, D], F32, kind="ExternalInput").ap()
        d['wr'] = nc.dram_tensor("wr%d" % l, [D, 20], F32, kind="ExternalInput").ap()
        d['w1'] = nc.dram_tensor("w1_%d" % l, [16, D, 512], F32, kind="ExternalInput").ap()
        d['w3'] = nc.dram_tensor("w3_%d" % l, [16, D, 512], F32, kind="ExternalInput").ap()
        d['w2'] = nc.dram_tensor("w2_%d" % l, [16, 512, D], F32, kind="ExternalInput").ap()
        IN.append(d)
    out_d = nc.dram_tensor("outT", [D, NQ1], F32, kind="ExternalOutput").ap()
    uT_d = nc.dram_tensor("uT_scr", [128, 8, NT], BF16).ap()
    ys_scr = [nc.dram_tensor("ys_scr%d" % l, [4, 4, 128, NT], BF16).ap() for l in range(2)]
    h1_d = nc.dram_tensor("h1_scr", [128, 8, NT], F32).ap()
    hmid_d = nc.dram_tensor("hmid_scr", [128, 8, NQ0], F32).ap()
    u2_d = nc.dram_tensor("u2_scr", [128, 8, NQ0], BF16).ap()
    hTv = hT_d.rearrange("(k p) t -> p k t", p=128)
    outv = out_d.rearrange("(k p) t -> p k t", p=128)

    with ExitStack() as st:
        K = KB(nc, st)
        S = K.S
        C = emit_consts(K, st)
        for l in range(2):
            d = IN[l]
            with ExitStack() as stl:
                prmA = K.sb(stl, [128, 4, wA], F32, 'prmA')
                r_prmA = Res()
                K.dma('sp', prmA[:], d['prmA'].rearrange("(h p) w -> p h w", p=128), [], [r_prmA])
                heads = []
                for hd in range(4):
                    def PRMh(name, rows=128, hd=hd):
                        o, w = offA[name]
                        return prmA[0:rows, hd, o:o + w]

                    def ys_dst(branch, t0, nb, hd=hd, l=l):
                        return ys_scr[l][branch, hd, :, t0:t0 + nb]
                    heads.append(dict(PRM=PRMh, r_prm=r_prmA,
                                      winv=d['win'][hd * D:(hd + 1) * D, :].rearrange("(k p) n -> p k n", p=128),
                                      rgw_d=d['rgw'][hd * 128:(hd + 1) * 128, :],
                                      wlr_d=d['wlr'][hd * 32:(hd + 1) * 32, :], ys_dst=ys_dst))
                emit_A(K, C, l, hTv if l == 0 else h1_d, d['adaw'][:, 0:2048], heads, offA, uT_d)
                S.barrier()
            with ExitStack() as stl:
                prmB = K.sb(stl, [128, wB], F32, 'prmB')
                r_prmB = Res()
                K.dma('sp', prmB[:], d['prmB'], [], [r_prmB])

                def PRMB(name, rows=128):
                    o, w = offB[name]
                    return prmB[0:rows, o:o + w]
                mods, r_mods = emit_B_mods(K, stl, C, d['adaw'], PRMB, r_prmB)
                ysv = ys_scr[l].rearrange("k c p t -> p (k c) t")
                if l == 0:
                    for j in range(4):
                        base = j * NQ0
                        if j == 0:
                            blks = [(0, 256, 1), (256, 512, 0), (768, 512, 0), (1280, 512, 0), (1792, 320, 0)]
                        else:
                            blks = [(0, 512, 0), (512, 512, 0), (1024, 512, 0), (1536, 512, 0), (2048, 64, 0)]
                        emit_B(K, C, False, NQ0, blks, mods, r_mods, PRMB, r_prmB, d,
                               (lambda t0, nb, base=base: hTv[:, :, base + t0:base + t0 + nb]), [],
                               (lambda t0, nb, base=base: ysv[:, :, base + t0:base + t0 + nb]),
                               (lambda t0, nb, base=base: h1_d[:, :, base + t0:base + t0 + nb]), Res(),
                               hmid_d, u2_d)
                else:
                    blks = [(i * 512, 512, 0) for i in range(4)]

                    def dyn(ap3):
                        def f(t0, nb):
                            return lambda e: ap3[:, :, bass.ds((e.partition_id() % 4) * NQ1 + (NCTX + t0), nb)]
                        return f
                    emit_B(K, C, True, NQ1, blks, mods, r_mods, PRMB, r_prmB, d,
                           dyn(h1_d), [], dyn(ysv), (lambda t0, nb: outv[:, :, t0:t0 + nb]), Res(),
                           hmid_d, u2_d)
                S.barrier()
        S.barrier()
        S.replay()
    return nc


_NC_CACHE = {}


def kernel(**inputs):
    inp = {k: np.asarray(v) for k, v in inputs.items()}
    in_maps = []
    offA = offB = None
    shared = {}
    for l in range(2):
        shared['adaw%d' % l] = inp['ada_w'][l]
        shared['win%d' % l] = np.ascontiguousarray(
            np.concatenate([gather_w_in(inp['w_in'][l], hd) for hd in range(4)], axis=0))
        shared['rgw%d' % l] = np.ascontiguousarray(
            np.concatenate([rg_gate_blockdiag(inp, l, hd) for hd in range(4)], axis=0))
        shared['wlr%d' % l] = np.ascontiguousarray(
            np.concatenate([gla_wlr_pad(inp, l, hd) for hd in range(4)], axis=0))
        shared['wm%d' % l] = inp['w_merge'][l]
        shared['wbr%d' % l] = np.ascontiguousarray(inp['w_branch'][l].reshape(2048, D))
        shared['wo%d' % l] = inp['w_out'][l]
        shared['wr%d' % l] = np.ascontiguousarray(
            np.concatenate([inp['moe_w_group'][l], inp['moe_w_expert'][l]], axis=1))
        shared['w1_%d' % l] = inp['moe_w1'][l]
        shared['w3_%d' % l] = inp['moe_w3'][l]
        shared['w2_%d' % l] = inp['moe_w2'][l]
    per_b = []
    for b in range(2):
        m = {'hT': np.ascontiguousarray(np.concatenate([inp['ctx'][b], inp['x'][b]], axis=0).T)}
        for l in range(2):
            packs = [pack_A(inp, l, b, hd) for hd in range(4)]
            offA = dict(packs[0].off)
            offA['_w'] = packs[0].w
            m['prmA%d' % l] = np.ascontiguousarray(np.concatenate([p.build() for p in packs], axis=0))
            PB = pack_Bf(inp, l, b)
            offB = dict(PB.off)
            offB['_w'] = PB.w
            m['prmB%d' % l] = PB.build()
        per_b.append(m)
    for core in range(8):
        m = dict(shared)
        m.update(per_b[core // 4])
        in_maps.append(m)
    if 'fused' not in _NC_CACHE:
        _NC_CACHE['fused'] = build_fused(offA, offB)
    res = run_bass_kernel_spmd(_NC_CACHE['fused'], in_maps, core_ids=list(range(8)))
    out = np.zeros((2, SEQ, D), np.float32)
    for c in range(8):
        b, q = c // 4, c % 4
        out[b, q * NQ1:(q + 1) * NQ1, :] = np.asarray(res.results[c]['outT']).T
    return out
```

```python
from contextlib import ExitStack
import numpy as np
import concourse.bass as bass
import concourse.mybir as mybir
from concourse.bass_utils import run_bass_kernel_spmd

F32 = mybir.dt.float32
BF16 = mybir.dt.bfloat16
ALU = mybir.AluOpType
AF = mybir.ActivationFunctionType

ENGS = ['pe', 'act', 'dve', 'pool', 'sp']
NDMASEM = 4

D = 1024
NCTX = 256
SEQ = 8192
NT = NCTX + SEQ
EPS = 1e-6
CH = 64
NCHUNK = NT // CH


class Res:
    __slots__ = ('name', 'w', 'r')

    def __init__(self, name=None):
        self.name = name
        self.w = None
        self.r = {}


MAX_EPOCH = 4
EMBED_WAIT = 1
SEM_SWITCH = 28000


class Sched:
    def __init__(self, nc, stack):
        self.nc = nc
        self.stack = stack
        self.epoch = 0
        self.tot = {}
        self.cur = None
        self.sim_tw = {}
        self.sim_tr = {}
        self.sim_free = {}
        self._new_sems()
        self.prog = {e: [] for e in ENGS}
        self.ninst = 0

    def _new_sems(self):
        nc, stack, ep = self.nc, self.stack, self.epoch
        self.sem = {e: stack.enter_context(nc.semaphore('sm%d_%s' % (ep, e))) for e in ENGS}
        self.cnt = {e: 0 for e in ENGS}
        self.dsem = {e: [stack.enter_context(nc.semaphore('dq%d_%s%d' % (ep, e, i))) for i in range(NDMASEM)]
                     for e in ('sp', 'act', 'pool')}
        self.dcnt = {e: 0 for e in ('sp', 'act', 'pool')}
        self.seen = {e: {} for e in ENGS}
        self.hist = {}
        self.hq = []
        self.gseq = 0

    def _semof(self, key, count):
        if isinstance(key, str):
            return self.sem[key], count
        q, slot = key
        return self.dsem[q][slot], 16 * count

    def _deps(self, eng, reads, writes):
        need = {}
        ep = self.epoch

        def add(key, count, epoch):
            if epoch != ep:
                return
            if key == 'pe' and eng == 'pe':
                return
            if need.get(key, 0) < count:
                need[key] = count
        for r in reads:
            if r.w is not None:
                add(*r.w)
        for w in writes:
            if w.w is not None:
                add(*w.w)
            for k, (c, e_) in w.r.items():
                add(k, c, e_)
        out = []
        clock = self.seen[eng]
        hist = self.hist
        items = sorted(need.items(), key=lambda kc: -hist.get(kc, (0, None))[0])
        for key, count in items:
            if clock.get(key, 0) >= count:
                continue
            out.append(self._semof(key, count))
            h = hist.get((key, count))
            if h is not None and h[1] is not None:
                for k2, c2 in h[1].items():
                    if clock.get(k2, 0) < c2:
                        clock[k2] = c2
            clock[key] = count
        return out

    def _record(self, eng, key, count):
        self.gseq += 1
        snap = dict(self.seen[eng])
        self.hist[(key, count)] = (self.gseq, snap)
        self.hq.append((key, count))
        if len(self.hq) > 6000:
            old = self.hq.pop(0)
            self.hist.pop(old, None)

    def _emit(self, eng, waits, fn, sem, inc):
        def run(e, waits=waits, fn=fn, sem=sem, inc=inc):
            ne = min(len(waits), EMBED_WAIT)
            for s, v in waits[:len(waits) - ne]:
                e.wait_ge(s, v)
            ins = fn(e)
            for s, v in waits[len(waits) - ne:]:
                ins._wait_ge(s, v)
            ins.then_inc(sem, inc)
        self.prog[eng].append(run)
        self.ninst += 1

    def _sim_start(self, eng, reads, writes, isdma):
        t = 0.0
        tw, tr = self.sim_tw, self.sim_tr
        for r in reads:
            v = tw.get(id(r))
            if v is not None and v[0] > t and not (v[1] == 'pe' and eng == 'pe'):
                t = v[0]
        for w in writes:
            v = tw.get(id(w))
            if v is not None and v[0] > t and not (v[1] == 'pe' and eng == 'pe'):
                t = v[0]
            v = tr.get(id(w))
            if v is not None and v > t:
                t = v
        t += 0.15
        key = ('q', eng) if isdma else eng
        return max(t, self.sim_free.get(key, 0.0))

    def _sim_commit(self, eng, reads, writes, isdma, cost):
        st = self._sim_start(eng, reads, writes, isdma)
        key = ('q', eng) if isdma else eng
        if isdma:
            self.sim_free[key] = st + 0.1
            fin = st + (cost or 3.0)
        else:
            fin = st + (cost or 0.5)
            self.sim_free[key] = fin
        for r in reads:
            if self.sim_tr.get(id(r), 0.0) < fin:
                self.sim_tr[id(r)] = fin
        for w in writes:
            self.sim_tw[id(w)] = (fin, eng)
            self.sim_tr.pop(id(w), None)

    def run_streams(self, builders):
        assert self.cur is None
        lists = []
        for b in builders:
            self.cur = []
            b()
            lists.append(self.cur)
        self.cur = None
        pos = [0] * len(lists)
        while True:
            best, bt = None, None
            for k, L in enumerate(lists):
                if pos[k] < len(L):
                    kind, a, cost = L[pos[k]]
                    t = self._sim_start(a[0], a[2], a[3], kind == 'dma')
                    if bt is None or t < bt:
                        best, bt = k, t
            if best is None:
                break
            kind, a, cost = lists[best][pos[best]]
            pos[best] += 1
            (self.op if kind == 'op' else self.dma)(*a, cost=cost)

    def op(self, eng, fn, reads=(), writes=(), cost=None):
        if self.cur is not None:
            self.cur.append(('op', (eng, fn, tuple(reads), tuple(writes)), cost))
            return
        self._sim_commit(eng, reads, writes, False, cost)
        waits = self._deps(eng, reads, writes)
        self.cnt[eng] += 1
        c = self.cnt[eng]
        ep = self.epoch
        self._emit(eng, waits, fn, self.sem[eng], 1)
        self._record(eng, eng, c)
        for r in reads:
            old = r.r.get(eng)
            if old is None or old[1] != ep or old[0] < c:
                r.r[eng] = (c, ep)
        for w in writes:
            w.w = (eng, c, ep)
            w.r = {}

    def dma(self, q, fn, reads=(), writes=(), cost=None):
        if self.cur is not None:
            self.cur.append(('dma', (q, fn, tuple(reads), tuple(writes)), cost))
            return
        self._sim_commit(q, reads, writes, True, cost)
        i = self.dcnt[q]
        self.dcnt[q] += 1
        slot = i % NDMASEM
        count = i // NDMASEM + 1
        key = (q, slot)
        ep = self.epoch
        waits = self._deps(q, reads, writes)
        if count > 1 and self.seen[q].get(key, 0) < count - 1:
            self.seen[q][key] = count - 1
            waits.append(self._semof(key, count - 1))
        self._emit(q, waits, fn, self.dsem[q][slot], 16)
        self._record(q, key, count)
        for r in reads:
            old = r.r.get(key)
            if old is None or old[1] != ep or old[0] < count:
                r.r[key] = (count, ep)
        for w in writes:
            w.w = (key, count, ep)
            w.r = {}

    def barrier(self):
        assert self.cur is None
        keys = [(e, self.cnt[e]) for e in ENGS if self.cnt[e] > 0]
        for q in ('sp', 'act', 'pool'):
            n = self.dcnt[q]
            for slot in range(NDMASEM):
                if n > slot:
                    keys.append(((q, slot), (n - 1 - slot) // NDMASEM + 1))
        for eng in ENGS:
            waits = []
            seen = self.seen[eng]
            for key, count in keys:
                if key == eng or seen.get(key, 0) >= count:
                    continue
                seen[key] = count
                waits.append(self._semof(key, count))

            def run(e, waits=waits):
                for s, v in waits:
                    e.wait_ge(s, v)
            self.prog[eng].append(run)
        if max(list(self.cnt.values()) + [v // NDMASEM for v in self.dcnt.values()]) > SEM_SWITCH \
                and self.epoch < MAX_EPOCH:
            self.tot = {e: self.tot.get(e, 0) + self.cnt[e] for e in ENGS}
            self.epoch += 1
            self._new_sems()

    def replay(self):
        nc = self.nc
        with nc.Block() as block:
            def mk(name):
                def f(e):
                    for run in self.prog[name]:
                        run(e)
                return f
            block.tensor(mk('pe'))
            block.scalar(mk('act'))
            block.vector(mk('dve'))
            block.gpsimd(mk('pool'))
            block.sync(mk('sp'))


class KB:
    def __init__(self, nc, st):
        self.nc = nc
        self.st = st
        self.S = Sched(nc, st)
        self.n = 0

    def sb(self, st, shape, dt=F32, name=None):
        self.n += 1
        return st.enter_context(self.nc.sbuf_tensor('%s_s%d' % (name or 't', self.n), list(shape), dt))

    def ps(self, st, shape, dt=F32, name=None):
        self.n += 1
        return st.enter_context(self.nc.psum_tensor('%s_p%d' % (name or 'p', self.n), list(shape), dt))

    @staticmethod
    def ecost(eng, out):
        n = int(np.prod(out.shape[1:]))
        return (0.13 + n / 900.0) if eng == 'dve' else (0.2 + n / 430.0)

    def mm(self, out, lhsT, rhs, start, stop, reads, writes):
        self.S.op('pe', lambda e: e.matmul(out, lhsT=lhsT, rhs=rhs, start=start, stop=stop), reads, writes,
                  cost=0.065 + int(np.prod(out.shape[1:])) / 2400.0)

    def tr(self, out, in_, ident, reads, writes):
        self.S.op('pe', lambda e: e.transpose(out=out, in_=in_, identity=ident), reads, writes, cost=0.12)

    def act(self, out, in_, func, reads, writes, bias=None, scale=None):
        kw = {}
        if bias is not None:
            kw['bias'] = bias
        if scale is not None:
            kw['scale'] = scale
        self.S.op('act', lambda e: e.activation(out=out, in_=in_, func=func, **kw), reads, writes,
                  cost=0.2 + int(np.prod(out.shape[1:])) / 1100.0)

    def tt(self, eng, out, in0, in1, op, reads, writes):
        self.S.op(eng, lambda e: e.tensor_tensor(out=out, in0=in0, in1=in1, op=op), reads, writes,
                  cost=self.ecost(eng, out))

    def ts(self, eng, out, in0, s1, s2, op0, op1, reads, writes):
        if s2 is None:
            self.S.op(eng, lambda e: e.tensor_scalar(out=out, in0=in0, scalar1=s1, scalar2=None, op0=op0),
                      reads, writes, cost=self.ecost(eng, out))
        else:
            self.S.op(eng, lambda e: e.tensor_scalar(out=out, in0=in0, scalar1=s1, scalar2=s2, op0=op0, op1=op1),
                      reads, writes, cost=self.ecost(eng, out))

    def stt(self, eng, out, in0, scalar, in1, op0, op1, reads, writes):
        self.S.op(eng, lambda e: e.scalar_tensor_tensor(out=out, in0=in0, scalar=scalar, in1=in1, op0=op0, op1=op1),
                  reads, writes, cost=self.ecost(eng, out))

    def copy(self, eng, out, in_, reads, writes):
        if eng == 'act':
            self.act(out, in_, AF.Copy, reads, writes)
        else:
            self.S.op(eng, lambda e: e.tensor_copy(out=out, in_=in_), reads, writes, cost=self.ecost(eng, out))

    def scan(self, out, d0, d1, init, op0, op1, reads, writes):
        self.S.op('dve', lambda e: e.tensor_tensor_scan(out=out, data0=d0, data1=d1, initial=init, op0=op0, op1=op1),
                  reads, writes, cost=self.ecost('dve', out))

    def recip(self, out, in_, reads, writes):
        self.S.op('dve', lambda e: e.reciprocal(out=out, in_=in_), reads, writes, cost=self.ecost('dve', out))

    def memset(self, eng, ap, val, writes):
        self.S.op(eng, lambda e: e.memset(ap, val), (), writes)

    def asel(self, out, in_, pattern, cmp, fill, base, cm, reads, writes):
        self.S.op('pool', lambda e: e.affine_select(out=out, in_=in_, pattern=pattern, compare_op=cmp, fill=fill,
                                                    base=base, channel_multiplier=cm), reads, writes)

    def dma(self, q, out, in_, reads, writes):
        def fn(e):
            o = out(e) if callable(out) else out
            i = in_(e) if callable(in_) else in_
            return e.dma_start(out=o, in_=i)
        self.S.dma(q, fn, reads, writes)


def rev(ap):
    (ps_, pn), (st_, n) = ap.ap
    return bass.AP(ap.tensor, ap.offset + (n - 1) * st_, [[ps_, pn], [-st_, n]])


def colT(v):
    v = np.asarray(v, np.float32)
    return np.ascontiguousarray(v.reshape(-1, 128).T)


class Pack:
    def __init__(self):
        self.items = []
        self.off = {}
        self.w = 0

    def add(self, name, arr):
        arr = np.asarray(arr, np.float32)
        if arr.ndim == 1:
            arr = arr[:, None]
        assert arr.shape[0] <= 128
        if arr.shape[0] < 128:
            arr = np.concatenate([arr, np.zeros((128 - arr.shape[0], arr.shape[1]), np.float32)], 0)
        self.off[name] = (self.w, arr.shape[1])
        self.items.append(arr)
        self.w += arr.shape[1]

    def build(self):
        return np.ascontiguousarray(np.concatenate(self.items, axis=1))


IN_OFF = {}
_o = 0
for _n, _w in (('rg_x', 512), ('rg_y', 512), ('gla_q', 256), ('gla_k', 256), ('gla_v', 512), ('gla_g', 512),
               ('gla_lr', 32), ('hg_q', 512), ('hg_i', 512), ('hg_f', 1024), ('hg_g', 512), ('ml_q', 512),
               ('ml_k', 512), ('ml_v', 512), ('ml_o', 512), ('ml_if', 16)):
    IN_OFF[_n] = _o
    _o += _w
assert _o == 7216

A_COLS = [('rg_x', 128), ('rg_y', 128),
          ('gla_q', 64), ('gla_k', 64), ('gla_g', 128), ('gla_lr', 32), ('gla_v', 128),
          ('hg_q', 128), ('hg_f0', 128), ('hg_f1', 128), ('hg_g', 128), ('hg_i', 128),
          ('ml_q', 128), ('ml_k', 128), ('ml_o', 128), ('ml_v', 128),
          ('ml_i0', 128), ('ml_f0', 128), ('ml_i1', 128), ('ml_f1', 128)]
A_OFF = {}
_o = 0
for _n, _w in A_COLS:
    A_OFF[_n] = (_o, _w)
    _o += _w
A_NCOL = _o


def gather_w_in(w_in_l, hd):
    cols = {}
    o = IN_OFF
    cols['rg_x'] = w_in_l[:, o['rg_x'] + hd * 128: o['rg_x'] + (hd + 1) * 128]
    cols['rg_y'] = w_in_l[:, o['rg_y'] + hd * 128: o['rg_y'] + (hd + 1) * 128]
    cols['gla_q'] = w_in_l[:, o['gla_q'] + hd * 64: o['gla_q'] + (hd + 1) * 64]
    cols['gla_k'] = w_in_l[:, o['gla_k'] + hd * 64: o['gla_k'] + (hd + 1) * 64]
    cols['gla_v'] = w_in_l[:, o['gla_v'] + hd * 128: o['gla_v'] + (hd + 1) * 128]
    cols['gla_g'] = w_in_l[:, o['gla_g'] + hd * 128: o['gla_g'] + (hd + 1) * 128]
    cols['gla_lr'] = w_in_l[:, o['gla_lr']: o['gla_lr'] + 32]
    cols['hg_q'] = w_in_l[:, o['hg_q'] + hd * 128: o['hg_q'] + (hd + 1) * 128]
    cols['hg_i'] = w_in_l[:, o['hg_i'] + hd * 128: o['hg_i'] + (hd + 1) * 128]
    for d in range(2):
        cols['hg_f%d' % d] = w_in_l[:, o['hg_f'] + d * 512 + hd * 128: o['hg_f'] + d * 512 + (hd + 1) * 128]
    cols['hg_g'] = w_in_l[:, o['hg_g'] + hd * 128: o['hg_g'] + (hd + 1) * 128]
    for nm in ('ml_q', 'ml_k', 'ml_v', 'ml_o'):
        cols[nm] = w_in_l[:, o[nm] + hd * 128: o[nm] + (hd + 1) * 128]
    for d in range(2):
        for g, gn in enumerate(('i', 'f')):
            c = o['ml_if'] + d * 8 + g * 4 + hd
            cols['ml_%s%d' % (gn, d)] = np.repeat(w_in_l[:, c:c + 1], 128, axis=1)
    return np.ascontiguousarray(np.concatenate([cols[n] for n, _ in A_COLS], axis=1))


A_BLOCKS = [(0, NCTX)] + [(NCTX + i * 512, 512) for i in range(SEQ // 512)]
NBLK = len(A_BLOCKS)


def blk_order(d):
    return list(range(NBLK)) if d == 0 else [0] + list(range(NBLK - 1, 0, -1))


def emit_consts(K, st):
    C = {}
    r = Res()
    C['res'] = r
    ones_f = K.sb(st, [128, 512], F32, 'ones_f')
    K.memset('pool', ones_f[:], 1.0, [r])
    C['ones_f'] = ones_f
    ones_b = K.sb(st, [128, 128], BF16, 'ones_b')
    K.memset('pool', ones_b[:], 1.0, [r])
    C['ones_b'] = ones_b
    cc = K.sb(st, [128, 4], F32, 'cconst')
    K.memset('pool', cc[:, 0:1], 1.0, [r])
    K.memset('pool', cc[:, 1:2], EPS, [r])
    K.memset('pool', cc[:, 2:3], 0.0, [r])
    K.memset('pool', cc[:, 3:4], float(np.log(128.0 ** -0.5)), [r])
    C['one'] = cc[:, 0:1]
    C['eps'] = cc[:, 1:2]
    C['zero'] = cc[:, 2:3]
    C['lns'] = cc[:, 3:4]
    ident_f = K.sb(st, [128, 128], F32, 'ident_f')
    K.memset('pool', ident_f[:], 0.0, [r])
    K.asel(ident_f[:], ident_f[:], [[-1, 128]], ALU.not_equal, 1.0, 0, 1, [r], [r])
    C['ident_f'] = ident_f
    ident_b = K.sb(st, [128, 128], BF16, 'ident_b')
    K.copy('pool', ident_b[:], ident_f[:], [r], [r])
    C['ident_b'] = ident_b
    return C


def emit_mod(K, st, C, ada_w_d, ncol, cT_ap, adab_ap, r_prm):
    nch = ncol // 128
    sc = K.sb(st, [128, 16], F32, 'silu_c')
    r_sc = Res()
    K.act(sc[:], cT_ap, AF.Silu, [r_prm], [r_sc])
    mod = K.sb(st, [128, nch, 2], F32, 'mod')
    r_mod = Res()
    with ExitStack() as st2:
        wbuf = [K.sb(st2, [128, 8, 512], F32, 'adaw%d' % i) for i in range(2)]
        rw = [Res(), Res()]
        row = K.sb(st2, [2, ncol], F32, 'modrow')
        r_row = Res()
        prow = [K.ps(st2, [128, 512], F32, 'ps_row%d' % i) for i in range(2)]
        r_prow = [Res(), Res()]
        pm = K.ps(st2, [128, 512], F32, 'ps_mod')
        r_pm = Res()
        wv = ada_w_d.rearrange("(k p) n -> p k n", p=128)
        sc3 = sc[:].rearrange("p (k t) -> p k t", t=2)
        for g in range(ncol // 512):
            wb, rb = wbuf[g % 2], rw[g % 2]
            pr_, rpr = prow[g % 2], r_prow[g % 2]
            K.dma('sp' if g % 2 == 0 else 'act', wb[:], wv[:, :, g * 512:(g + 1) * 512], [], [rb])
            for k in range(8):
                K.mm(pr_[0:2, :], sc3[:, k, :], wb[:, k, :], k == 0, k == 7, [rb, r_sc], [rpr])
            K.copy('dve', row[:, g * 512:(g + 1) * 512], pr_[0:2, :], [rpr], [r_row])
        for ch in range(nch):
            K.tr(pm[:, ch * 2:ch * 2 + 2], row[:, ch * 128:(ch + 1) * 128], C['ident_f'][0:2, 0:2], [r_row, C['res']],
                 [r_pm])
        K.tt('dve', mod[:], pm[:, 0:nch * 2].rearrange("p (c t) -> p c t", t=2),
             adab_ap.unsqueeze(2).to_broadcast([128, nch, 2]), ALU.add, [r_pm, r_prm], [r_mod])
        K.S.barrier()
    return mod, r_mod


def emit_norm_mod(K, C, x, rx, nb, ty, gsc, sh, r_gs, sq, r_sq, pss, r_pss, rstd, r_rstd, tmp, r_tmp, out, r_out,
                  out_f32=None, r_of=None, mul_eng='pool'):
    K.act(sq[:, :, 0:nb], x[:, :, 0:nb], AF.Square, [rx], [r_sq])
    for k in range(8):
        K.mm(pss[:, 0:nb], C['ones_b'][:], sq[:, k, 0:nb], k == 0, k == 7, [r_sq, C['res']], [r_pss])
    K.act(rstd[:, 0:nb], pss[:, 0:nb], AF.Sqrt, [r_pss, C['res']], [r_rstd], bias=C['eps'], scale=1.0 / D)
    K.recip(rstd[:, 0:nb], rstd[:, 0:nb], [r_rstd], [r_rstd])
    for k in range(8):
        K.tt('dve', tmp[:, k, 0:nb], x[:, k, 0:nb], rstd[:, 0:nb], ALU.mult, [rx, r_rstd], [r_tmp])
    for k in range(8):
        if out_f32 is not None:
            K.ts(mul_eng, out_f32[:, k, 0:nb], tmp[:, k, 0:nb], gsc[:, k, ty:ty + 1], sh[:, k, ty:ty + 1],
                 ALU.mult, ALU.add, [r_tmp, r_gs], [r_of])
            K.copy('act', out[:, k, 0:nb], out_f32[:, k, 0:nb], [r_of], [r_out])
        else:
            K.ts(mul_eng, out[:, k, 0:nb], tmp[:, k, 0:nb], gsc[:, k, ty:ty + 1], sh[:, k, ty:ty + 1],
                 ALU.mult, ALU.add, [r_tmp, r_gs], [r_out])


def pack_A(inp, l, b, hd):
    P = Pack()
    cT = np.stack([colT(inp['c'][b]), colT(inp['c_ctx'])], axis=2).reshape(128, 16)
    P.add('cT', cT)
    P.add('adab', colT(inp['ada_b'][l][0:2048]))
    P.add('gmix', colT(inp['norm_mix_g'][l]))
    hs = slice(hd * 128, (hd + 1) * 128)
    P.add('rg_cw', inp['rg_conv_w'][l][:, hs].T)
    P.add('rg_cb', inp['rg_conv_b'][l][hs])
    P.add('rg_gb', inp['rg_gate_b'][l][:, :, hs].reshape(4, 128).T)
    P.add('rg_lam', inp['rg_lambda'][l][:, hs].T)
    P.add('gla_blr', inp['gla_b_lr'][l][:, hd * 64:(hd + 1) * 64].T)
    P.add('gla_ng', inp['gla_norm_g'][l])
    P.add('hg_l0', inp['hgrn_lb_logits'][0][:, hs].T)
    P.add('hg_l1', inp['hgrn_lb_logits'][1][:, hs].T)
    P.add('hg_ng', inp['hgrn_norm_g'][l])
    P.add('ml_cwq', inp['ml_conv_w'][l][:, hs].T)
    P.add('ml_cwk', inp['ml_conv_w'][l][:, 512 + hd * 128: 512 + (hd + 1) * 128].T)
    P.add('ml_cbq', inp['ml_conv_b'][l][hs])
    P.add('ml_cbk', inp['ml_conv_b'][l][512 + hd * 128: 512 + (hd + 1) * 128])
    gb = inp['ml_gate_b'][l][:, :, hd].reshape(4)
    P.add('ml_gb', np.repeat(gb[None, :], 128, axis=0))
    P.add('ml_ng', inp['ml_norm_g'][l])
    return P


def rg_gate_blockdiag(inp, l, hd):
    out = np.zeros((128, 4, 128), np.float32)
    gw = inp['rg_gate_w'][l]
    for d in range(2):
        for g in range(2):
            for kk in range(2):
                out[kk * 64:(kk + 1) * 64, d * 2 + g, kk * 64:(kk + 1) * 64] = gw[d, g, hd * 2 + kk]
    return np.ascontiguousarray(out.reshape(128, 512))


def gla_wlr_pad(inp, l, hd):
    out = np.zeros((32, 2, 64), np.float32)
    for d in range(2):
        out[d * 16:(d + 1) * 16, d, :] = inp['gla_w_lr'][l][d][:, hd * 64:(hd + 1) * 64]
    return np.ascontiguousarray(out.reshape(32, 128))


def emit_A(K, C, l, hsrc, adaw_d, heads, prm_off, uT_d, mixers=('rg', 'gla', 'hg', 'ml')):
    S = K.S
    PRM, r_prm = heads[0]['PRM'], heads[0]['r_prm']
    with ExitStack() as st:
        gsc = K.sb(st, [128, 8, 2], F32, 'gsc')
        sh = K.sb(st, [128, 8, 2], F32, 'sh')
        r_gs = Res()
        with ExitStack() as st0:
            mod, r_mod = emit_mod(K, st0, C, adaw_d, 2048, PRM('cT'), PRM('adab'), r_prm)
            K.ts('dve', gsc[:], mod[:, 8:16, :], 1.0, None, ALU.add, None, [r_mod], [r_gs])
            K.tt('dve', gsc[:], gsc[:], PRM('gmix').unsqueeze(2).to_broadcast([128, 8, 2]), ALU.mult,
                 [r_gs, r_prm], [r_gs])
            K.copy('dve', sh[:], mod[:, 0:8, :], [r_mod], [r_gs])
            S.barrier()

        ures = [Res() for _ in range(NBLK)]
        with ExitStack() as st1:
            xb_ = [K.sb(st1, [128, 8, 512], F32, 'x%d' % i) for i in range(2)]
            rx_ = [Res(), Res()]
            sq = K.sb(st1, [128, 8, 512], BF16, 'sq')
            r_sq = Res()
            pss = [K.ps(st1, [128, 512], F32, 'pss%d' % i) for i in range(2)]
            r_pss = [Res(), Res()]
            rstd = [K.sb(st1, [128, 512], F32, 'rstd%d' % i) for i in range(2)]
            r_rstd = [Res(), Res()]
            tmp = K.sb(st1, [128, 8, 512], F32, 'tmp')
            r_tmp = Res()
            ub = [K.sb(st1, [128, 8, 512], BF16, 'u%d' % i) for i in range(2)]
            r_ub = [Res(), Res()]
            for bi, (t0, nb) in enumerate(A_BLOCKS):
                i2 = bi % 2
                ty = 1 if bi == 0 else 0
                K.dma('sp', xb_[i2][:, :, 0:nb], hsrc[:, :, t0:t0 + nb], [], [rx_[i2]])
                emit_norm_mod(K, C, xb_[i2], rx_[i2], nb, ty, gsc, sh, r_gs, sq, r_sq, pss[i2], r_pss[i2],
                              rstd[i2], r_rstd[i2], tmp, r_tmp, ub[i2], r_ub[i2])
                K.dma('act', uT_d[:, :, t0:t0 + nb], ub[i2][:, :, 0:nb], [r_ub[i2]], [ures[bi]])
            S.barrier()

        outres = []
        for hdd in heads:
            for mx in mixers:
                with ExitStack() as stm:
                    emit_mixer(K, stm, C, mx, l, hdd['PRM'], hdd['r_prm'], prm_off, hdd['winv'], hdd['rgw_d'],
                               hdd['wlr_d'], uT_d, ures, hdd['ys_dst'], outres)
                    S.barrier()
        S.barrier()


def emit_mixer(K, st, C, mx, l, PRM, r_prm, prm_off, winv, rgw_d, wlr_d, uT_d, ures, ys_d, outres):
    S = K.S
    branch = {'rg': 0, 'gla': 1, 'hg': 2, 'ml': 3}[mx]
    wnames = {'rg': ['rg_x', 'rg_y'],
              'gla': ['gla_q', 'gla_k', 'gla_g', 'gla_lr', 'gla_v'],
              'hg': ['hg_q', 'hg_f0', 'hg_f1', 'hg_g', 'hg_i'],
              'ml': ['ml_q', 'ml_k', 'ml_o', 'ml_v', 'ml_i0', 'ml_f0', 'ml_i1', 'ml_f1']}[mx]
    c_lo = A_OFF[wnames[0]][0]
    c_hi = A_OFF[wnames[-1]][0] + A_OFF[wnames[-1]][1]
    ncol = c_hi - c_lo
    wt = K.sb(st, [128, 8, ncol], BF16, 'w_' + mx)
    r_w = Res()
    for k in range(8):
        K.dma('pool', wt[:, k, :], winv[:, k, c_lo:c_hi], [], [r_w])

    def W(name, k):
        o, w = A_OFF[name]
        return wt[:, k, o - c_lo:o - c_lo + w]

    ubuf = [K.sb(st, [128, 8, 512], BF16, 'ub%d' % i) for i in range(2)]
    r_ubuf = [Res(), Res()]
    uctr = [0]

    def load_u(bi):
        t0, nb = A_BLOCKS[bi]
        i = uctr[0] % 2
        uctr[0] += 1
        K.dma('sp', ubuf[i][:, :, 0:nb], uT_d[:, :, t0:t0 + nb], [ures[bi]], [r_ubuf[i]])
        return ubuf[i], r_ubuf[i]

    pproj = [K.ps(st, [128, 512], F32, 'pproj%d' % i) for i in range(2)]
    r_pproj = [Res(), Res()]
    pctr = [0]

    def proj(u, ru, name, nb, M=None):
        o, w = A_OFF[name]
        M = M or w
        i = pctr[0] % 2
        pctr[0] += 1
        for k in range(8):
            K.mm(pproj[i][0:M, 0:nb], W(name, k)[:, 0:M], u[:, k, 0:nb], k == 0, k == 7, [r_w, ru], [r_pproj[i]])
        return pproj[i][0:M, 0:nb], r_pproj[i]

    def conv_block(raw, rraw, bi, cw, cb, outt, r_out):
        t0, nb = A_BLOCKS[bi]
        s0, s1 = (0, NCTX) if bi == 0 else (NCTX, NT)
        rd = [rraw[j] for j in (bi - 1, bi, bi + 1) if 0 <= j < NBLK]
        K.ts('pool', outt[:, 0:nb], raw[:, t0:t0 + nb], cw[:, 2:3], cb, ALU.mult, ALU.add, rd + [r_prm], [r_out])
        for j in (0, 1, 3):
            o = j - 2
            a = max(t0, s0 - o)
            e = min(t0 + nb, s1 - o)
            K.stt('dve', outt[:, a - t0:e - t0], raw[:, a + o:e + o], cw[:, j:j + 1], outt[:, a - t0:e - t0],
                  ALU.mult, ALU.add, rd + [r_prm, r_out], [r_out])

    def final_alloc():
        sqf = [K.sb(st, [128, 512], BF16, 'sqf%d' % i) for i in range(2)]
        rsf = [K.sb(st, [128, 512], F32, 'rsf%d' % i) for i in range(2)]
        yf = [K.sb(st, [128, 512], F32, 'yf%d' % i) for i in range(2)]
        yb = [K.sb(st, [128, 512], BF16, 'yb%d' % i) for i in range(2)]
        return dict(sqf=sqf, rsf=rsf, yf=yf, yb=yb, r_sqf=[Res(), Res()], r_rsf=[Res(), Res()],
                    r_yf=[Res(), Res()], r_yb=[Res(), Res()], n=[0])

    def final_block(T, bi, o_acc, r_oacc, gate, r_gate, ng_ap, pfin, r_pfin):
        t0, nb = A_BLOCKS[bi]
        i = T['n'][0] % 2
        T['n'][0] += 1
        sqf, rsf, yf, yb = T['sqf'], T['rsf'], T['yf'], T['yb']
        r_sqf, r_rsf, r_yf, r_yb = T['r_sqf'], T['r_rsf'], T['r_yf'], T['r_yb']
        K.act(sqf[i][:, 0:nb], o_acc[:, t0:t0 + nb], AF.Square, [r_oacc[bi]], [r_sqf[i]])
        K.mm(pfin[i][:, 0:nb], C['ones_b'][:], sqf[i][:, 0:nb], True, True, [r_sqf[i], C['res']], [r_pfin[i]])
        K.act(rsf[i][:, 0:nb], pfin[i][:, 0:nb], AF.Sqrt, [r_pfin[i], C['res']], [r_rsf[i]], bias=C['eps'],
              scale=1.0 / 128)
        K.recip(rsf[i][:, 0:nb], rsf[i][:, 0:nb], [r_rsf[i]], [r_rsf[i]])
        K.stt('dve', yf[i][:, 0:nb], o_acc[:, t0:t0 + nb], ng_ap, rsf[i][:, 0:nb], ALU.mult, ALU.mult,
              [r_oacc[bi], r_rsf[i], r_prm], [r_yf[i]])
        K.tt('pool', yb[i][:, 0:nb], yf[i][:, 0:nb], gate[:, t0:t0 + nb], ALU.mult, [r_yf[i], r_gate[bi]],
             [r_yb[i]])
        ro = Res()
        K.dma('act', ys_d(branch, t0, nb), yb[i][:, 0:nb], [r_yb[i]], [ro])
        outres.append(ro)

    if mx == 'rg':
        raw = K.sb(st, [128, NT], F32, 'rg_raw')
        r_raw = [Res() for _ in range(NBLK)]
        xb = K.sb(st, [128, NT], F32, 'rg_xb')
        xbb = K.sb(st, [128, NT], BF16, 'rg_xbb')
        r_xb = [Res() for _ in range(NBLK)]
        gy = K.sb(st, [128, NT], BF16, 'rg_gy')
        r_gy = [Res() for _ in range(NBLK)]
        wg = K.sb(st, [128, 512], BF16, 'rg_wg')
        r_wg = Res()
        K.dma('pool', wg[:], rgw_d, [], [r_wg])
        clam = K.sb(st, [128, 2], F32, 'rg_clam')
        r_clam = Res()
        K.act(clam[:], PRM('rg_lam'), AF.Exp, [r_prm], [r_clam], scale=-1.0)
        K.act(clam[:], clam[:], AF.Ln, [r_clam, C['res']], [r_clam], bias=C['one'])
        K.ts('dve', clam[:], clam[:], -8.0, None, ALU.mult, None, [r_clam], [r_clam])
        t1 = [K.sb(st, [128, 512], F32, 'rg_t1_%d' % i) for i in range(2)]
        r_t1 = [Res(), Res()]
        t2 = [K.sb(st, [128, 512], F32, 'rg_t2_%d' % i) for i in range(2)]
        r_t2 = [Res(), Res()]
        xs = [K.sb(st, [128, 512], F32, 'rg_xs_%d' % i) for i in range(2)]
        r_xs = [Res(), Res()]
        for bi, (t0, nb) in enumerate(A_BLOCKS):
            i = bi % 2
            u, ru = load_u(bi)
            p, rp = proj(u, ru, 'rg_x', nb)
            K.copy('act', raw[:, t0:t0 + nb], p, [rp], [r_raw[bi]])
            p, rp = proj(u, ru, 'rg_y', nb)
            K.act(t1[i][:, 0:nb], p, AF.Square, [rp], [r_t1[i]])
            K.copy('act', xs[i][:, 0:nb], p, [rp], [r_xs[i]])
            K.ts('dve', t1[i][:, 0:nb], t1[i][:, 0:nb], 0.044715, 1.0, ALU.mult, ALU.add, [r_t1[i]], [r_t1[i]])
            K.tt('dve', t2[i][:, 0:nb], t1[i][:, 0:nb], xs[i][:, 0:nb], ALU.mult, [r_t1[i], r_xs[i]], [r_t2[i]])
            K.act(t2[i][:, 0:nb], t2[i][:, 0:nb], AF.Sigmoid, [r_t2[i]], [r_t2[i]], scale=1.5957691216)
            K.tt('dve', gy[:, t0:t0 + nb], xs[i][:, 0:nb], t2[i][:, 0:nb], ALU.mult, [r_xs[i], r_t2[i]],
                 [r_gy[bi]])
        for bi, (t0, nb) in enumerate(A_BLOCKS):
            conv_block(raw, r_raw, bi, PRM('rg_cw'), PRM('rg_cb'), xb[:, t0:t0 + nb], r_xb[bi])
            K.copy('act', xbb[:, t0:t0 + nb], xb[:, t0:t0 + nb], [r_xb[bi]], [r_xb[bi]])
        rr = [K.sb(st, [128, 512], F32, 'rg_r%d' % i) for i in range(2)]
        r_rr = [Res(), Res()]
        ii = [K.sb(st, [128, 512], F32, 'rg_i%d' % i) for i in range(2)]
        r_ii = [Res(), Res()]
        aa = [K.sb(st, [128, 512], F32, 'rg_a%d' % i) for i in range(2)]
        r_aa = [Res(), Res()]
        ss = [K.sb(st, [128, 512], F32, 'rg_s%d' % i) for i in range(2)]
        r_ss = [Res(), Res()]
        hh = [K.sb(st, [128, 512], F32, 'rg_h%d' % i) for i in range(2)]
        r_hh = [Res(), Res()]
        gb = PRM('rg_gb')
        n = 0
        for d in range(2):
            carry = 0.0
            r_carry = []
            for bi in blk_order(d):
                t0, nb = A_BLOCKS[bi]
                i = n % 2
                n += 1
                pr = pproj[pctr[0] % 2]
                rpr = r_pproj[pctr[0] % 2]
                pctr[0] += 1
                K.mm(pr[:, 0:nb], wg[:, (d * 2) * 128:(d * 2 + 1) * 128], xbb[:, t0:t0 + nb], True, True,
                     [r_wg, r_xb[bi]], [rpr])
                K.act(rr[i][:, 0:nb], pr[:, 0:nb], AF.Sigmoid, [rpr, r_prm], [r_rr[i]], bias=gb[:, d * 2:d * 2 + 1])
                pi = pproj[pctr[0] % 2]
                rpi = r_pproj[pctr[0] % 2]
                pctr[0] += 1
                K.mm(pi[:, 0:nb], wg[:, (d * 2 + 1) * 128:(d * 2 + 2) * 128], xbb[:, t0:t0 + nb], True, True,
                     [r_wg, r_xb[bi]], [rpi])
                K.act(ii[i][:, 0:nb], pi[:, 0:nb], AF.Sigmoid, [rpi, r_prm], [r_ii[i]],
                      bias=gb[:, d * 2 + 1:d * 2 + 2])
                K.act(aa[i][:, 0:nb], rr[i][:, 0:nb], AF.Exp, [r_rr[i], r_clam], [r_aa[i]], scale=clam[:, d:d + 1])
                K.tt('dve', ss[i][:, 0:nb], aa[i][:, 0:nb], aa[i][:, 0:nb], ALU.mult, [r_aa[i]], [r_ss[i]])
                K.act(ss[i][:, 0:nb], ss[i][:, 0:nb], AF.Sqrt, [r_ss[i], C['res']], [r_ss[i]], bias=C['one'],
                      scale=-1.0)
                K.tt('dve', ii[i][:, 0:nb], ii[i][:, 0:nb], xb[:, t0:t0 + nb], ALU.mult, [r_ii[i], r_xb[bi]],
                     [r_ii[i]])
                K.tt('dve', ii[i][:, 0:nb], ii[i][:, 0:nb], ss[i][:, 0:nb], ALU.mult, [r_ii[i], r_ss[i]],
                     [r_ii[i]])
                ha, aa_, bt_ = hh[i][:, 0:nb], aa[i][:, 0:nb], ii[i][:, 0:nb]
                if d == 1:
                    ha, aa_, bt_ = rev(ha), rev(aa_), rev(bt_)
                K.scan(ha, aa_, bt_, carry, ALU.mult, ALU.add, [r_aa[i], r_ii[i]] + r_carry, [r_hh[i]])
                carry = hh[i][:, nb - 1:nb] if d == 0 else hh[i][:, 0:1]
                r_carry = [r_hh[i]]
                if d == 0:
                    K.copy('pool', raw[:, t0:t0 + nb], hh[i][:, 0:nb], [r_hh[i]], [r_raw[bi]])
                else:
                    K.tt('pool', raw[:, t0:t0 + nb], raw[:, t0:t0 + nb], hh[i][:, 0:nb], ALU.add,
                         [r_hh[i], r_raw[bi]], [r_raw[bi]])
        yb = [K.sb(st, [128, 512], BF16, 'rg_yb%d' % i) for i in range(2)]
        r_yb = [Res(), Res()]
        for bi, (t0, nb) in enumerate(A_BLOCKS):
            i = bi % 2
            K.tt('dve', yb[i][:, 0:nb], raw[:, t0:t0 + nb], gy[:, t0:t0 + nb], ALU.mult, [r_raw[bi], r_gy[bi]],
                 [r_yb[i]])
            ro = Res()
            K.dma('act', ys_d(branch, t0, nb), yb[i][:, 0:nb], [r_yb[i]], [ro])
            outres.append(ro)
        return

    dk = 64 if mx == 'gla' else 128
    SW = 256 if mx == 'ml' else 128
    vname = {'gla': 'gla_v', 'hg': 'hg_i', 'ml': 'ml_v'}[mx]
    q_bf = K.sb(st, [dk, NT], BF16, mx + '_q')
    r_q = [Res() for _ in range(NBLK)]
    if mx != 'hg':
        k_bf = K.sb(st, [dk, NT], BF16, mx + '_k')
        r_k = [Res() for _ in range(NBLK)]
    v_bf = K.sb(st, [64, NCHUNK, 128], BF16, mx + '_v')
    r_v = [Res() for _ in range(NBLK)]
    gate = K.sb(st, [128, NT], BF16, mx + '_gate')
    r_gate = [Res() for _ in range(NBLK)]
    o_acc = K.sb(st, [128, NT], F32, mx + '_oacc')
    r_oacc = [Res() for _ in range(NBLK)]
    stpv = ExitStack()
    pv = K.ps(stpv, [64, 4, 128], F32, 'pv')
    r_pv = Res()

    def proj_v(u, ru, bi, scale):
        t0, nb = A_BLOCKS[bi]
        o, w = A_OFF[vname]
        for g0 in range(0, nb // 64, 4):
            for j in range(4):
                for k in range(8):
                    K.mm(pv[:, j, :], u[:, k, (g0 + j) * 64:(g0 + j + 1) * 64], W(vname, k), k == 0, k == 7,
                         [ru, r_w], [r_pv])
            c0 = t0 // 64 + g0
            K.act(v_bf[:, c0:c0 + 4, :], pv[:], AF.Copy, [r_pv], [r_v[bi]], scale=scale)

    if mx == 'gla':
        def p_work(bi, u, ru):
            t0, nb = A_BLOCKS[bi]
            p, rp = proj(u, ru, 'gla_q', nb)
            K.act(q_bf[:, t0:t0 + nb], p, AF.Copy, [rp], [r_q[bi]], scale=0.125)
            p, rp = proj(u, ru, 'gla_k', nb)
            K.copy('dve', k_bf[:, t0:t0 + nb], p, [rp], [r_k[bi]])
            p, rp = proj(u, ru, 'gla_g', nb)
            K.act(gate[:, t0:t0 + nb], p, AF.Silu, [rp], [r_gate[bi]])
            proj_v(u, ru, bi, 1.0)
    elif mx == 'hg':
        def p_work(bi, u, ru):
            t0, nb = A_BLOCKS[bi]
            p, rp = proj(u, ru, 'hg_q', nb)
            K.act(q_bf[:, t0:t0 + nb], p, AF.Silu, [rp], [r_q[bi]])
            p, rp = proj(u, ru, 'hg_g', nb)
            K.act(gate[:, t0:t0 + nb], p, AF.Silu, [rp], [r_gate[bi]])
            proj_v(u, ru, bi, 128.0 ** -0.5)
    else:
        stp = ExitStack()
        ctmp = [K.sb(stp, [128, 512], F32, 'ml_ct%d' % i) for i in range(2)]
        r_ct = [Res(), Res()]
        for which in range(2):
            nm = ('ml_q', 'ml_k')[which]
            dst, rdst = ((q_bf, r_q), (k_bf, r_k))[which]
            cw = PRM(('ml_cwq', 'ml_cwk')[which])
            cb = PRM(('ml_cbq', 'ml_cbk')[which])
            for bi, (t0, nb) in enumerate(A_BLOCKS):
                u, ru = load_u(bi)
                p, rp = proj(u, ru, nm, nb)
                K.copy('act', o_acc[:, t0:t0 + nb], p, [rp], [r_oacc[bi]])
                if which == 0:
                    p, rp = proj(u, ru, 'ml_o', nb)
                    K.act(gate[:, t0:t0 + nb], p, AF.Sigmoid, [rp], [r_gate[bi]])
                    proj_v(u, ru, bi, 1.0)
            for bi, (t0, nb) in enumerate(A_BLOCKS):
                i = bi % 2
                conv_block(o_acc, r_oacc, bi, cw, cb, ctmp[i][:, 0:nb], r_ct[i])
                K.act(dst[:, t0:t0 + nb], ctmp[i][:, 0:nb], AF.Silu, [r_ct[i]], [rdst[bi]])
        S.barrier()
        stp.close()

    if mx == 'ml':
        S.barrier()
        stpv.close()
    else:
        fin_t = final_alloc()
    std = ExitStack()
    mask = K.sb(std, [64, 2, 64], F32, 'mask')
    r_mask = Res()
    K.memset('pool', mask[:], 1.0, [r_mask])
    K.asel(mask[:, 0, :], mask[:, 0, :], [[1, 64]], ALU.is_ge, 0.0, 0, -1, [r_mask], [r_mask])
    K.asel(mask[:, 1, :], mask[:, 1, :], [[-1, 64]], ALU.is_ge, 0.0, 0, 1, [r_mask], [r_mask])
    rmask = K.sb(std, [128, 2, 512], F32, 'rmask')
    r_rmask = Res()
    K.memset('pool', rmask[:], 1.0, [r_rmask])
    K.memset('pool', rmask[:, 0, :].rearrange("p (c i) -> p c i", i=64)[:, :, 0:1], 0.0, [r_rmask])
    K.memset('pool', rmask[:, 1, :].rearrange("p (c i) -> p c i", i=64)[:, :, 63:64], 0.0, [r_rmask])
    hmask = K.sb(std, [128, 2, 512], BF16, 'hmask')
    K.memset('pool', hmask[:], 1.0, [r_rmask])
    K.memset('pool', hmask[:, 0, :].rearrange("p (c i) -> p c i", i=64)[:, :, 32:64], 0.0, [r_rmask])
    K.memset('pool', hmask[:, 1, :].rearrange("p (c i) -> p c i", i=64)[:, :, 0:32], 0.0, [r_rmask])

    if mx == 'gla':
        wlr = K.sb(std, [32, 128], BF16, 'wlr')
        r_wlr = Res()
        K.dma('pool', wlr[:], wlr_d, [], [r_wlr])
        nblr = K.sb(std, [64, 2], F32, 'nblr')
        r_nblr = Res()
        K.ts('dve', nblr[:], PRM('gla_blr', 64), -1.0, None, ALU.mult, None, [r_prm], [r_nblr])
        lrs = [K.sb(std, [32, 512], BF16, 'lrs%d' % i) for i in range(2)]
        r_lrs = [Res(), Res()]
    if mx == 'hg':
        lbt = K.sb(std, [128, 4], F32, 'lbt')
        r_lbt = Res()
        if l == 0:
            K.memset('pool', lbt[:, 0:2], 0.0, [r_lbt])
        else:
            K.tt('dve', lbt[:, 0:2], PRM('hg_l1'), PRM('hg_l0'), ALU.subtract, [r_prm], [r_lbt])
            K.act(lbt[:, 0:2], lbt[:, 0:2], AF.Sigmoid, [r_lbt], [r_lbt])
        K.ts('dve', lbt[:, 2:4], lbt[:, 0:2], -1.0, 1.0, ALU.mult, ALU.add, [r_lbt], [r_lbt])
    if mx == 'ml':
        ngb = K.sb(std, [128, 4], F32, 'ngb')
        r_ngb = Res()
        K.ts('dve', ngb[:], PRM('ml_gb'), -1.0, None, ALU.mult, None, [r_prm], [r_ngb])
        carG = K.sb(std, [128, 1], F32, 'carG')
        carM = K.sb(std, [128, 1], F32, 'carM')
        r_car = Res()

    def T2(name, shape, dt=F32):
        return [K.sb(std, shape, dt, '%s_%s%d' % (mx, name, i)) for i in range(2)], [Res(), Res()]

    glog, r_glog = T2('glog', [dk, 512])
    bb, r_bb = T2('b', [dk, 512])
    d1, r_d1 = T2('d1', [dk, 512])
    if mx == 'ml':
        d2, r_d2 = T2('d2', [dk, 512])
        d3, r_d3 = T2('d3', [dk, 512])
    qt, r_qt = T2('qt', [dk, 512], BF16)
    kt, r_kt = T2('kt', [dk, 512], BF16)
    if mx != 'ml':
        ktz, r_ktz = T2('ktz', [dk, 512], BF16)
    e2b, r_e2b = T2('e2b', [dk, 512], BF16)
    e3b, r_e3b = T2('e3b', [dk, 512], BF16)
    ke, r_ke = T2('ke', [dk, 512], BF16)
    keT, r_keT = T2('keT', [64, 8, dk], BF16)
    sm, r_sm = T2('sm', [dk, 32])
    if mx == 'hg':
        kraw, r_kraw = T2('kraw', [dk, 512])
    if mx == 'ml':
        ip, r_ip = T2('ip', [128, 512])
        clampt, r_cl = T2('clamp', [128, 512])
        hht, r_hht = ip, r_ip
    stm, r_stm = T2('stm', [64, 64], BF16)
    Sfl = [K.sb(std, [dk, SW], F32, mx + '_Sf%d' % i) for i in range(2)]
    r_Sfl = [Res(), Res()]
    Sb, r_Sb = T2('Sb', [dk, SW], BF16)
    p_stt = K.ps(std, [128, 512], F32, 'p_st')
    p_st = [p_stt[0:64, 0:64], p_stt[0:64, 0:64]]
    r_pst = [Res()] * 2
    p_o = K.ps(std, [128, 512], F32, 'p_o')
    r_po = Res()
    if mx == 'ml':
        p_den = K.ps(std, [128, 512], F32, 'p_den')
        r_pden = Res()
    p_trt = K.ps(std, [64, 8, 128], BF16, 'p_tr')
    p_tr = p_trt[:, :, 0:dk]
    r_ptr = Res()
    p_dst = [K.ps(std, [128, 512], F32, 'p_ds%d' % i) for i in range(2)]
    p_dsl = [p_dst[0][0:dk, 0:SW], p_dst[1][0:dk, 0:SW]]
    r_pdsl = [Res(), Res()]

    nch_ctr = [0]
    ng = PRM({'gla': 'gla_ng', 'hg': 'hg_ng', 'ml': 'ml_ng'}[mx])

    def gate_phase(d, bi, i, first):
        t0, nb = A_BLOCKS[bi]
        nchk = nb // 64
        if first and mx == 'ml':
            K.memset('dve', carG[:], 0.0, [r_car])
            K.memset('dve', carM[:], 0.0, [r_car])
        u, ru = load_u(bi)
        if d == 0 and mx != 'ml':
            p_work(bi, u, ru)

        def c3(ap):
            return ap.rearrange("p (c i) -> p c i", i=64)
        endi = 63 if d == 0 else 0

        if mx in ('gla', 'hg'):
            if mx == 'gla':
                p, rp = proj(u, ru, 'gla_lr', nb)
                K.copy('act', lrs[i][:, 0:nb], p, [rp], [r_lrs[i]])
                pp = pproj[pctr[0] % 2]
                rpp = r_pproj[pctr[0] % 2]
                pctr[0] += 1
                K.mm(pp[0:64, 0:nb], wlr[:, d * 64:(d + 1) * 64], lrs[i][:, 0:nb], True, True,
                     [r_wlr, r_lrs[i]], [rpp])
                K.act(glog[i][:, 0:nb], pp[0:64, 0:nb], AF.Exp, [rpp, r_nblr], [r_glog[i]],
                      bias=nblr[:, d:d + 1], scale=-1.0)
                K.act(glog[i][:, 0:nb], glog[i][:, 0:nb], AF.Ln, [r_glog[i], C['res']], [r_glog[i]],
                      bias=C['one'][0:64, :])
                ksrc, r_ksrc = k_bf[:, t0:t0 + nb], r_k[bi]
            else:
                p, rp = proj(u, ru, 'hg_f%d' % d, nb)
                K.act(kraw[i][:, 0:nb], p, AF.Sigmoid, [rp], [r_kraw[i]])
                K.ts('dve', kraw[i][:, 0:nb], kraw[i][:, 0:nb], lbt[:, 2 + d:3 + d], lbt[:, d:d + 1],
                     ALU.mult, ALU.add, [r_kraw[i], r_lbt], [r_kraw[i]])
                K.act(glog[i][:, 0:nb], kraw[i][:, 0:nb], AF.Ln, [r_kraw[i]], [r_glog[i]])
                K.ts('pool', kraw[i][:, 0:nb], kraw[i][:, 0:nb], -1.0, 1.0, ALU.mult, ALU.add, [r_kraw[i]],
                     [r_kraw[i]])
                ksrc, r_ksrc = kraw[i][:, 0:nb], r_kraw[i]
            ba, ga, ma = bb[i][:, 0:nb], glog[i][:, 0:nb], rmask[0:dk, d, 0:nb]
            if d == 1:
                ba, ga, ma = rev(ba), rev(ga), rev(ma)
            K.scan(ba, ma, ga, 0.0, ALU.mult, ALU.add, [r_glog[i], r_rmask], [r_bb[i]])
            b3 = c3(bb[i][:, 0:nb])
            bend = b3[:, :, endi]
            href = sm[i][:, 0:nchk]
            midi = 31 if d == 0 else 32
            sc = -1.0 / 16.0 if mx == 'gla' else 1.0
            K.copy('pool', href, b3[:, :, midi], [r_bb[i]], [r_sm[i]])
            K.act(sm[i][:, 8:8 + nchk], bend, AF.Exp, [r_bb[i]], [r_sm[i]], scale=sc)
            K.act(sm[i][:, 16:16 + nchk], b3[:, :, midi], AF.Exp, [r_bb[i]], [r_sm[i]], scale=sc)
            K.tt('dve', c3(d1[i][:, 0:nb]), b3, href.unsqueeze(2).to_broadcast([dk, nchk, 64]), ALU.subtract,
                 [r_bb[i], r_sm[i]], [r_d1[i]])
            K.act(e2b[i][:, 0:nb], d1[i][:, 0:nb], AF.Exp, [r_d1[i]], [r_e2b[i]], scale=sc)
            K.act(e3b[i][:, 0:nb], d1[i][:, 0:nb], AF.Exp, [r_d1[i]], [r_e3b[i]], scale=-sc)
            ratio = sm[i][:, 24:24 + nchk]
            K.tt('pool', ratio, bend, href, ALU.subtract, [r_bb[i], r_sm[i]], [r_sm[i]])
            K.act(ratio, ratio, AF.Exp, [r_sm[i]], [r_sm[i]], scale=sc)
            K.tt('dve', qt[i][:, 0:nb], q_bf[:, t0:t0 + nb], e2b[i][:, 0:nb], ALU.mult, [r_q[bi], r_e2b[i]],
                 [r_qt[i]])
            K.tt('dve', kt[i][:, 0:nb], ksrc, e3b[i][:, 0:nb], ALU.mult, [r_ksrc, r_e3b[i]], [r_kt[i]])
            K.tt('dve', ktz[i][:, 0:nb], kt[i][:, 0:nb], hmask[0:dk, d, 0:nb], ALU.mult, [r_kt[i], r_rmask],
                 [r_ktz[i]])
            K.tt('dve', c3(ke[i][:, 0:nb]), c3(kt[i][:, 0:nb]), ratio.unsqueeze(2).to_broadcast([dk, nchk, 64]),
                 ALU.mult, [r_kt[i], r_sm[i]], [r_ke[i]])
            dec_col = lambda c: sm[i][:, 8 + c:9 + c]
            eref_col = lambda c: sm[i][:, 16 + c:17 + c]
        else:
            p, rp = proj(u, ru, 'ml_i%d' % d, nb)
            K.act(ip[i][:, 0:nb], p, AF.Identity, [rp, r_prm], [r_ip[i]],
                  bias=PRM('ml_gb')[:, d * 2:d * 2 + 1])
            p, rp = proj(u, ru, 'ml_f%d' % d, nb)
            K.act(glog[i][:, 0:nb], p, AF.Exp, [rp, r_ngb], [r_glog[i]], bias=ngb[:, d * 2 + 1:d * 2 + 2],
                  scale=-1.0)
            K.act(glog[i][:, 0:nb], glog[i][:, 0:nb], AF.Ln, [r_glog[i], C['res']], [r_glog[i]],
                  bias=C['one'])
            ba, ga, oa = bb[i][:, 0:nb], glog[i][:, 0:nb], C['ones_f'][:, 0:nb]
            if d == 1:
                ba, ga = rev(ba), rev(ga)
            K.scan(ba, oa, ga, carG[:, 0:1], ALU.mult, ALU.add, [r_glog[i], C['res'], r_car], [r_bb[i]])
            K.tt('dve', d1[i][:, 0:nb], ip[i][:, 0:nb], bb[i][:, 0:nb], ALU.add, [r_ip[i], r_bb[i]],
                 [r_d1[i]])
            ma, aa_ = d2[i][:, 0:nb], d1[i][:, 0:nb]
            if d == 1:
                ma, aa_ = rev(ma), rev(aa_)
            K.scan(ma, aa_, aa_, carM[:, 0:1], ALU.max, ALU.max, [r_d1[i], r_car], [r_d2[i]])
            A3, M3 = c3(d1[i][:, 0:nb]), c3(d2[i][:, 0:nb])
            Rb = sm[i][:, 24:24 + nchk]
            if d == 0:
                K.copy('pool', sm[i][:, 24:25], carM[:, 0:1], [r_car], [r_sm[i]])
                if nchk > 1:
                    K.copy('pool', sm[i][:, 25:24 + nchk], M3[:, 0:nchk - 1, 63], [r_d2[i]], [r_sm[i]])
                Rn = M3[:, :, 63]
                last = nb - 1
            else:
                K.copy('pool', sm[i][:, 24 + nchk - 1:24 + nchk], carM[:, 0:1], [r_car], [r_sm[i]])
                if nchk > 1:
                    K.copy('pool', sm[i][:, 24:24 + nchk - 1], M3[:, 1:nchk, 0], [r_d2[i]], [r_sm[i]])
                Rn = M3[:, :, 0]
                last = 0
            K.tt('dve', clampt[i][:, 0:nb], bb[i][:, 0:nb], d2[i][:, 0:nb], ALU.subtract, [r_bb[i], r_d2[i]],
                 [r_cl[i]])
            K.act(clampt[i][:, 0:nb], clampt[i][:, 0:nb], AF.Exp, [r_cl[i]], [r_cl[i]])
            K.tt('dve', sm[i][:, 8:8 + nchk], Rb, Rn, ALU.subtract, [r_sm[i], r_d2[i]], [r_sm[i]])
            K.act(sm[i][:, 8:8 + nchk], sm[i][:, 8:8 + nchk], AF.Exp, [r_sm[i]], [r_sm[i]])
            K.copy('pool', carG[:, 0:1], bb[i][:, last:last + 1], [r_bb[i]], [r_car])
            K.copy('pool', carM[:, 0:1], d2[i][:, last:last + 1], [r_d2[i]], [r_car])
            Rbb = Rb.unsqueeze(2).to_broadcast([128, nchk, 64])
            K.tt('dve', c3(d3[i][:, 0:nb]), A3, Rbb, ALU.subtract, [r_d1[i], r_sm[i]], [r_d3[i]])
            K.act(e3b[i][:, 0:nb], d3[i][:, 0:nb], AF.Exp, [r_d3[i]], [r_e3b[i]])
            K.tt('dve', kt[i][:, 0:nb], k_bf[:, t0:t0 + nb], e3b[i][:, 0:nb], ALU.mult, [r_k[bi], r_e3b[i]],
                 [r_kt[i]])
            K.tt('dve', c3(glog[i][:, 0:nb]), M3, Rbb, ALU.subtract, [r_d2[i], r_sm[i]], [r_glog[i]])
            K.act(e2b[i][:, 0:nb], glog[i][:, 0:nb], AF.Exp, [r_glog[i], C['res']], [r_e2b[i]],
                  bias=C['lns'], scale=-1.0)
            K.tt('dve', qt[i][:, 0:nb], q_bf[:, t0:t0 + nb], e2b[i][:, 0:nb], ALU.mult, [r_q[bi], r_e2b[i]],
                 [r_qt[i]])
            K.tt('dve', c3(ke[i][:, 0:nb]), c3(kt[i][:, 0:nb]),
                 sm[i][:, 8:8 + nchk].unsqueeze(2).to_broadcast([128, nchk, 64]), ALU.mult, [r_kt[i], r_sm[i]],
                 [r_ke[i]])
            dec_col = lambda c: sm[i][:, 8 + c:9 + c]
            eref_col = None
        for c in range(nchk):
            K.tr(p_tr[:, c, :], ke[i][:, c * 64:(c + 1) * 64], C['ident_b'][0:dk, 0:dk], [r_ke[i], C['res']],
                 [r_ptr])
        K.copy('act', keT[i][:, 0:nchk, :], p_tr[:, 0:nchk, :], [r_ptr], [r_keT[i]])

    def chunk_phase(d, bi, i, first):
        t0, nb = A_BLOCKS[bi]
        nchk = nb // 64
        if first:
            K.memset('dve', Sfl[nch_ctr[0] % 2][:], 0.0, [r_Sfl[nch_ctr[0] % 2]])
        dec_col = lambda c: sm[i][:, 8 + c:9 + c]
        eref_col = (lambda c: sm[i][:, 16 + c:17 + c]) if mx != 'ml' else None
        corder = range(nchk) if d == 0 else range(nchk - 1, -1, -1)
        for c in corder:
            gch = t0 // 64 + c
            j = nch_ctr[0] % 2
            nch_ctr[0] += 1
            Sf, r_Sf = Sfl[j], r_Sfl[j]
            Sn, r_Sn = Sfl[1 - j], r_Sfl[1 - j]
            p_ds, r_pds = p_dsl[j], r_pdsl[j]
            cs = slice(c * 64, (c + 1) * 64)
            if eref_col is not None:
                K.act(Sb[j][:], Sf[:], AF.Copy, [r_Sf, r_sm[i]], [r_Sb[j]], scale=eref_col(c))
            else:
                K.copy('act', Sb[j][:], Sf[:], [r_Sf], [r_Sb[j]])
            if mx == 'ml':
                K.mm(p_st[j], kt[i][:, cs], qt[i][:, cs], True, True, [r_kt[i], r_qt[i]], [r_pst[j]])
            else:
                lo = slice(c * 64, c * 64 + 32)
                hi = slice(c * 64 + 32, c * 64 + 64)
                full_i, full_o, z_i, z_o = (hi, slice(32, 64), lo, slice(0, 32)) if d == 0 else \
                    (lo, slice(0, 32), hi, slice(32, 64))
                K.mm(p_st[j][:, full_o], kt[i][:, cs], qt[i][:, full_i], True, True, [r_kt[i], r_qt[i]],
                     [r_pst[j]])
                K.mm(p_st[j][:, z_o], ktz[i][:, cs], qt[i][:, z_i], True, True, [r_ktz[i], r_qt[i]],
                     [r_pst[j]])
            K.mm(p_ds[:, 0:128], keT[i][:, c, :], v_bf[:, gch, :], True, True, [r_keT[i], r_v[bi]], [r_pds])
            if mx == 'ml':
                K.mm(p_ds[:, 128:256], keT[i][:, c, :], C['ones_b'][0:64, :], True, True,
                     [r_keT[i], C['res']], [r_pds])
            K.tt('dve', stm[j][:], p_st[j], mask[:, d, :], ALU.mult, [r_pst[j], r_mask], [r_stm[j]])
            K.stt('dve', Sn[:], Sf[:], dec_col(c), p_ds, ALU.mult, ALU.add, [r_Sf, r_sm[i], r_pds], [r_Sn])
            K.mm(p_o[:, cs], v_bf[:, gch, :], stm[j][:], True, False, [r_v[bi], r_stm[j]], [r_po])
            K.mm(p_o[:, cs], Sb[j][:, 0:128], qt[i][:, cs], False, True, [r_Sb[j], r_qt[i]], [r_po])
            if mx == 'ml':
                K.mm(p_den[:, cs], C['ones_b'][0:64, :], stm[j][:], True, False, [r_stm[j], C['res']], [r_pden])
                K.mm(p_den[:, cs], Sb[j][:, 128:256], qt[i][:, cs], False, True, [r_Sb[j], r_qt[i]], [r_pden])
        if mx == 'ml':
            K.act(hht[i][:, 0:nb], p_den[:, 0:nb], AF.Abs, [r_pden], [r_hht[i]])
            K.tt('dve', hht[i][:, 0:nb], hht[i][:, 0:nb], clampt[i][:, 0:nb], ALU.max, [r_hht[i], r_cl[i]],
                 [r_hht[i]])
            K.recip(hht[i][:, 0:nb], hht[i][:, 0:nb], [r_hht[i]], [r_hht[i]])
            K.tt('dve', hht[i][:, 0:nb], p_o[:, 0:nb], hht[i][:, 0:nb], ALU.mult, [r_po, r_hht[i]], [r_hht[i]])
            if d == 0:
                K.copy('pool', o_acc[:, t0:t0 + nb], hht[i][:, 0:nb], [r_hht[i]], [r_oacc[bi]])
            else:
                K.tt('pool', o_acc[:, t0:t0 + nb], o_acc[:, t0:t0 + nb], hht[i][:, 0:nb], ALU.add,
                     [r_hht[i], r_oacc[bi]], [r_oacc[bi]])
                Tm = dict(sqf=[e2b[i]] * 2, yb=[e3b[i]] * 2, rsf=[hht[i]] * 2, yf=[clampt[i]] * 2,
                          r_sqf=[r_e2b[i]] * 2, r_yb=[r_e3b[i]] * 2, r_rsf=[r_hht[i]] * 2, r_yf=[r_cl[i]] * 2, n=[0])
                final_block(Tm, bi, o_acc, r_oacc, gate, r_gate, ng, pproj, r_pproj)
        else:
            if d == 0:
                K.copy('act', o_acc[:, t0:t0 + nb], p_o[:, 0:nb], [r_po], [r_oacc[bi]])
            else:
                K.tt('dve', o_acc[:, t0:t0 + nb], p_o[:, 0:nb], o_acc[:, t0:t0 + nb], ALU.add,
                     [r_po, r_oacc[bi]], [r_oacc[bi]])
                final_block(fin_t, bi, o_acc, r_oacc, gate, r_gate, ng, pproj, r_pproj)

    seq = [(d, bi, k == 0) for d in range(2) for k, bi in enumerate(blk_order(d))]
    for s_ in range(len(seq) + 1):
        builders = []
        if s_ < len(seq):
            builders.append(lambda a=seq[s_], i=s_ % 2: gate_phase(a[0], a[1], i, a[2]))
        if s_ >= 1:
            builders.append(lambda a=seq[s_ - 1], i=(s_ - 1) % 2: chunk_phase(a[0], a[1], i, a[2]))
        S.run_streams(builders)
    S.barrier()
    std.close()
    if mx != 'ml':
        S.barrier()
        stpv.close()


def b_blocks(n):
    blks = [(0, 256, 1)]
    t = 256
    while t < n:
        nb = min(512, n - t)
        blks.append((t, nb, 0))
        t += nb
    return blks


def pack_B(inp, l, b, q, last):
    P = Pack()
    ca = inp['c_ctx'] if (not last and q == 0) else inp['c'][b]
    cT = np.stack([colT(inp['c'][b]), colT(ca)], axis=2).reshape(128, 16)
    P.add('cT', cT)
    P.add('adab', colT(inp['ada_b'][l]))
    P.add('gmix', colT(inp['norm_mix_g'][l]))
    P.add('gffn', colT(inp['norm_ffn_g'][l]))
    P.add('bm', colT(inp['b_merge'][l]))
    P.add('gfin', colT(inp['final_norm_g']))
    rb = np.concatenate([inp['moe_b_group'][l], inp['moe_b_expert'][l]])[None, :]
    P.add('rb', np.repeat(rb, 128, axis=0))
    return P


def emit_B_mods(K, st, C, adaw_d, PRM, r_prm):
    S = K.S
    mods = K.sb(st, [128, 6, 8, 2], F32, 'mods')
    r_mods = Res()
    with ExitStack() as st0:
        mod, r_mod = emit_mod(K, st0, C, adaw_d, 6144, PRM('cT'), PRM('adab'), r_prm)
        for dst, src, gname in ((0, 1, 'gmix'), (3, 4, 'gffn')):
            K.ts('dve', mods[:, dst], mod[:, src * 8:(src + 1) * 8, :], 1.0, None, ALU.add, None, [r_mod],
                 [r_mods])
            K.tt('dve', mods[:, dst], mods[:, dst], PRM(gname).unsqueeze(2).to_broadcast([128, 8, 2]),
                 ALU.mult, [r_mods, r_prm], [r_mods])
        for dst, src in ((1, 0), (2, 2), (4, 3), (5, 5)):
            K.copy('dve', mods[:, dst], mod[:, src * 8:(src + 1) * 8, :], [r_mod], [r_mods])
        S.barrier()
    return mods, r_mods


def emit_B(K, C, last, n, blks, mods, r_mods, PRM, r_prm, Wd, h_src, h_reads, ys_src, out_dst, out_res, hmid_d, u2_d):
    S = K.S
    gsc1, sh1, g1, gsc2, sh2, g2 = [mods[:, i] for i in range(6)]
    with ExitStack() as st:
        wtsT = K.sb(st, [16, n], F32, 'wtsT')
        r_wtsT = [Res() for _ in blks]
        r_hmid = [Res() for _ in blks]
        r_u2 = [Res() for _ in blks]

        with ExitStack() as st1:
            wm = K.sb(st1, [128, 8, 4096], BF16, 'wm')
            r_wm = Res()
            wmv = Wd['wm'].rearrange("(k p) n -> p k n", p=128)
            for k in range(8):
                K.dma('pool', wm[:, k, :], wmv[:, k, :], [], [r_wm])
            wbr = K.sb(st1, [128, 16, D], BF16, 'wbr')
            r_wbr = Res()
            wbrv = Wd['wbr'].rearrange("(j p) n -> p j n", p=128)
            for j in range(0, 16, 4):
                K.dma('pool', wbr[:, j:j + 4, :], wbrv[:, j:j + 4, :], [], [r_wbr])
            wo = K.sb(st1, [128, 8, D], BF16, 'wo')
            r_wo = Res()
            wov = Wd['wo'].rearrange("(k p) n -> p k n", p=128)
            for k in range(0, 8, 4):
                K.dma('pool', wo[:, k:k + 4, :], wov[:, k:k + 4, :], [], [r_wo])
            wr = K.sb(st1, [128, 8, 20], F32, 'wr')
            r_wr = Res()
            K.dma('act', wr[:], Wd['wr'].rearrange("(k p) n -> p k n", p=128), [], [r_wr])
            x = K.sb(st1, [128, 8, 512], F32, 'bx')
            r_x = Res()
            ysb = K.sb(st1, [128, 16, 512], BF16, 'bys')
            r_ys = Res()
            rstd = K.sb(st1, [128, 512], F32, 'brstd')
            r_rstd = Res()
            tmp = K.sb(st1, [128, 8, 512], F32, 'btmp')
            r_tmp = Res()
            u = K.sb(st1, [128, 8, 512], BF16, 'bu')
            r_u = Res()
            u2f, r_u2f = tmp, r_tmp
            merged = K.sb(st1, [128, 8, 512], BF16, 'bmerged')
            r_merged = Res()
            sq, r_sq = merged, r_merged
            gt = [K.sb(st1, [128, 512], F32, 'bgt%d' % i) for i in range(2)]
            r_gt = [Res(), Res()]
            mt = [K.sb(st1, [128, 512], F32, 'bmt%d' % i) for i in range(2)]
            r_mt = [Res(), Res()]
            macc = K.sb(st1, [128, 512], F32, 'bmacc')
            r_macc = Res()
            rt = K.sb(st1, [128, 64], F32, 'brt')
            r_rt = Res()
            pss = K.ps(st1, [128, 512], F32, 'bpss')
            r_pss = Res()
            pg = [K.ps(st1, [128, 512], F32, 'bpg%d' % i) for i in range(2)]
            r_pg = [Res(), Res()]
            pb = [K.ps(st1, [128, 512], F32, 'bpb%d' % i) for i in range(2)]
            r_pb = [Res(), Res()]
            pm = K.ps(st1, [128, 512], F32, 'bpm')
            r_pm = Res()
            pr = K.ps(st1, [128, 512], F32, 'bpr')
            r_pr = Res()
            ctr = 0
            for bi, (t0, nb, ty) in enumerate(blks):
                K.dma('sp', x[:, :, 0:nb], h_src(t0, nb), h_reads, [r_x])
                K.dma('act', ysb[:, :, 0:nb], ys_src(t0, nb), [], [r_ys])
                emit_norm_mod(K, C, x, r_x, nb, ty, gsc1, sh1, r_mods, sq, r_sq, pss, r_pss, rstd, r_rstd, tmp, r_tmp,
                              u, r_u)
                for dc in range(8):
                    for k in range(4):
                        i = ctr % 2
                        ctr += 1
                        for kc in range(8):
                            K.mm(pg[i][:, 0:nb], wm[:, kc, k * 1024 + dc * 128:k * 1024 + (dc + 1) * 128],
                                 u[:, kc, 0:nb], kc == 0, kc == 7, [r_wm, r_u], [r_pg[i]])
                        K.act(gt[i][:, 0:nb], pg[i][:, 0:nb], AF.Sigmoid, [r_pg[i], r_prm], [r_gt[i]],
                              bias=PRM('bm')[:, k * 8 + dc:k * 8 + dc + 1])
                        for cc in range(4):
                            K.mm(pb[i][:, 0:nb], wbr[:, k * 4 + cc, dc * 128:(dc + 1) * 128], ysb[:, k * 4 + cc, 0:nb],
                                 cc == 0, cc == 3, [r_wbr, r_ys], [r_pb[i]])
                        if k == 0:
                            K.tt('dve', macc[:, 0:nb], pb[i][:, 0:nb], gt[i][:, 0:nb], ALU.mult, [r_pb[i], r_gt[i]],
                                 [r_macc])
                        else:
                            K.tt('dve', mt[i][:, 0:nb], pb[i][:, 0:nb], gt[i][:, 0:nb], ALU.mult,
                                 [r_pb[i], r_gt[i]], [r_mt[i]])
                            if k < 3:
                                K.tt('pool', macc[:, 0:nb], macc[:, 0:nb], mt[i][:, 0:nb], ALU.add,
                                     [r_macc, r_mt[i]], [r_macc])
                            else:
                                K.tt('pool', merged[:, dc, 0:nb], macc[:, 0:nb], mt[i][:, 0:nb], ALU.add,
                                     [r_macc, r_mt[i]], [r_merged])
                for dc in range(8):
                    for kc in range(8):
                        K.mm(pm[:, 0:nb], wo[:, kc, dc * 128:(dc + 1) * 128], merged[:, kc, 0:nb], kc == 0, kc == 7,
                             [r_wo, r_merged], [r_pm])
                    K.stt('dve', x[:, dc, 0:nb], pm[:, 0:nb], g1[:, dc, ty:ty + 1], x[:, dc, 0:nb], ALU.mult, ALU.add,
                          [r_pm, r_mods, r_x], [r_x])
                K.dma('sp', hmid_d[:, :, t0:t0 + nb], x[:, :, 0:nb], [r_x], [r_hmid[bi]])
                emit_norm_mod(K, C, x, r_x, nb, ty, gsc2, sh2, r_mods, sq, r_sq, pss, r_pss, rstd, r_rstd, tmp, r_tmp,
                              u, r_u, out_f32=u2f, r_of=r_u2f)
                K.dma('act', u2_d[:, :, t0:t0 + nb], u[:, :, 0:nb], [r_u], [r_u2[bi]])
                for s0 in range(0, nb, 128):
                    m = min(128, nb - s0)
                    for k in range(8):
                        K.mm(pr[0:m, 0:20], u2f[:, k, s0:s0 + m], wr[:, k, :], k == 0, k == 7, [r_u2f, r_wr], [r_pr])
                    lg = rt[0:m, 0:20]
                    K.tt('dve', lg, pr[0:m, 0:20], PRM('rb', m), ALU.add, [r_pr, r_prm], [r_rt])
                    gmax, ngmax, gsum = rt[0:m, 20:21], rt[0:m, 21:22], rt[0:m, 22:23]
                    K.S.op('dve', (lambda o, i_: (lambda e: e.tensor_reduce(out=o, in_=i_, axis=mybir.AxisListType.X,
                                                                             op=ALU.max)))(gmax, rt[0:m, 0:4]),
                           [r_rt], [r_rt])
                    K.ts('dve', ngmax, gmax, -1.0, None, ALU.mult, None, [r_rt], [r_rt])
                    ge = rt[0:m, 24:28]
                    K.act(ge, rt[0:m, 0:4], AF.Exp, [r_rt], [r_rt], bias=ngmax)
                    K.S.op('dve', (lambda o, i_: (lambda e: e.tensor_reduce(out=o, in_=i_, axis=mybir.AxisListType.X,
                                                                             op=ALU.add)))(gsum, ge), [r_rt], [r_rt])
                    pgr = rt[0:m, 23:24]
                    K.recip(pgr, gsum, [r_rt], [r_rt])
                    pen = rt[0:m, 28:32]
                    K.ts('dve', pen, rt[0:m, 0:4], gmax, None, ALU.is_equal, None, [r_rt], [r_rt])
                    K.ts('dve', pen, pen, 1e30, -1e30, ALU.mult, ALU.add, [r_rt], [r_rt])
                    em = rt[0:m, 32:48]
                    K.tt('dve', em.rearrange("p (g e) -> p g e", e=4), rt[0:m, 4:20].rearrange("p (g e) -> p g e", e=4),
                         pen.unsqueeze(2).to_broadcast([m, 4, 4]), ALU.add, [r_rt], [r_rt])
                    m1, m2, dd = rt[0:m, 48:49], rt[0:m, 49:50], rt[0:m, 50:51]
                    K.S.op('dve', (lambda o, i_: (lambda e: e.tensor_reduce(out=o, in_=i_, axis=mybir.AxisListType.X,
                                                                             op=ALU.max)))(m1, em), [r_rt], [r_rt])
                    mk1 = rt[0:m, 4:20]
                    K.ts('dve', mk1, em, m1, None, ALU.is_equal, None, [r_rt], [r_rt])
                    K.stt('dve', em, mk1, -1e30, em, ALU.mult, ALU.add, [r_rt], [r_rt])
                    K.S.op('dve', (lambda o, i_: (lambda e: e.tensor_reduce(out=o, in_=i_, axis=mybir.AxisListType.X,
                                                                             op=ALU.max)))(m2, em), [r_rt], [r_rt])
                    K.ts('dve', em, em, m2, None, ALU.is_equal, None, [r_rt], [r_rt])
                    K.tt('dve', dd, m2, m1, ALU.subtract, [r_rt], [r_rt])
                    ee, wa, wb_ = rt[0:m, 51:52], rt[0:m, 52:53], rt[0:m, 53:54]
                    K.act(ee, dd, AF.Exp, [r_rt], [r_rt])
                    K.ts('dve', wa, ee, 1.0, None, ALU.add, None, [r_rt], [r_rt])
                    K.recip(wa, wa, [r_rt], [r_rt])
                    K.tt('dve', wb_, ee, wa, ALU.mult, [r_rt], [r_rt])
                    K.tt('dve', wa, wa, pgr, ALU.mult, [r_rt], [r_rt])
                    K.tt('dve', wb_, wb_, pgr, ALU.mult, [r_rt], [r_rt])
                    K.ts('dve', mk1, mk1, wa, None, ALU.mult, None, [r_rt], [r_rt])
                    K.stt('dve', mk1, em, wb_, mk1, ALU.mult, ALU.add, [r_rt], [r_rt])
                    K.tr(pr[0:16, 128:128 + m], mk1, C['ident_f'][0:m, 0:m], [r_rt, C['res']], [r_pr])
                    K.copy('act', wtsT[:, t0 + s0:t0 + s0 + m], pr[0:16, 128:128 + m], [r_pr], [r_wtsT[bi]])
            S.barrier()

        with ExitStack() as st2:
            h = K.sb(st2, [128, 8, n], F32, 'bh')
            r_h = [Res() for _ in blks]
            u2 = K.sb(st2, [128, 8, n], BF16, 'bu2')
            r_u2s = [Res() for _ in blks]
            for bi, (t0, nb, ty) in enumerate(blks):
                K.dma('sp', h[:, :, t0:t0 + nb], hmid_d[:, :, t0:t0 + nb], [r_hmid[bi]], [r_h[bi]])
                K.dma('act', u2[:, :, t0:t0 + nb], u2_d[:, :, t0:t0 + nb], [r_u2[bi]], [r_u2s[bi]])
            st2o = st2
            st2 = ExitStack()
            sel = K.sb(st2, [16, 16, 128], F32, 'sel')
            r_sel = Res()
            K.copy('dve', sel[:], C['ident_f'][0:16, 0:16].unsqueeze(2).to_broadcast([16, 16, 128]), [C['res']],
                   [r_sel])
            w1b = [K.sb(st2, [128, 8, 512], BF16, 'w1b%d' % i) for i in range(2)]
            w3b = [K.sb(st2, [128, 8, 512], BF16, 'w3b%d' % i) for i in range(2)]
            w2b = [K.sb(st2, [128, 4, D], BF16, 'w2b%d' % i) for i in range(2)]
            r_we = [Res(), Res()]
            wrep = [K.sb(st2, [128, 512], F32, 'wrep%d' % i) for i in range(2)]
            r_wrep = [Res(), Res()]
            sa = [K.sb(st2, [128, 512], F32, 'sa%d' % i) for i in range(2)]
            r_sa = [Res(), Res()]
            hid = [K.sb(st2, [128, 4, 512], BF16, 'hid%d' % i) for i in range(2)]
            r_hid = [Res(), Res()]
            pa = [K.ps(st2, [128, 512], F32, 'mpa%d' % i) for i in range(2)]
            r_pa = [Res(), Res()]
            pb2 = [K.ps(st2, [128, 512], F32, 'mpb%d' % i) for i in range(2)]
            r_pb2 = [Res(), Res()]
            py = [K.ps(st2, [128, 512], F32, 'mpy%d' % i) for i in range(2)]
            r_py = [Res(), Res()]
            pw = K.ps(st2, [128, 512], F32, 'mpw')
            r_pw = Res()
            cc = [0, 0]

            def stage1(e, bi, ib):
                ie = e % 2
                t0, nb, ty = blks[bi]
                if bi == 0:
                    K.dma('pool', w1b[ie][:], Wd['w1'][e].rearrange("(k p) n -> p k n", p=128), [], [r_we[ie]])
                    K.dma('pool', w3b[ie][:], Wd['w3'][e].rearrange("(k p) n -> p k n", p=128), [], [r_we[ie]])
                    K.dma('pool', w2b[ie][:], Wd['w2'][e].rearrange("(k p) n -> p k n", p=128), [], [r_we[ie]])
                K.mm(pw[:, 0:nb], sel[:, e, :], wtsT[:, t0:t0 + nb], True, True, [r_sel, r_wtsT[bi]], [r_pw])
                K.copy('act', wrep[ib][:, 0:nb], pw[:, 0:nb], [r_pw], [r_wrep[ib]])
                for hc in range(4):
                    i = cc[0] % 2
                    cc[0] += 1
                    for k in range(8):
                        K.mm(pa[i][:, 0:nb], w1b[ie][:, k, hc * 128:(hc + 1) * 128], u2[:, k, t0:t0 + nb],
                             k == 0, k == 7, [r_we[ie], r_u2s[bi]], [r_pa[i]])
                    for k in range(8):
                        K.mm(pb2[i][:, 0:nb], w3b[ie][:, k, hc * 128:(hc + 1) * 128], u2[:, k, t0:t0 + nb],
                             k == 0, k == 7, [r_we[ie], r_u2s[bi]], [r_pb2[i]])
                    K.act(sa[i][:, 0:nb], pa[i][:, 0:nb], AF.Silu, [r_pa[i]], [r_sa[i]])
                    K.tt('dve', sa[i][:, 0:nb], pb2[i][:, 0:nb], sa[i][:, 0:nb], ALU.mult, [r_pb2[i], r_sa[i]],
                         [r_sa[i]])
                    K.tt('pool' if hc % 2 else 'dve', hid[ib][:, hc, 0:nb], sa[i][:, 0:nb], wrep[ib][:, 0:nb],
                         ALU.mult, [r_sa[i], r_wrep[ib]], [r_hid[ib]])

            def stage2(e, bi, ib):
                ie = e % 2
                t0, nb, ty = blks[bi]
                for dc in range(8):
                    i = cc[1] % 2
                    cc[1] += 1
                    for hc in range(4):
                        K.mm(py[i][:, 0:nb], w2b[ie][:, hc, dc * 128:(dc + 1) * 128], hid[ib][:, hc, 0:nb],
                             hc == 0, hc == 3, [r_we[ie], r_hid[ib]], [r_py[i]])
                    K.stt('dve', h[:, dc, t0:t0 + nb], py[i][:, 0:nb], g2[:, dc, ty:ty + 1], h[:, dc, t0:t0 + nb],
                          ALU.mult, ALU.add, [r_py[i], r_mods, r_h[bi]], [r_h[bi]])

            items = [(e, bi) for e in range(16) for bi in range(len(blks))]
            for s_ in range(len(items) + 1):
                builders = []
                if s_ < len(items):
                    builders.append(lambda a=items[s_], ib=s_ % 2: stage1(a[0], a[1], ib))
                if s_ >= 1:
                    builders.append(lambda a=items[s_ - 1], ib=(s_ - 1) % 2: stage2(a[0], a[1], ib))
                S.run_streams(builders)
            S.barrier()
            st2.close()
            st2 = st2o
            if not last:
                for bi, (t0, nb, ty) in enumerate(blks):
                    K.dma('sp', out_dst(t0, nb), h[:, :, t0:t0 + nb], [r_h[bi]], [out_res])
            else:
                S.barrier()
                gz = K.sb(st2, [128, 2, 8, 1], F32, 'gz')
                r_gz = Res()
                K.copy('dve', gz[:, 0, :, 0], PRM('gfin'), [r_prm], [r_gz])
                K.memset('dve', gz[:, 1], 0.0, [r_gz])
                sq = K.sb(st2, [128, 8, 512], BF16, 'fsq')
                r_sq = Res()
                rstd = K.sb(st2, [128, 512], F32, 'frstd')
                r_rstd = Res()
                tmp = K.sb(st2, [128, 8, 512], F32, 'ftmp')
                r_tmp = Res()
                of = [K.sb(st2, [128, 8, 512], F32, 'fo%d' % i) for i in range(2)]
                r_of = [Res(), Res()]
                pw = K.ps(st2, [128, 512], F32, 'fpss')
                r_pw = Res()
                for bi, (t0, nb, ty) in enumerate(blks):
                    i = bi % 2
                    emit_norm_mod(K, C, h[:, :, t0:t0 + nb], r_h[bi], nb, 0, gz[:, 0], gz[:, 1], r_gz, sq, r_sq,
                                  pw, r_pw, rstd, r_rstd, tmp, r_tmp, of[i], r_of[i])
                    K.dma('sp', out_dst(t0, nb), of[i][:, :, 0:nb], [r_of[i]], [out_res])
            S.barrier()
        S.barrier()


NQ0 = NT // 4
NQ1 = SEQ // 4


def pack_Bu(inp, l, b, q, last):
    P = Pack()
    ca = inp['c_ctx'] if (not last and q == 0) else inp['c'][b]
    cT = np.stack([colT(inp['c'][b]), colT(ca)], axis=2).reshape(128, 16)
    P.add('cT', cT)
    P.add('adab', colT(inp['ada_b'][l]))
    P.add('gmix', colT(inp['norm_mix_g'][l]))
    P.add('gffn', colT(inp['norm_ffn_g'][l]))
    P.add('bm', colT(inp['b_merge'][l]))
    P.add('gfin', colT(inp['final_norm_g']))
    rb = np.concatenate([inp['moe_b_group'][l], inp['moe_b_expert'][l]])[None, :]
    P.add('rb', np.repeat(rb, 128, axis=0))
    return P


def build_A(l, off):
    nc = bass.Bass("TRN2", target_bir_lowering=False)
    hT_d = nc.dram_tensor("hT", [D, NT], F32, kind="ExternalInput").ap()
    adaw_d = nc.dram_tensor("adaw", [D, 2048], F32, kind="ExternalInput").ap()
    win_d = nc.dram_tensor("win", [D, A_NCOL], F32, kind="ExternalInput").ap()
    prm_d = nc.dram_tensor("prm", [128, off['_w']], F32, kind="ExternalInput").ap()
    rgw_d = nc.dram_tensor("rgw", [128, 512], F32, kind="ExternalInput").ap()
    wlr_d = nc.dram_tensor("wlr", [32, 128], F32, kind="ExternalInput").ap()
    ys_d = nc.dram_tensor("ys", [4, 128, NT], BF16, kind="ExternalOutput").ap()
    uT_d = nc.dram_tensor("uT_scr", [128, 8, NT], BF16).ap()
    with ExitStack() as st:
        K = KB(nc, st)
        S = K.S
        C = emit_consts(K, st)
        prm = K.sb(st, [128, off['_w']], F32, 'prm_sb')
        r_prm = Res()
        K.dma('sp', prm[:], prm_d, [], [r_prm])

        def PRM(name, rows=128):
            o, w = off[name]
            return prm[0:rows, o:o + w]
        heads = [dict(PRM=PRM, r_prm=r_prm, winv=win_d.rearrange("(k p) n -> p k n", p=128), rgw_d=rgw_d,
                      wlr_d=wlr_d, ys_dst=lambda branch, t0, nb: ys_d[branch, :, t0:t0 + nb])]
        emit_A(K, C, l, hT_d.rearrange("(k p) t -> p k t", p=128), adaw_d, heads, off, uT_d)
        S.barrier()
        S.replay()
    return nc


def build_B(l, last, n, offB):
    nc = bass.Bass("TRN2", target_bir_lowering=False)
    d = {}
    hT_d = nc.dram_tensor("hT", [D, n], F32, kind="ExternalInput").ap()
    ys_d = nc.dram_tensor("ysT", [2048, n], BF16, kind="ExternalInput").ap()
    d['adaw'] = nc.dram_tensor("adaw", [D, 6144], F32, kind="ExternalInput").ap()
    prm_d = nc.dram_tensor("prmB", [128, offB['_w']], F32, kind="ExternalInput").ap()
    d['wm'] = nc.dram_tensor("wm", [D, 4096], F32, kind="ExternalInput").ap()
    d['wbr'] = nc.dram_tensor("wbr", [2048, D], F32, kind="ExternalInput").ap()
    d['wo'] = nc.dram_tensor("wo", [D, D], F32, kind="ExternalInput").ap()
    d['wr'] = nc.dram_tensor("wr", [D, 20], F32, kind="ExternalInput").ap()
    d['w1'] = nc.dram_tensor("w1", [16, D, 512], F32, kind="ExternalInput").ap()
    d['w3'] = nc.dram_tensor("w3", [16, D, 512], F32, kind="ExternalInput").ap()
    d['w2'] = nc.dram_tensor("w2", [16, 512, D], F32, kind="ExternalInput").ap()
    out_d = nc.dram_tensor("outT", [D, n], F32, kind="ExternalOutput").ap()
    hmid_d = nc.dram_tensor("hmid_scr", [128, 8, n], F32).ap()
    u2_d = nc.dram_tensor("u2_scr", [128, 8, n], BF16).ap()
    hTv = hT_d.rearrange("(k p) t -> p k t", p=128)
    ysv = ys_d.rearrange("(j p) t -> p j t", p=128)
    outv = out_d.rearrange("(k p) t -> p k t", p=128)
    with ExitStack() as st:
        K = KB(nc, st)
        S = K.S
        C = emit_consts(K, st)
        prmB = K.sb(st, [128, offB['_w']], F32, 'prmB')
        r_prmB = Res()
        K.dma('sp', prmB[:], prm_d, [], [r_prmB])

        def PRMB(name, rows=128):
            o, w = offB[name]
            return prmB[0:rows, o:o + w]
        mods, r_mods = emit_B_mods(K, st, C, d['adaw'], PRMB, r_prmB)
        if last:
            blks = [(i * 512, 512, 0) for i in range(4)]
        else:
            blks = [(0, 256, 1), (256, 512, 0), (768, 512, 0), (1280, 512, 0), (1792, 320, 0)]
        emit_B(K, C, last, n, blks, mods, r_mods, PRMB, r_prmB, d,
               (lambda t0, nb: hTv[:, :, t0:t0 + nb]), [], (lambda t0, nb: ysv[:, :, t0:t0 + nb]),
               (lambda t0, nb: outv[:, :, t0:t0 + nb]), Res(), hmid_d, u2_d)
        S.barrier()
        S.replay()
    return nc


_NC_CACHE = {}


def run_A(inp, l, hT):
    in_maps = []
    off = None
    for core in range(8):
        b, hd = core // 4, core % 4
        P = pack_A(inp, l, b, hd)
        off = dict(P.off)
        off['_w'] = P.w
        in_maps.append({'hT': hT[b], 'adaw': np.ascontiguousarray(inp['ada_w'][l][:, 0:2048]),
                        'win': gather_w_in(inp['w_in'][l], hd), 'prm': P.build(),
                        'rgw': rg_gate_blockdiag(inp, l, hd), 'wlr': gla_wlr_pad(inp, l, hd)})
    key = ('A', l)
    if key not in _NC_CACHE:
        _NC_CACHE[key] = build_A(l, off)
    res = run_bass_kernel_spmd(_NC_CACHE[key], in_maps, core_ids=list(range(8)))
    out = []
    for b in range(2):
        ys = np.stack([np.asarray(res.results[b * 4 + hd]['ys']) for hd in range(4)], axis=1)
        out.append(ys.reshape(4, 512, NT))
    return out


def run_B(inp, l, last, hT, ysT):
    n = NQ1 if last else NQ0
    in_maps = []
    offB = None
    wr = np.ascontiguousarray(np.concatenate([inp['moe_w_group'][l], inp['moe_w_expert'][l]], axis=1))
    wbr = np.ascontiguousarray(inp['w_branch'][l].reshape(2048, D))
    for core in range(8):
        b, q = core // 4, core % 4
        t0 = (NCTX + q * n) if last else q * n
        P = pack_Bu(inp, l, b, q, last)
        offB = dict(P.off)
        offB['_w'] = P.w
        in_maps.append({'hT': np.ascontiguousarray(hT[b][:, t0:t0 + n]),
                        'ysT': np.ascontiguousarray(ysT[b][:, :, t0:t0 + n]).reshape(2048, n),
                        'adaw': inp['ada_w'][l], 'prmB': P.build(), 'wm': inp['w_merge'][l], 'wbr': wbr,
                        'wo': inp['w_out'][l], 'wr': wr, 'w1': inp['moe_w1'][l], 'w3': inp['moe_w3'][l],
                        'w2': inp['moe_w2'][l]})
    key = ('B', l, last)
    if key not in _NC_CACHE:
        _NC_CACHE[key] = build_B(l, last, n, offB)
    res = run_bass_kernel_spmd(_NC_CACHE[key], in_maps, core_ids=list(range(8)))
    return [np.asarray(res.results[c]['outT']) for c in range(8)]


def kernel(**inputs):
    inp = {k: np.asarray(v) for k, v in inputs.items()}
    hT = [np.ascontiguousarray(np.concatenate([inp['ctx'][b], inp['x'][b]], axis=0).T) for b in range(2)]
    out = np.zeros((2, SEQ, D), np.float32)
    for l in range(2):
        last = (l == 1)
        ys = run_A(inp, l, hT)
        o = run_B(inp, l, last, hT, ys)
        if not last:
            hT = [np.ascontiguousarray(np.concatenate(o[b * 4:(b + 1) * 4], axis=1)) for b in range(2)]
        else:
            for c in range(8):
                out[c // 4, (c % 4) * NQ1:(c % 4 + 1) * NQ1, :] = o[c].T
    return out
```
